# Optimizing a Trainium2 kernel written in Bass

```python
import math
import jax, jax.numpy as jnp
from jax import lax
import numpy as np

D_MODEL = 1024
BATCH = 8
SEQ = 4096
DEPTH = 2

CTX_LEN = 256
GRID_W = 64

HY_WIDTH = 512
SSM_WIDTH = 512
ATT_WIDTH = 512
D_MIX = HY_WIDTH + SSM_WIDTH + ATT_WIDTH

HY_SHORT = 3
HY_BANDS = 16
HY_EMB = 1 + 2 * HY_BANDS
HY_FILTER_HIDDEN = 64
HY_DECAY_TARGET = 1e-2
HY_FAST_PCT = 0.3
HY_SLOW_PCT = 1.5
HY_FILTER_OUT_SCALE = 0.05

SSM_HEADS = 8
SSM_HEADDIM = 64
SSM_GROUPS = 2
SSM_STATE = 128
SSM_CONV = 3
SSM_CHUNK = 128
SSM_CONV_DIM = SSM_WIDTH + 2 * SSM_GROUPS * SSM_STATE

ATT_HEADS = 8
ATT_KV_HEADS = 2
ATT_HEADDIM = 64
ATT_GROUP = ATT_HEADS // ATT_KV_HEADS
ATT_KV = ATT_KV_HEADS * ATT_HEADDIM
WINDOW = 128
ROPE_BASE = 10000.0

HY_IN = 4 * HY_WIDTH
SSM_IN = SSM_CONV_DIM + SSM_WIDTH + 2 * SSM_HEADS
ATT_IN = ATT_WIDTH + 2 * ATT_KV + ATT_WIDTH
N_IN = HY_IN + SSM_IN + ATT_IN

DEEPNORM_ALPHA = (2 * DEPTH) ** 0.25
DEEPNORM_BETA = (8 * DEPTH) ** -0.25
LN_EPS = 1e-6
RMS_EPS = 1e-5

kernel_name = "hymba_hyena_ssd_swa_prefix_dit"


def _layernorm(x):
    xf = x.astype(jnp.float32)
    mu = jnp.mean(xf, -1, keepdims=True)
    var = jnp.mean(jnp.square(xf - mu), -1, keepdims=True)
    return ((xf - mu) * lax.rsqrt(var + LN_EPS)).astype(x.dtype)


def _centred_dwconv(u, w, b):
    k = w.shape[0]
    pad = k // 2
    L = u.shape[1]
    up = jnp.pad(u, ((0, 0), (pad, pad), (0, 0)))
    out = up[:, 0:L] * w[0]
    for j in range(1, k):
        out = out + up[:, j:j + L] * w[j]
    return out + b


def _hyena_filters(L, w1, b1, w2, b2, w3, b3, freq, w_out):
    t = jnp.linspace(0.0, 1.0, L, dtype=jnp.float32)[:, None]
    w = 2.0 * math.pi * jnp.arange(L, dtype=jnp.float32)[:, None] / L
    f = jnp.linspace(1e-4, HY_BANDS - 1, HY_BANDS, dtype=jnp.float32)[None]
    z = jnp.concatenate([t, jnp.cos(f * w), -jnp.sin(f * w)], -1)
    h = jnp.sin(freq * (z @ w1 + b1))
    h = jnp.sin(freq * (h @ w2 + b2))
    h = jnp.sin(freq * (h @ w3 + b3))
    h = (h @ w_out).reshape(L, 2, 2, HY_WIDTH)
    max_decay = math.log(HY_DECAY_TARGET) / HY_FAST_PCT
    min_decay = math.log(HY_DECAY_TARGET) / HY_SLOW_PCT
    deltas = jnp.linspace(min_decay, max_decay, HY_WIDTH, dtype=jnp.float32)
    window = jnp.exp(-t * jnp.abs(deltas))
    h = h.astype(jnp.float32) * window[:, None, None, :]
    fwd, bwd = h[:, :, 0], h[:, :, 1]
    k_full = jnp.concatenate([fwd.at[0].add(bwd[0]), jnp.zeros_like(fwd[:1]), bwd[1:][::-1]], 0)
    return jnp.fft.rfft(k_full, axis=0)


def _long_conv(u, k_f, bias):
    L = u.shape[1]
    uf = u.astype(jnp.float32)
    y = jnp.fft.irfft(jnp.fft.rfft(uf, n=2 * L, axis=1) * k_f, n=2 * L, axis=1)[:, :L]
    return (y + uf * bias).astype(u.dtype)


def _hyena(u, conv_w, conv_b, filt_f, hy_bias):
    vxx = _centred_dwconv(u[..., :3 * HY_WIDTH], conv_w, conv_b)
    v, x1, x2 = jnp.split(vxx, 3, axis=-1)
    gate = u[..., 3 * HY_WIDTH:]
    y = x1 * _long_conv(v, filt_f[:, 0], hy_bias[0])
    y = x2 * _long_conv(y, filt_f[:, 1], hy_bias[1])
    return y * jax.nn.silu(gate)


def _ssd_scan(x, dt, a, B, C, init):
    b, L, h, p = x.shape
    n = B.shape[-1]
    nc = L // SSM_CHUNK
    f32 = jnp.float32
    xd = (x.astype(f32) * dt[..., None]).reshape(b, nc, SSM_CHUNK, h, p)
    da = (dt * a).reshape(b, nc, SSM_CHUNK, h)
    Bc = B.astype(f32).reshape(b, nc, SSM_CHUNK, h, n)
    Cc = C.astype(f32).reshape(b, nc, SSM_CHUNK, h, n)
    acs = jnp.cumsum(da, axis=2)
    seg = acs[:, :, :, None, :] - acs[:, :, None, :, :]
    lower = jnp.tril(jnp.ones((SSM_CHUNK, SSM_CHUNK), bool))[None, None, :, :, None]
    decay_ls = jnp.exp(jnp.where(lower, seg, -jnp.inf))
    y_diag = jnp.einsum("bclhn,bcshn,bclsh,bcshp->bclhp", Cc, Bc, decay_ls, xd)
    decay_to_end = jnp.exp(acs[:, :, -1:, :] - acs)
    chunk_states = jnp.einsum("bclhn,bclh,bclhp->bchpn", Bc, decay_to_end, xd)
    chunk_decay = jnp.exp(acs[:, :, -1, :])

    def step(s, inp):
        dec, st = inp
        return s * dec[:, :, None, None] + st, s

    final, states_in = lax.scan(step, init.astype(f32),
                                (jnp.moveaxis(chunk_decay, 1, 0), jnp.moveaxis(chunk_states, 1, 0)))
    states_in = jnp.moveaxis(states_in, 0, 1)
    y_off = jnp.einsum("bclhn,bchpn,bclh->bclhp", Cc, states_in, jnp.exp(acs))
    return (y_diag + y_off).reshape(b, L, h, p), final


def _ssd_prep(u, conv_w, conv_b, dt_bias, a_log):
    b, L, _ = u.shape
    gn = SSM_GROUPS * SSM_STATE
    xbc = jax.nn.silu(_centred_dwconv(u[..., :SSM_CONV_DIM], conv_w, conv_b))
    xs = xbc[..., :SSM_WIDTH].reshape(b, L, SSM_HEADS, SSM_HEADDIM)
    rep = SSM_HEADS // SSM_GROUPS
    Bh = jnp.repeat(xbc[..., SSM_WIDTH:SSM_WIDTH + gn].reshape(b, L, SSM_GROUPS, SSM_STATE), rep, axis=2)
    Ch = jnp.repeat(xbc[..., SSM_WIDTH + gn:].reshape(b, L, SSM_GROUPS, SSM_STATE), rep, axis=2)
    gate = u[..., SSM_CONV_DIM:SSM_CONV_DIM + SSM_WIDTH]
    dt_raw = u[..., SSM_CONV_DIM + SSM_WIDTH:].reshape(b, L, 2, SSM_HEADS)
    dt = jax.nn.softplus(dt_raw.astype(jnp.float32) + dt_bias.astype(jnp.float32))
    a = -jnp.exp(a_log.astype(jnp.float32))
    return xs, Bh, Ch, gate, dt, a


def _ssd_bidir(xs, Bh, Ch, dt, a, init_f, init_b):
    flip = lambda t: t[:, ::-1]
    y_f, s_f = _ssd_scan(xs, dt[:, :, 0], a[0], Bh, Ch, init_f)
    y_b, s_b = _ssd_scan(flip(xs), flip(dt[:, :, 1]), a[1], flip(Bh), flip(Ch), init_b)
    return y_f + flip(y_b), s_f, s_b


def _ssd_finish(y, xs, gate, d_skip, norm_w):
    b, L = xs.shape[:2]
    y = (y + xs.astype(jnp.float32) * d_skip[:, None]).reshape(b, L, SSM_WIDTH)
    y = y * jax.nn.silu(gate.astype(jnp.float32))
    yg = y.reshape(b, L, SSM_GROUPS, SSM_WIDTH // SSM_GROUPS)
    yg = yg * lax.rsqrt(jnp.mean(jnp.square(yg), -1, keepdims=True) + RMS_EPS)
    return yg.reshape(b, L, SSM_WIDTH).astype(gate.dtype) * norm_w


def _axial_angles(L):
    rows = L // GRID_W
    row = jnp.broadcast_to(jnp.arange(rows)[:, None], (rows, GRID_W)).reshape(L)
    col = jnp.broadcast_to(jnp.arange(GRID_W)[None, :], (rows, GRID_W)).reshape(L)
    nf = ATT_HEADDIM // 4
    inv = ROPE_BASE ** (-jnp.arange(nf, dtype=jnp.float32) / nf)
    ang = jnp.stack([row, col], -1).astype(jnp.float32)[:, :, None] * inv
    return jnp.cos(ang), jnp.sin(ang)


def _apply_rope(x, cos, sin):
    b, L, h, d = x.shape
    xr = x.astype(jnp.float32).reshape(b, L, h, 2, 2, d // 4)
    x1, x2 = xr[..., 0, :], xr[..., 1, :]
    c = cos[None, :, None]
    s = sin[None, :, None]
    out = jnp.stack([x1 * c - x2 * s, x1 * s + x2 * c], axis=-2)
    return out.reshape(b, L, h, d).astype(x.dtype)


def _attn_split(u):
    b, L, _ = u.shape
    q = u[..., :ATT_WIDTH].reshape(b, L, ATT_HEADS, ATT_HEADDIM)
    k = u[..., ATT_WIDTH:ATT_WIDTH + ATT_KV].reshape(b, L, ATT_KV_HEADS, ATT_HEADDIM)
    v = u[..., ATT_WIDTH + ATT_KV:ATT_WIDTH + 2 * ATT_KV].reshape(b, L, ATT_KV_HEADS, ATT_HEADDIM)
    gate = u[..., ATT_WIDTH + 2 * ATT_KV:]
    return q, k, v, gate


def _ctx_attention(q, k, v, sinks):
    b, L = q.shape[:2]
    scale = ATT_HEADDIM ** -0.5
    qg = q.reshape(b, L, ATT_KV_HEADS, ATT_GROUP, ATT_HEADDIM)
    s = jnp.einsum("bqhgd,bkhd->bhgqk", qg, k).astype(jnp.float32) * scale
    sink = jnp.broadcast_to(sinks.astype(jnp.float32).reshape(ATT_KV_HEADS, ATT_GROUP, 1, 1),
                            (b, ATT_KV_HEADS, ATT_GROUP, L, 1))
    p = jax.nn.softmax(jnp.concatenate([s, sink], -1), axis=-1)[..., :-1]
    o = jnp.einsum("bhgqk,bkhd->bqhgd", p.astype(v.dtype), v)
    return o.reshape(b, L, ATT_WIDTH)


def _window_attention(q, k, v, kc, vc, sinks):
    b, L = q.shape[:2]
    nb = L // WINDOW
    nloc = 3 * WINDOW
    lc = kc.shape[1]
    scale = ATT_HEADDIM ** -0.5
    qb = q.reshape(b, nb, WINDOW, ATT_KV_HEADS, ATT_GROUP, ATT_HEADDIM)
    pad = ((0, 0), (WINDOW, WINDOW), (0, 0), (0, 0))
    kp = jnp.pad(k, pad).reshape(b, nb + 2, WINDOW, ATT_KV_HEADS, ATT_HEADDIM)
    vp = jnp.pad(v, pad).reshape(b, nb + 2, WINDOW, ATT_KV_HEADS, ATT_HEADDIM)
    kband = jnp.concatenate([kp[:, :-2], kp[:, 1:-1], kp[:, 2:]], axis=2)
    vband = jnp.concatenate([vp[:, :-2], vp[:, 1:-1], vp[:, 2:]], axis=2)
    qi = jnp.arange(WINDOW)[:, None]
    kj = jnp.arange(nloc)[None, :] - WINDOW
    kpos = jnp.arange(nb)[:, None, None] * WINDOW + kj[None]
    mask = (jnp.abs(qi - kj) <= WINDOW)[None] & (kpos >= 0) & (kpos < L)
    s_loc = jnp.einsum("bnqhgd,bnkhd->bnhgqk", qb, kband).astype(jnp.float32) * scale
    s_loc = jnp.where(mask[None, :, None, None], s_loc, -jnp.inf)
    s_ctx = jnp.einsum("bnqhgd,bkhd->bnhgqk", qb, kc).astype(jnp.float32) * scale
    sink = jnp.broadcast_to(sinks.astype(jnp.float32).reshape(1, 1, ATT_KV_HEADS, ATT_GROUP, 1, 1),
                            (b, nb, ATT_KV_HEADS, ATT_GROUP, WINDOW, 1))
    p = jax.nn.softmax(jnp.concatenate([s_loc, s_ctx, sink], -1), axis=-1).astype(v.dtype)
    o = (jnp.einsum("bnhgqk,bnkhd->bnqhgd", p[..., :nloc], vband)
         + jnp.einsum("bnhgqk,bkhd->bnqhgd", p[..., nloc:nloc + lc], vc))
    return o.reshape(b, L, ATT_WIDTH)


def _modulate(x, shift, scale):
    return _layernorm(x) * (1.0 + scale) + shift


def setup_inputs(seed: int = 0) -> dict:
    key = jax.random.key(seed)
    ks = jax.random.split(key, 32)
    f32 = jnp.float32

    def nrm(k, shape, s):
        return jax.random.normal(k, shape, f32) * s

    dt0 = jnp.exp(jax.random.uniform(ks[20], (DEPTH, 2, SSM_HEADS), f32, math.log(1e-3), math.log(1e-1)))
    return {
        "x": nrm(ks[0], (BATCH, SEQ, D_MODEL), 1.0),
        "c": nrm(ks[1], (BATCH, D_MODEL), 1.0),
        "ctx": nrm(ks[2], (BATCH, CTX_LEN, D_MODEL), 1.0),
        "c_ctx": nrm(ks[3], (D_MODEL,), 1.0),
        "w_mod": nrm(ks[4], (DEPTH, D_MODEL, 3 * D_MODEL), D_MODEL ** -0.5),
        "b_mod": nrm(ks[5], (DEPTH, 3 * D_MODEL), 0.02),
        "w_in": nrm(ks[6], (DEPTH, D_MODEL, N_IN), D_MODEL ** -0.5),
        "hy_conv_w": nrm(ks[7], (DEPTH, HY_SHORT, 3 * HY_WIDTH), HY_SHORT ** -0.5),
        "hy_conv_b": nrm(ks[8], (DEPTH, 3 * HY_WIDTH), 0.02),
        "hy_f_w1": nrm(ks[9], (DEPTH, HY_EMB, HY_FILTER_HIDDEN), HY_EMB ** -0.5),
        "hy_f_b1": nrm(ks[10], (DEPTH, HY_FILTER_HIDDEN), 0.02),
        "hy_f_w2": nrm(ks[11], (DEPTH, HY_FILTER_HIDDEN, HY_FILTER_HIDDEN), HY_FILTER_HIDDEN ** -0.5),
        "hy_f_b2": nrm(ks[12], (DEPTH, HY_FILTER_HIDDEN), 0.02),
        "hy_f_w3": nrm(ks[13], (DEPTH, HY_FILTER_HIDDEN, HY_FILTER_HIDDEN), HY_FILTER_HIDDEN ** -0.5),
        "hy_f_b3": nrm(ks[14], (DEPTH, HY_FILTER_HIDDEN), 0.02),
        "hy_f_freq": 1.0 + nrm(ks[15], (DEPTH, HY_FILTER_HIDDEN), 0.05),
        "hy_f_wout": nrm(ks[16], (DEPTH, HY_FILTER_HIDDEN, 4 * HY_WIDTH), HY_FILTER_OUT_SCALE * HY_FILTER_HIDDEN ** -0.5),
        "hy_bias": nrm(ks[17], (DEPTH, 2, HY_WIDTH), 0.1),
        "ssm_conv_w": nrm(ks[18], (DEPTH, SSM_CONV, SSM_CONV_DIM), SSM_CONV ** -0.5),
        "ssm_conv_b": nrm(ks[19], (DEPTH, SSM_CONV_DIM), 0.02),
        "ssm_dt_bias": dt0 + jnp.log(-jnp.expm1(-dt0)),
        "ssm_a_log": jnp.log(jax.random.uniform(ks[21], (DEPTH, 2, SSM_HEADS), f32, 1.0, 16.0)),
        "ssm_d": 1.0 + nrm(ks[22], (DEPTH, SSM_HEADS), 0.1),
        "ssm_norm_w": 1.0 + nrm(ks[23], (DEPTH, SSM_WIDTH), 0.05),
        "attn_sinks": nrm(ks[24], (DEPTH, ATT_HEADS), 0.5),
        "w_out": nrm(ks[25], (DEPTH, D_MIX, D_MODEL), DEEPNORM_BETA * D_MIX ** -0.5),
        "ln_g": 1.0 + nrm(ks[26], (DEPTH, D_MODEL), 0.05),
        "ln_b": nrm(ks[27], (DEPTH, D_MODEL), 0.02),
    }


def reference(x, c, ctx, c_ctx, w_mod, b_mod, w_in, hy_conv_w, hy_conv_b, hy_f_w1, hy_f_b1,
              hy_f_w2, hy_f_b2, hy_f_w3, hy_f_b3, hy_f_freq, hy_f_wout, hy_bias, ssm_conv_w,
              ssm_conv_b, ssm_dt_bias, ssm_a_log, ssm_d, ssm_norm_w, attn_sinks, w_out, ln_g, ln_b):
    b, L, _ = x.shape
    lc = ctx.shape[1]
    cos, sin = _axial_angles(L)
    h_lat, h_ctx = x, ctx
    for i in range(DEPTH):
        ctx_needed = i < DEPTH - 1
        mod = jax.nn.silu(c) @ w_mod[i] + b_mod[i]
        mod_c = jax.nn.silu(c_ctx) @ w_mod[i] + b_mod[i]
        sh, sc, g = jnp.split(mod[:, None, :], 3, axis=-1)
        sh_c, sc_c, g_c = jnp.split(mod_c, 3, axis=-1)
        u = _modulate(h_lat, sh, sc) @ w_in[i]
        uc = _modulate(h_ctx, sh_c, sc_c) @ w_in[i]
        u_hy, u_ss, u_at = u[..., :HY_IN], u[..., HY_IN:HY_IN + SSM_IN], u[..., HY_IN + SSM_IN:]
        uc_hy, uc_ss, uc_at = uc[..., :HY_IN], uc[..., HY_IN:HY_IN + SSM_IN], uc[..., HY_IN + SSM_IN:]

        filt_args = (hy_f_w1[i], hy_f_b1[i], hy_f_w2[i], hy_f_b2[i], hy_f_w3[i], hy_f_b3[i],
                     hy_f_freq[i], hy_f_wout[i])
        y_hy = _hyena(u_hy, hy_conv_w[i], hy_conv_b[i], _hyena_filters(L, *filt_args), hy_bias[i])

        xs_c, B_c, C_c, z_c, dt_c, a = _ssd_prep(uc_ss, ssm_conv_w[i], ssm_conv_b[i], ssm_dt_bias[i], ssm_a_log[i])
        zero_state = jnp.zeros((b, SSM_HEADS, SSM_HEADDIM, SSM_STATE), jnp.float32)
        y_ss_c, s_f, s_b = _ssd_bidir(xs_c, B_c, C_c, dt_c, a, zero_state, zero_state)
        xs, Bh, Ch, z_s, dt, a = _ssd_prep(u_ss, ssm_conv_w[i], ssm_conv_b[i], ssm_dt_bias[i], ssm_a_log[i])
        y_ss, _, _ = _ssd_bidir(xs, Bh, Ch, dt, a, s_f, s_b)
        y_ss = _ssd_finish(y_ss, xs, z_s, ssm_d[i], ssm_norm_w[i])

        q, k, v, z_a = _attn_split(u_at)
        q, k = _apply_rope(q, cos, sin), _apply_rope(k, cos, sin)
        qc, kc, vc, z_ac = _attn_split(uc_at)
        y_at = _window_attention(q, k, v, kc, vc, attn_sinks[i]) * jax.nn.silu(z_a)

        out = jnp.concatenate([y_hy, y_ss, y_at], axis=-1) @ w_out[i]
        new_lat = _layernorm(DEEPNORM_ALPHA * h_lat + g * out) * ln_g[i] + ln_b[i]

        if ctx_needed:
            y_hy_c = _hyena(uc_hy, hy_conv_w[i], hy_conv_b[i], _hyena_filters(lc, *filt_args), hy_bias[i])
            y_ss_c = _ssd_finish(y_ss_c, xs_c, z_c, ssm_d[i], ssm_norm_w[i])
            y_at_c = _ctx_attention(qc, kc, vc, attn_sinks[i]) * jax.nn.silu(z_ac)
            out_c = jnp.concatenate([y_hy_c, y_ss_c, y_at_c], axis=-1) @ w_out[i]
            h_ctx = _layernorm(DEEPNORM_ALPHA * h_ctx + g_c * out_c) * ln_g[i] + ln_b[i]
        h_lat = new_lat
    return h_lat
```

```python
import math


import numpy as np
import concourse.bass as bass
import concourse.mybir as mybir
from concourse.bass_utils import run_bass_kernel_spmd

F32 = mybir.dt.float32
BF16 = mybir.dt.bfloat16
AF = mybir.ActivationFunctionType
ALU = mybir.AluOpType
AX = mybir.AxisListType

COMPUTE = ('pe', 'act', 'dve', 'pool')
SAME_ENGINE_SYNC = True
ALLK = '__all__'


class Buf:
    def __init__(self, t, name):
        self.t = t
        self.name = name
        self.st = {}
        self.is_psum = False

    def __getitem__(self, k):
        return self.t[k]

    def conflicts(self, key):
        if key == ALLK:
            return list(self.st.keys())
        r = []
        if key in self.st:
            r.append(key)
        if ALLK in self.st:
            r.append(ALLK)
        return r


def _norm(lst):
    out = []
    for x in lst:
        if x is None:
            continue
        if isinstance(x, Buf):
            out.append((x, ALLK))
        elif x[0].is_psum:
            out.append((x[0], ALLK))
        else:
            out.append((x[0], x[1]))
    return out


class Prog:
    def __init__(self, nc, n_dma=24):
        self.nc = nc
        self.streams = {e: [] for e in ('pe', 'act', 'dve', 'pool', 'sp')}
        self.know = {e: {} for e in self.streams}
        self.clock_ops = {}
        self.snap = {}
        self.n_dma = n_dma
        self.dma_next = 0
        self.bufs = []
        self._keep = []
        self.fence = None

    def _uniq(self, name):
        self._nid = getattr(self, '_nid', 0) + 1
        return "%s_%d" % (name, self._nid)

    def sbuf(self, name, shape, dtype):
        name = self._uniq(name)
        t = self.nc.alloc_sbuf_tensor(name, list(shape), dtype)
        b = Buf(t, name)
        self.bufs.append(b)
        return b

    def psum(self, name, shape, dtype=F32):
        name = self._uniq(name)
        t = self.nc.alloc_psum_tensor(name, list(shape), dtype)
        b = Buf(t, name)
        b.is_psum = True
        self.bufs.append(b)
        return b

    def dram(self, name, shape, dtype, kind="Internal"):
        t = self.nc.dram_tensor(name, list(shape), dtype, kind=kind)
        b = Buf(t, name)
        b.ap = t.ap()
        self.bufs.append(b)
        return b

    def _collect(self, reads, writes, multi, eng=None):
        deps = set()
        for (b, k) in reads:
            for kk in b.conflicts(k):
                deps.update(b.st[kk][0])
                if b.is_psum:
                    for c, i in b.st[kk][1].items():
                        if c != eng:
                            deps.add((c, i))
        for (b, k) in writes:
            for kk in b.conflicts(k):
                lw, rd = b.st[kk]
                if not multi:
                    deps.update(lw)
                for c, i in rd.items():
                    deps.add((c, i))
        return deps

    def _update(self, reads, writes, ev, multi):
        for (b, k) in reads:
            ent = b.st.setdefault(k, [set(), {}])
            c, i = ev
            if ent[1].get(c, 0) < i:
                ent[1][c] = i
        for (b, k) in writes:
            if multi and k in b.st:
                b.st[k][0].add(ev)
                b.st[k][1] = {}
            elif k == ALLK:
                b.st = {ALLK: [{ev}, {}]}
            else:
                b.st[k] = [{ev}, {}]

    def _issue(self, eng, fn, reads, writes, clock, is_dma, extra_deps=(), multi=False):
        reads = _norm(reads)
        writes = _norm(writes)
        deps = self._collect(reads, writes, multi, eng)
        deps.update(extra_deps)
        if self.fence is not None:
            deps.add(self.fence)
        K = self.know[eng]
        waits = []
        for (c, i) in sorted(deps):
            if (not is_dma) and c == eng and (eng == 'pe' or not SAME_ENGINE_SYNC):
                continue
            if K.get(c, 0) >= i:
                continue
            waits.append((c, i))
            self.clock_ops[c][i - 1]['signal'] = True
            for c2, i2 in self.snap[(c, i)].items():
                if K.get(c2, 0) < i2:
                    K[c2] = i2
        lst = self.clock_ops.setdefault(clock, [])
        idx = len(lst) + 1
        ev = (clock, idx)
        rec = dict(fn=fn, waits=waits, ev=ev, signal=is_dma, is_dma=is_dma)
        lst.append(rec)
        self.streams[eng].append(rec)
        s = dict(K)
        s[clock] = idx
        self.snap[ev] = s
        self._update(reads, writes, ev, multi)
        return ev

    def op(self, eng, fn, reads=(), writes=()):
        return self._issue(eng, fn, reads, writes, eng, False)

    def E(self, eng, meth, *args, reads=(), writes=(), **kw):
        def fn(e, meth=meth, args=args, kw=kw):
            return getattr(e, meth)(*args, **kw)
        return self._issue(eng, fn, reads, writes, eng, False)

    def barrier(self):
        deps = set()
        for c, lst in self.clock_ops.items():
            if lst:
                deps.add((c, len(lst)))
        self.fence = None
        ev = self._issue('sp', lambda e: e.nop(), (), (), 'sp', False, extra_deps=deps)
        self.fence = ev
        return ev

    def mark(self):
        return (self.nc.sbuf_base, self.nc.psum_base)

    def release(self, m):
        self.barrier()
        self.nc.sbuf_base, self.nc.psum_base = m

    def dma(self, out_ap, in_ap, reads=(), writes=(), eng='sp', multi=False, **kw):
        slot = 'd%d' % (self.dma_next % self.n_dma)
        self.dma_next += 1
        prev = len(self.clock_ops.get(slot, []))
        extra = [(slot, prev)] if prev > 0 else []

        def fn(e, out_ap=out_ap, in_ap=in_ap, kw=kw):
            return e.dma_start(out=out_ap, in_=in_ap, **kw)
        return self._issue(eng, fn, reads, writes, slot, True, extra, multi=multi)

    def emit(self):
        nc = self.nc
        deps = set()
        for c, lst in self.clock_ops.items():
            if lst:
                deps.add((c, len(lst)))
        K = self.know['sp']
        fin = []
        for (c, i) in sorted(deps):
            if K.get(c, 0) >= i:
                continue
            fin.append((c, i))
            self.clock_ops[c][i - 1]['signal'] = True
        sems = {}
        for c, lst in self.clock_ops.items():
            sems[c] = nc.alloc_semaphore(name='s_' + c)
            n = 0
            for rec in lst:
                if rec['signal']:
                    n += 16 if rec['is_dma'] else 1
                rec['val'] = n
        self.sems = sems

        def val(c, i):
            return self.clock_ops[c][i - 1]['val']

        def run(eng_obj, stream, tail=None):
            for rec in stream:
                for (c, i) in rec['waits']:
                    eng_obj.wait_ge(sems[c], val(c, i))
                ins = rec['fn'](eng_obj)
                if rec['signal']:
                    ins.then_inc(sems[rec['ev'][0]], 16 if rec['is_dma'] else 1)
            if tail:
                for (c, i) in tail:
                    eng_obj.wait_ge(sems[c], val(c, i))

        with nc.Block() as block:
            @block.tensor
            def _(e):
                run(e, self.streams['pe'])

            @block.scalar
            def _(e):
                run(e, self.streams['act'])

            @block.vector
            def _(e):
                run(e, self.streams['dve'])

            @block.gpsimd
            def _(e):
                run(e, self.streams['pool'])

            @block.sync
            def _(e):
                run(e, self.streams['sp'], fin)

    def stats(self):
        out = {}
        for e, s in self.streams.items():
            out[e] = (len(s), sum(len(r['waits']) for r in s))
        return out


def AP(t, offset, dims):
    if isinstance(t, Buf):
        t = t.t
    return bass.AP(t, offset, [list(d) for d in dims])


D = 1024; L = 4096; LC = 256; NIN = 4880; DEPTH = 2
HYW = 512
LN_EPS = 1e-6; RMS_EPS = 1e-5
ALPHA = (2 * DEPTH) ** 0.25
NFM = 3072
MAGIC = 12582912.0
STAGE = 9
EVAC_DVE_ONLY = False
NT_LAT = L // 512


def bf16_np(a):
    import ml_dtypes
    return np.asarray(a, dtype=np.float32).astype(ml_dtypes.bfloat16)


class MKBase:
    def __init__(self, dbg=(), ext=()):
        self.dbg = set(dbg)
        self.ext = set(ext)
        self.nc = bass.Bass("TRN2", target_bir_lowering=False)
        self.P = Prog(self.nc)
        self.inputs = {}
        self.consts = {}

    def inp(self, name, shape, dtype=F32):
        b = self.P.dram(name, shape, dtype, kind="ExternalInput")
        self.inputs[name] = b
        return b

    def const(self, name, arr, dtype=F32):
        self.consts[name] = arr
        return self.inp(name, arr.shape, dtype)

    def scratch(self, name, shape, dtype):
        kind = "ExternalOutput" if name in self.dbg else ("ExternalInput" if name in self.ext else "Internal")
        return self.P.dram(name, shape, dtype, kind=kind)

    def declare(self):
        P = self.P
        self.x = self.inp("x", [L, D])
        self.ctx = self.inp("ctx", [LC, D])
        self.cvec = self.inp("cvec", [128, 16])
        self.w_mod = self.inp("w_mod", [DEPTH, D, 3 * D])
        self.b_mod = self.inp("b_mod", [DEPTH, 3 * D])
        self.b_modT = self.inp("b_modT", [DEPTH, 128, 24])
        self.w_in = self.inp("w_in", [DEPTH, D, NIN])
        self.w_out = self.inp("w_out", [DEPTH, 1536, D])
        self.ln_g = self.inp("ln_g", [DEPTH, D])
        self.ln_b = self.inp("ln_b", [DEPTH, D])
        self.out = P.dram("out", [L, D], F32, kind="ExternalOutput")
        self.uT = self.scratch("uT", [NFM, L], BF16)
        self.u_sg = self.scratch("u_sg", [L, 512], BF16)
        self.u_dt = self.scratch("u_dt", [L, 16], F32)
        self.u_at = self.scratch("u_at", [L, 1280], BF16)
        self.ucT = self.scratch("ucT", [NFM, LC], BF16)
        self.uc_sg = self.scratch("uc_sg", [LC, 512], BF16)
        self.uc_dt = self.scratch("uc_dt", [LC, 16], F32)
        self.uc_at = self.scratch("uc_at", [LC, 1280], BF16)
        self.yT = self.scratch("yT", [1536, L], BF16)
        self.ycT = self.scratch("ycT", [1536, LC], BF16)
        self.h1 = self.scratch("h1", [L, D], F32)
        self.hc1 = self.scratch("hc1", [LC, D], F32)
        ident = np.eye(128, dtype=np.float32)
        self.identd = self.const("identd", bf16_np(ident), BF16)
        self.ident = P.sbuf("ident", [128, 128], BF16)
        P.dma(self.ident[:], self.identd.ap, writes=[self.ident])
        self.onesb = P.sbuf("onesb", [128, 128], BF16)
        P.E('pool', 'memset', self.onesb[:], 1.0, writes=[self.onesb])
        self.epsln = P.sbuf("epsln", [128, 1], F32)
        P.E('pool', 'memset', self.epsln[:], LN_EPS, writes=[self.epsln])

    def phase_mod(self, i):
        P = self.P
        if not hasattr(self, 'modc'):
            self.modc = [P.sbuf("modc%d" % k, [128, 24, 2], F32) for k in range(DEPTH)]
            self.g_bc = [P.sbuf("g_bc%d" % k, [128, D], F32) for k in range(DEPTH)]
            self.gc_bc = P.sbuf("gc_bc", [128, D], F32)
            self.scc = P.sbuf("scc", [128, 16], F32)
            cv = P.sbuf("cv", [128, 16], F32)
            P.dma(cv[:], self.cvec.ap, writes=[cv])
            P.E('act', 'activation', self.scc[:], cv[:], AF.Silu, reads=[cv], writes=[self.scc])
        m = P.mark()
        wm = P.sbuf("wm", [128, 8, 3 * D], F32)
        for kc in range(8):
            P.dma(wm[:, kc, :], self.w_mod.ap[i, kc * 128:(kc + 1) * 128, :], writes=[(wm, kc)])
        bT = P.sbuf("bT", [128, 24], F32)
        P.dma(bT[:], self.b_modT.ap[i], writes=[bT])
        ps = P.psum("mod_ps", [128, 24, 2], F32)
        modc = self.modc[i]
        for j in range(24):
            for kc in range(8):
                rhs = AP(self.scc, kc, [[16, 128], [8, 2]])
                P.E('pe', 'matmul', ps[:, j, :], wm[:, kc, j * 128:(j + 1) * 128], rhs,
                    start=(kc == 0), stop=(kc == 7), reads=[(wm, kc), self.scc], writes=[(ps, j)])
        P.E('dve', 'tensor_tensor', modc[:], ps[:], bT[:].unsqueeze(2).to_broadcast([128, 24, 2]), op=ALU.add,
            reads=[ps, bT], writes=[modc])
        P.E('dve', 'tensor_scalar_add', modc[:, 8:16, :], modc[:, 8:16, :], 1.0, reads=[modc], writes=[modc])
        brow = P.sbuf("brow", [128, D], F32)
        P.dma(brow[:], self.b_mod.ap[i:i + 1, 2 * D:3 * D].partition_broadcast(128), writes=[brow])
        for which in range(2 if i < DEPTH - 1 else 1):
            dst = self.g_bc[i] if which == 0 else self.gc_bc
            gps = P.psum("g_ps%d" % which, [128, 2, 512], F32)
            for n in range(2):
                for kc in range(8):
                    lhsT = AP(self.scc, which * 8 + kc, [[16, 128], [0, 128]])
                    P.E('pe', 'matmul', gps[:, n, :], lhsT, wm[:, kc, 2 * D + n * 512:2 * D + (n + 1) * 512],
                        start=(kc == 0), stop=(kc == 7), reads=[(wm, kc), self.scc], writes=[(gps, n)])
            P.E('dve', 'tensor_tensor', dst[:], gps[:].rearrange("p a b -> p (a b)"), brow[:], op=ALU.add,
                reads=[gps, brow], writes=[dst])
        P.release(m)

    def phase_in(self, i):
        P = self.P
        m = P.mark()
        modc = self.modc[i]
        Wb = P.sbuf("Wb", [128, 8, NIN], BF16)
        stg = [P.sbuf("wstg%d" % k, [128, NIN // 2], F32) for k in range(2)]
        n = 0
        for kc in range(8):
            for hf in range(2):
                st = stg[n % 2]; n += 1
                c0 = hf * (NIN // 2)
                P.dma(st[:], self.w_in.ap[i, kc * 128:(kc + 1) * 128, c0:c0 + NIN // 2], writes=[st])
                P.E('pool', 'tensor_copy', Wb[:, kc, c0:c0 + NIN // 2], st[:], reads=[st], writes=[(Wb, (kc, hf))])
        wall = [(Wb, (kc, hf)) for kc in range(8) for hf in range(2)]
        xin = [P.sbuf("xin%d" % k, [128, D], F32) for k in range(3)]
        xnb = [P.sbuf("xnb%d" % k, [128, D], BF16) for k in range(2)]
        stt = P.sbuf("stt", [128, 2, 6], F32)
        mv = P.sbuf("mv", [128, 2], F32)
        rstd = P.sbuf("rstd", [128, 1], F32)
        xmT = [P.sbuf("xmT%d" % k, [128, 8, 512], BF16) for k in range(2)]
        tps = [P.psum("tps%d" % k, [128, 8, 128], BF16) for k in range(2)]
        ops = [P.psum("ops%d" % k, [128, 512], F32) for k in range(4)]
        ost = [P.sbuf("ost%d" % k, [128, 512], BF16) for k in range(4)]
        dst_ = [P.sbuf("dst%d" % k, [128, 16], F32) for k in range(2)]
        cnt = dict(x=0, ev=0, o=0, d=0, t=0)

        def tile(src, ntok, tok0, col, uT, u_sg, u_dt, u_at, T):
            xm = xmT[T % 2]
            nsub = ntok // 128
            for s in range(nsub):
                xi = xin[cnt['x'] % 3]; xb = xnb[cnt['x'] % 2]; cnt['x'] += 1
                tp = tps[cnt['t'] % 2]; cnt['t'] += 1
                r0 = tok0 + s * 128
                P.dma(xi[:], src.ap[r0:r0 + 128, :], reads=[src], writes=[xi])
                for hf in range(2):
                    P.E('dve', 'bn_stats', stt[:, hf, :], xi[:, hf * 512:(hf + 1) * 512], reads=[xi], writes=[(stt, hf)])
                P.E('dve', 'bn_aggr', mv[:], stt[:], reads=[stt], writes=[mv])
                P.E('act', 'activation', rstd[:], mv[:, 1:2], AF.Sqrt, bias=self.epsln[:, 0:1], reads=[mv, self.epsln], writes=[rstd])
                P.E('dve', 'reciprocal', rstd[:], rstd[:], reads=[rstd], writes=[rstd])
                P.E('dve', 'tensor_scalar', xb[:], xi[:], mv[:, 0:1], rstd[:, 0:1], op0=ALU.subtract, op1=ALU.mult,
                    reads=[xi, mv, rstd], writes=[xb])
                if STAGE < 2: continue
                for kc in range(8):
                    P.E('pe', 'transpose', tp[:, kc, :], xb[:, kc * 128:(kc + 1) * 128], self.ident[:],
                        reads=[xb, self.ident], writes=[(tp, kc)])
                for kc in range(8):
                    o = xm[:, kc, s * 128:(s + 1) * 128]
                    sc1 = modc[:, 8 + kc, col:col + 1]; sh = modc[:, kc, col:col + 1]
                    if s % 2 == 0 or EVAC_DVE_ONLY:
                        P.E('dve', 'tensor_scalar', o, tp[:, kc, :], sc1, sh, op0=ALU.mult, op1=ALU.add,
                            reads=[(tp, kc), modc], writes=[(xm, (s, kc))])
                    else:
                        P.E('act', 'activation', o, tp[:, kc, :], AF.Identity, bias=sh, scale=sc1,
                            reads=[(tp, kc), modc], writes=[(xm, (s, kc))])
            if STAGE < 3: return
            for j in range(NFM // 128):
                op_ = ops[cnt['o'] % 4]; os_ = ost[cnt['o'] % 4]; cnt['o'] += 1
                for kc in range(8):
                    P.E('pe', 'matmul', op_[:, 0:ntok], Wb[:, kc, j * 128:(j + 1) * 128], xm[:, kc, 0:ntok],
                        start=(kc == 0), stop=(kc == 7), reads=wall + [xm], writes=[op_])
                if cnt['ev'] % 2 == 0:
                    P.E('act', 'copy', os_[:, 0:ntok], op_[:, 0:ntok], reads=[op_], writes=[os_])
                else:
                    P.E('dve', 'tensor_copy', os_[:, 0:ntok], op_[:, 0:ntok], reads=[op_], writes=[os_])
                cnt['ev'] += 1
                P.dma(uT.ap[j * 128:(j + 1) * 128, tok0:tok0 + ntok], os_[:, 0:ntok], reads=[os_],
                      writes=[(uT, j)], multi=True)
            if STAGE < 4: return
            groups = [(3072, 512, u_sg, 0), (3584, 16, u_dt, 0), (3600, 512, u_at, 0), (4112, 512, u_at, 512), (4624, 256, u_at, 1024)]
            for s in range(nsub):
                r0 = tok0 + s * 128
                for (c0, w, dstb, d0) in groups:
                    op_ = ops[cnt['o'] % 4]; os_ = ost[cnt['o'] % 4]; cnt['o'] += 1
                    for kc in range(8):
                        P.E('pe', 'matmul', op_[:, 0:w], xm[:, kc, s * 128:(s + 1) * 128], Wb[:, kc, c0:c0 + w],
                            start=(kc == 0), stop=(kc == 7), reads=wall + [xm], writes=[op_])
                    if dstb is u_dt:
                        ds_ = dst_[cnt['d'] % 2]; cnt['d'] += 1
                        P.E('dve', 'tensor_copy', ds_[:], op_[:, 0:16], reads=[op_], writes=[ds_])
                        P.dma(dstb.ap[r0:r0 + 128, :], ds_[:], reads=[ds_], writes=[(dstb, 0)], multi=True)
                    else:
                        if cnt['ev'] % 2 == 0:
                            P.E('act', 'copy', os_[:, 0:w], op_[:, 0:w], reads=[op_], writes=[os_])
                        else:
                            P.E('dve', 'tensor_copy', os_[:, 0:w], op_[:, 0:w], reads=[op_], writes=[os_])
                        cnt['ev'] += 1
                        P.dma(dstb.ap[r0:r0 + 128, d0:d0 + w], os_[:, 0:w], reads=[os_], writes=[(dstb, 0)], multi=True)

        hsrc = self.x if i == 0 else self.h1
        csrc = self.ctx if i == 0 else self.hc1
        tile(csrc, LC, 0, 1, self.ucT, self.uc_sg, self.uc_dt, self.uc_at, 0)
        for T in range(NT_LAT):
            tile(hsrc, 512, T * 512, 0, self.uT, self.u_sg, self.u_dt, self.u_at, T + 1)
        P.release(m)


NFFT = 8192
HY_BANDS = 16


def pap(buf, p0, off, dims):
    t = buf.t if isinstance(buf, Buf) else buf
    ps = t[:].ap[0][0]
    return bass.AP(t, p0 * ps + off, [[ps, dims[0]]] + [list(d) for d in dims[1:]])


def hyena_consts(Lh, sfx=""):
    N = 2 * Lh
    N1 = N // 64; NK = N1 // 2 + 1; NH = N1 // 2
    n1 = np.arange(N1)[:, None]; k1 = np.arange(NK)[None, :]
    th = 2 * np.pi * n1 * k1 / float(N1)
    F1 = np.concatenate([np.cos(th), -np.sin(th)], 1)
    n2 = np.arange(64)[:, None, None]; k1_ = np.arange(NK)[None, :, None]; k2 = np.arange(64)[None, None, :]
    th = 2 * np.pi * n2 * (k1_ + N1 * k2) / N
    Fr, Fi = np.cos(th), -np.sin(th)
    cb1 = np.concatenate([Fr, -Fi], 0); cb2 = np.concatenate([Fi, Fr], 0)
    S2c = np.concatenate([cb2, cb1, cb2], 2)
    k2 = np.arange(64)[:, None]; n2 = np.arange(64)[None, :]
    ph = 2 * np.pi * k2 * n2 / 64.0
    Gr, Gi = np.cos(ph), np.sin(ph)
    G = np.block([[Gr, Gi], [-Gi, Gr]])
    k1 = np.arange(NK)[:, None, None]; n2 = np.arange(64)[None, :, None]; n1 = np.arange(NH)[None, None, :]
    ps = 2 * np.pi * k1 * (64 * n1 + n2) / N
    w = np.full((NK, 1, 1), 2.0); w[0] = 1.0; w[NK - 1] = 1.0
    T4 = np.stack([w * np.cos(ps) / N, -w * np.sin(ps) / N], 1)
    t = np.linspace(0.0, 1.0, Lh, dtype=np.float32)[:, None]
    wv = (np.float32(2.0 * math.pi) * np.arange(Lh, dtype=np.float32)[:, None] / np.float32(Lh)).astype(np.float32)
    f = np.linspace(1e-4, HY_BANDS - 1, HY_BANDS, dtype=np.float32)[None]
    z = np.concatenate([t, np.cos(f * wv), -np.sin(f * wv)], -1).astype(np.float32)
    pos = np.arange(N); pos = np.where(pos <= Lh, pos, N - pos); pos[Lh] = 0
    zT = np.ascontiguousarray(z[pos].T)
    max_decay = math.log(1e-2) / 0.3; min_decay = math.log(1e-2) / 1.5
    deltas = np.linspace(min_decay, max_decay, HYW, dtype=np.float32)
    window = np.exp(-t * np.abs(deltas)[None, :]).astype(np.float32)
    wf = window[pos]; wf[Lh] = 0.0
    win = wf.reshape(N // 64, 64, HYW)
    d = dict(hF1=bf16_np(F1), hS2=bf16_np(S2c), hG=bf16_np(G), hT4=bf16_np(T4), hzT=zT.astype(np.float32), hwin=bf16_np(win))
    return {k + sfx: v for k, v in d.items()}


def conv_fm(P, u, w3, b, acc, out, Lx, func, cdeps=()):
    P.E('act', 'activation', acc[:, 0:Lx], u[:, 0:Lx], AF.Identity, bias=b, scale=w3[:, 1:2], reads=[u] + list(cdeps), writes=[acc])
    P.E('dve', 'scalar_tensor_tensor', acc[:, 1:Lx], u[:, 0:Lx - 1], w3[:, 0:1], acc[:, 1:Lx], op0=ALU.mult, op1=ALU.add,
        reads=[u, acc] + list(cdeps), writes=[acc])
    P.E('dve', 'scalar_tensor_tensor', acc[:, 0:Lx - 1], u[:, 1:Lx], w3[:, 2:3], acc[:, 0:Lx - 1], op0=ALU.mult, op1=ALU.add,
        reads=[u, acc] + list(cdeps), writes=[acc])
    P.E('act', 'activation', out[:, 0:Lx], acc[:, 0:Lx], func, reads=[acc], writes=[out])


class HyenaMixin:
    def declare_hyena(self):
        P = self.P
        self.hcfg = {}
        for Lh, sfx in ((L, ""), (LC, "c")):
            N = 2 * Lh
            hc = dict(Lh=Lh, N=N, N1=N // 64, NK=N // 128 + 1, NH=N // 128, sfx=sfx)
            for k, v in hyena_consts(Lh, sfx).items():
                hc[k[:len(k) - len(sfx)] if sfx else k] = self.const(k, v, F32 if k.startswith('hzT') else BF16)
            hc['Kf'] = self.scratch("Kf" + sfx, [2, 4, 2, 128, hc['NK'] * 128], BF16)
            self.hcfg[sfx] = hc
        self.hy_cw = self.inp("hy_cw", [DEPTH, 128, 12, 3])
        self.hy_cb = self.inp("hy_cb", [DEPTH, 128, 12])
        self.hy_b2 = self.inp("hy_b2", [DEPTH, 128, 4, 2])
        self.hy_w1 = self.inp("hy_w1", [DEPTH, 33, 128])
        self.hy_w23 = self.inp("hy_w23", [DEPTH, 2, 128, 128])
        self.hy_fb = self.inp("hy_fb", [DEPTH, 128, 4])
        self.hy_wo = self.inp("hy_wo", [DEPTH, 128, 2, 512])

    def hy_load_consts(self, hc):
        P = self.P
        N1, NK, NH = hc['N1'], hc['NK'], hc['NH']
        self.hc = hc
        self.cF1 = P.sbuf("cF1", [N1, 2 * NK], BF16); P.dma(self.cF1[:], hc['hF1'].ap, writes=[self.cF1])
        self.cS2 = P.sbuf("cS2", [128, NK, 192], BF16)
        for q in range(0, NK, 13):
            q1 = min(NK, q + 13)
            P.dma(self.cS2[:, q:q1, :], hc['hS2'].ap[:, q:q1, :], writes=[(self.cS2, q)])
        self.cG = P.sbuf("cG", [128, 128], BF16); P.dma(self.cG[:], hc['hG'].ap, writes=[self.cG])
        self.cT4 = P.sbuf("cT4", [NK, 2, 64, NH], BF16); P.dma(self.cT4[:], hc['hT4'].ap, writes=[self.cT4])

    def fft_fwd(self, lay, K, A, tagbufs, laydep=None):
        P = self.P
        NK = self.hc['NK']
        ps1 = tagbufs['ps1']
        n = 0
        for c0 in range(0, 128, 7):
            nch = min(7, 128 - c0)
            ps = ps1[n % 2]; n += 1
            for j in range(nch):
                lhsT = pap(lay, 0, c0 + j, [K, [128, 64]])
                P.E('pe', 'matmul', ps[0:64, j, :], lhsT, self.cF1[0:K, 0:NK], start=True, stop=True, tile_position=(0, 0),
                    reads=[laydep or lay, self.cF1], writes=[ps])
                P.E('pe', 'matmul', ps[64:128, j, :], lhsT, self.cF1[0:K, NK:2 * NK], start=True, stop=True, tile_position=(0, 64),
                    reads=[laydep or lay, self.cF1], writes=[ps])
            o_ = pap(A, 0, c0, [128, [1, nch], [128, NK]]); i_ = pap(ps, 0, 0, [128, [NK, nch], [1, NK]])
            if n % 2:
                P.E('dve', 'tensor_copy', o_, i_, reads=[ps], writes=[(A, c0)])
            else:
                P.E('act', 'copy', o_, i_, reads=[ps], writes=[(A, c0)])

    def phase_hyfilt(self, i, sfx=""):
        P = self.P
        hc = self.hcfg[sfx]
        Lh, NFFT, N1, NK, NH = hc['Lh'], hc['N'], hc['N1'], hc['NK'], hc['NH']
        m = P.mark()
        self.hy_load_consts(hc)
        Hb = P.sbuf("fHb", [128, NFFT], BF16)
        wo = P.sbuf("fwo", [128, 2, 512], F32); P.dma(wo[:], self.hy_wo.ap[i], writes=[wo])
        wob = P.sbuf("fwob", [128, 2, 512], BF16)
        P.E('pool', 'tensor_copy', wob[:], wo[:], reads=[wo], writes=[wob])
        m2 = P.mark()
        zT = P.sbuf("zT", [33, NFFT], F32)
        ZB = min(2048, NFFT)
        for q in range(NFFT // ZB):
            P.dma(zT[:, q * ZB:(q + 1) * ZB], hc['hzT'].ap[:, q * ZB:(q + 1) * ZB], writes=[(zT, q)])
        w1 = P.sbuf("fw1", [33, 128], F32); P.dma(w1[:], self.hy_w1.ap[i], writes=[w1])
        w23 = P.sbuf("fw23", [128, 2, 128], F32)
        for q in range(2):
            P.dma(w23[:, q, :], self.hy_w23.ap[i, q], writes=[(w23, q)])
        fb = P.sbuf("ffb", [128, 4], F32); P.dma(fb[:], self.hy_fb.ap[i], writes=[fb])
        sb = P.sbuf("fsb", [128, 4], F32)
        P.E('dve', 'tensor_scalar', sb[:, 0:1], fb[:, 0:1], float(1.0 / (2 * np.pi)), None, op0=ALU.mult, reads=[fb], writes=[sb])
        P.E('dve', 'tensor_scalar', sb[:, 1:4], fb[:, 1:4], sb[:, 0:1], None, op0=ALU.mult, reads=[fb, sb], writes=[sb])
        ha = P.sbuf("fha", [128, NFFT], F32)
        hb = P.sbuf("fhb", [128, NFFT], F32)
        t1 = [P.sbuf("ft1_%d" % k, [128, 512], F32) for k in range(2)]
        t2 = [P.sbuf("ft2_%d" % k, [128, 512], F32) for k in range(2)]
        pps = [P.psum("fps%d" % k, [128, 512], F32) for k in range(2)]
        n = 0
        for layer in range(3):
            src = zT if layer == 0 else (ha if layer == 1 else hb)
            dst = ha if layer != 1 else hb
            for blk in range(NFFT // 512):
                ps = pps[n % 2]; a = t1[n % 2]; d = t2[n % 2]; n += 1
                sl = slice(blk * 512, (blk + 1) * 512)
                if layer == 0:
                    P.E('pe', 'matmul', ps[:], w1[:], zT[:, sl], start=True, stop=True, reads=[w1, (zT, (blk * 512) // ZB)], writes=[ps])
                else:
                    P.E('pe', 'matmul', ps[:], w23[:, layer - 1, :], src[:, sl], start=True, stop=True,
                        reads=[w23, (src, blk)], writes=[ps])
                P.E('dve', 'tensor_scalar', a[:], ps[:], sb[:, 0:1], sb[:, 1 + layer:2 + layer], op0=ALU.mult, op1=ALU.add,
                    reads=[ps, sb], writes=[a])
                P.E('dve', 'tensor_scalar_add', d[:], a[:], MAGIC, reads=[a], writes=[d])
                P.E('dve', 'scalar_tensor_tensor', d[:], d[:], MAGIC, a[:], op0=ALU.subtract, op1=ALU.subtract,
                    reads=[a, d], writes=[d])
                P.E('act', 'activation', dst[:, sl], d[:], AF.Sin, scale=-6.28318, reads=[d], writes=[(dst, blk)])
        P.E('pool', 'memset', Hb[:], 0.0, writes=[Hb])
        P.E('act', 'copy', Hb[0:64, 0:Lh], ha[0:64, 0:Lh], reads=[ha, Hb], writes=[(Hb, 0)])
        P.E('dve', 'tensor_copy', Hb[64:128, Lh + 1:NFFT], ha[64:128, Lh + 1:NFFT], reads=[ha, Hb], writes=[(Hb, 1)])
        P.E('dve', 'tensor_copy', Hb[64:128, 0:1], ha[64:128, 0:1], reads=[ha, Hb], writes=[(Hb, 2)])
        P.release(m2)
        klay = P.sbuf("klay", [N1, 64, 128], BF16)
        wing = P.sbuf("wing", [N1, 64, 128], BF16)
        Af = P.sbuf("Af", [128, NK, 128], BF16)
        KA = P.sbuf("KA", [128, NK, 128], BF16)
        KB = P.sbuf("KB", [128, NK, 128], BF16)
        kps = [P.psum("kps%d" % k, [128, 4, 128], F32) for k in range(2)]
        ps1 = [P.psum("fps1_%d" % k, [128, 7, NK], F32) for k in range(2)]
        for g in range(4):
            P.dma(wing[:], hc['hwin'].ap[:, :, g * 128:(g + 1) * 128], reads=[hc['hwin']], writes=[wing])
            for cv in range(2):
                n = 0
                for q in range(16):
                    ps = kps[n % 2]; n += 1
                    for j in range(4):
                        n2 = q * 4 + j
                        P.E('pe', 'matmul', ps[0:N1, j, :], pap(Hb, 0, n2, [128, [64, N1]]), wob[:, cv, g * 128:(g + 1) * 128],
                            start=True, stop=True, reads=[Hb, wob], writes=[ps])
                    P.E('dve', 'tensor_tensor', klay[:, q * 4:q * 4 + 4, :], ps[0:N1, :, :], wing[:, q * 4:q * 4 + 4, :], op=ALU.mult,
                        reads=[ps, wing], writes=[(klay, q)])
                self.fft_fwd(klay, N1, Af, dict(ps1=ps1))
                n = 0
                for q in range(0, NK, 4):
                    nk = min(4, NK - q)
                    psa = kps[n % 2]; n += 1
                    for j in range(nk):
                        k1 = q + j
                        P.E('pe', 'matmul', psa[0:64, j, :], self.cS2[:, k1, 64:128], Af[:, k1, :], tile_position=(0, 0),
                            start=True, stop=True, reads=[self.cS2, Af], writes=[psa])
                        P.E('pe', 'matmul', psa[64:128, j, :], self.cS2[:, k1, 64:128], Af[:, k1, :], tile_position=(0, 64),
                            start=True, stop=True, reads=[self.cS2, Af], writes=[psa])
                    P.E('act', 'copy', KA[:, q:q + nk, :], psa[:, 0:nk, :], reads=[psa], writes=[(KA, q)])
                    psb = kps[n % 2]; n += 1
                    for j in range(nk):
                        k1 = q + j
                        P.E('pe', 'matmul', psb[0:64, j, :], self.cS2[:, k1, 0:64], Af[:, k1, :], tile_position=(0, 0),
                            start=True, stop=True, reads=[self.cS2, Af], writes=[psb])
                        P.E('pe', 'matmul', psb[64:128, j, :], self.cS2[:, k1, 0:64], Af[:, k1, :], tile_position=(0, 64),
                            start=True, stop=True, reads=[self.cS2, Af], writes=[psb])
                    P.E('dve', 'tensor_scalar', KB[0:64, q:q + nk, :], psb[0:64, 0:nk, :], -1.0, None, op0=ALU.mult,
                        reads=[psb], writes=[(KB, q)])
                    P.E('dve', 'tensor_copy', KB[64:128, q:q + nk, :], psb[64:128, 0:nk, :], reads=[psb], writes=[(KB, -q - 1)])
                P.dma(hc['Kf'].ap[cv, g, 0], KA[:].rearrange("p a b -> p (a b)"), reads=[KA], writes=[(hc['Kf'], (cv, g, 0))])
                P.dma(hc['Kf'].ap[cv, g, 1], KB[:].rearrange("p a b -> p (a b)"), reads=[KB], writes=[(hc['Kf'], (cv, g, 1))])
        P.release(m)

    def hy_conv(self, cv, g, xT, zT, bufs):
        P = self.P
        hc = self.hc
        NK, NH = hc['NK'], hc['NH']
        CK = min(13, NK)
        A, Y, Bt, KAc, KBc = bufs['A'], bufs['Y'], bufs['B'], bufs['KA'], bufs['KB']
        tp = bufs['tp']; n = 0
        for q in range(8):
            ps = tp[n % 2]; n += 1
            for j in range(8):
                n2 = q * 8 + j
                P.E('pe', 'transpose', ps[0:NH, j, :], pap(xT, 0, n2, [128, [64, NH]]), self.ident[:], reads=[xT, self.ident], writes=[ps])
            o_ = pap(Bt, 0, q * 1024, [NH, [128, 8], [1, 128]])
            if q % 2 == 0:
                P.E('act', 'copy', o_, ps[0:NH, :, :], reads=[ps], writes=[(Bt, ('lay', q))])
            else:
                P.E('dve', 'tensor_copy', o_, ps[0:NH, :, :], reads=[ps], writes=[(Bt, ('lay', q))])
        self.fft_fwd(Bt, NH, A, bufs, laydep=Bt)
        p2 = bufs['p2']; tm = bufs['tm']; n = 0
        Kf = hc['Kf']
        for k1 in range(NK):
            ch = k1 // CK
            KA = KAc[ch % 2]; KB = KBc[ch % 2]
            if k1 % CK == 0:
                P.dma(KA[:, 0:CK, :].rearrange("p a b -> p (a b)"), Kf.ap[cv, g, 0, :, ch * CK * 128:(ch + 1) * CK * 128], reads=[(Kf, (cv, g, 0))], writes=[KA])
                P.dma(KB[:, 0:CK, :].rearrange("p a b -> p (a b)"), Kf.ap[cv, g, 1, :, ch * CK * 128:(ch + 1) * CK * 128], reads=[(Kf, (cv, g, 1))], writes=[KB])
            kk = k1 % CK
            ps = p2[n % 2]; ta = tm[n % 3]; n += 1
            P.E('pe', 'matmul', ps[:, 0, :], self.cS2[:, k1, 64:192], A[:, k1, :], start=True, stop=True, reads=[self.cS2, A], writes=[ps])
            P.E('pe', 'matmul', ps[:, 1, :], self.cS2[:, k1, 0:128], A[:, k1, :], start=True, stop=True, reads=[self.cS2, A], writes=[ps])
            P.E('dve', 'tensor_tensor', ta[:, 0, :], ps[:, 0, :], KA[:, kk, :], op=ALU.mult, reads=[ps, KA], writes=[(ta, 0)])
            P.E('dve', 'tensor_tensor', ta[:, 1, :], ps[:, 1, :], KB[:, kk, :], op=ALU.mult, reads=[ps, KB], writes=[(ta, 1)])
            P.E('pool', 'tensor_tensor', Y[:, k1, :], ta[:, 0, :], ta[:, 1, :], op=ALU.add, reads=[ta], writes=[(Y, k1)])
        p3 = bufs['p3']; n = 0
        for c0 in range(0, 128, 4):
            ps = p3[n % 2]; n += 1
            for j in range(4):
                P.E('pe', 'matmul', ps[0:NK, j, :], pap(Y, 0, c0 + j, [128, [128, NK]]), self.cG[:], start=True, stop=True,
                    reads=[Y, self.cG], writes=[ps])
            o_ = pap(Bt, 0, c0, [NK, [1, 4], [128, 128]]); i_ = pap(ps, 0, 0, [NK, [128, 4], [1, 128]])
            if n % 2:
                P.E('act', 'copy', o_, i_, reads=[ps], writes=[(Bt, c0)])
            else:
                P.E('dve', 'tensor_copy', o_, i_, reads=[ps], writes=[(Bt, c0)])
        p4 = bufs['p4']; n = 0
        for q in range(8):
            ps = p4[n % 2]; n += 1
            for j in range(8):
                n2 = q * 8 + j
                for ri in range(2):
                    P.E('pe', 'matmul', ps[:, j, 0:NH], Bt[0:NK, ri * 64 + n2, :], self.cT4[0:NK, ri, n2, :], start=(ri == 0), stop=(ri == 1),
                        reads=[Bt, self.cT4], writes=[ps])
            o_ = pap(zT, 0, q * 8, [128, [1, 8], [64, NH]]); i_ = pap(ps, 0, 0, [128, [64, 8], [1, NH]])
            if q % 2:
                P.E('act', 'copy', o_, i_, reads=[ps], writes=[(zT, q)])
            else:
                P.E('dve', 'tensor_copy', o_, i_, reads=[ps], writes=[(zT, q)])

    def phase_hyena(self, i, sfx=""):
        P = self.P
        hc = self.hcfg[sfx]
        L, NK = hc['Lh'], hc['NK']
        uT_ = self.ucT if sfx else self.uT
        yT_ = self.ycT if sfx else self.yT
        m = P.mark()
        self.hy_load_consts(hc)
        cw = P.sbuf("hcw", [128, 12, 3], F32); P.dma(cw[:], self.hy_cw.ap[i], writes=[cw])
        cb = P.sbuf("hcb", [128, 12], F32); P.dma(cb[:], self.hy_cb.ap[i], writes=[cb])
        b2 = P.sbuf("hb2", [128, 4, 2], F32); P.dma(b2[:], self.hy_b2.ap[i], writes=[b2])
        uraw = P.sbuf("huraw", [128, L], BF16)
        acc = P.sbuf("hacc", [128, L], F32)
        vT = P.sbuf("hvT", [128, L], BF16); x1T = P.sbuf("hx1T", [128, L], BF16); x2T = P.sbuf("hx2T", [128, L], BF16)
        bufs = dict(A=P.sbuf("hA", [128, NK, 128], BF16), Y=P.sbuf("hY", [128, NK, 128], BF16),
                    B=P.sbuf("hB", [65, 128, 128], BF16), KA=[P.sbuf("hKA%d" % k, [128, 13, 128], BF16) for k in range(2)],
                    KB=[P.sbuf("hKB%d" % k, [128, 13, 128], BF16) for k in range(2)],
                    tm=[P.sbuf("htm%d" % k, [128, 2, 128], F32) for k in range(3)])
        pA = [P.psum("hpA%d" % k, [128, 512], F32) for k in range(2)]
        pT = [P.psum("hpT%d" % k, [128, 1024], BF16) for k in range(2)]
        bufs['tp'] = [PV(b, [64, 8, 128]) for b in pT]
        bufs['ps1'] = [PV(b, [128, 7, NK]) for b in pA]
        bufs['p2'] = [PV(b, [128, 2, 128]) for b in pA]
        bufs['p3'] = [PV(b, [128, 4, 128]) for b in pA]
        bufs['p4'] = [PV(b, [128, 8, 64]) for b in pA]
        for g in range(4 if HY_GROUPS is None else HY_GROUPS):
            for k, dstT in enumerate((vT, x1T, x2T)):
                ct = k * 4 + g
                P.dma(uraw[:], uT_.ap[ct * 128:(ct + 1) * 128, :], reads=[(uT_, ct)], writes=[uraw])
                conv_fm(P, uraw, cw[:, ct, :], cb[:, ct:ct + 1], acc, dstT, L, AF.Identity, cdeps=[cw, cb])
            self.hy_conv(0, g, vT, acc, bufs)
            P.E('dve', 'scalar_tensor_tensor', acc[:], vT[:], b2[:, g, 0:1], acc[:], op0=ALU.mult, op1=ALU.add, reads=[vT, acc, b2], writes=[acc])
            P.E('dve', 'tensor_tensor', vT[:], acc[:], x1T[:], op=ALU.mult, reads=[acc, x1T], writes=[vT])
            self.hy_conv(1, g, vT, acc, bufs)
            P.dma(uraw[:], uT_.ap[(12 + g) * 128:(13 + g) * 128, :], reads=[(uT_, 12 + g)], writes=[uraw])
            P.E('act', 'activation', x1T[:], uraw[:], AF.Silu, reads=[uraw], writes=[x1T])
            P.E('dve', 'scalar_tensor_tensor', acc[:], vT[:], b2[:, g, 1:2], acc[:], op0=ALU.mult, op1=ALU.add, reads=[vT, acc, b2], writes=[acc])
            P.E('dve', 'tensor_tensor', acc[:], acc[:], x2T[:], op=ALU.mult, reads=[acc, x2T], writes=[acc])
            P.E('dve', 'tensor_tensor', vT[:], acc[:], x1T[:], op=ALU.mult, reads=[acc, x1T], writes=[vT])
            P.dma(yT_.ap[g * 128:(g + 1) * 128, :], vT[:], reads=[vT], writes=[(yT_, g)])
        P.release(m)


HY_GROUPS = None


class PV(Buf):
    def __init__(self, base, shape):
        self.base = base
        self.name = base.name
        self.is_psum = True
        t = base.t
        ps = t[:].ap[0][0]
        self.shape = shape
        self.ps = ps
        self._t = t

    @property
    def st(self):
        return self.base.st

    @st.setter
    def st(self, v):
        self.base.st = v

    @property
    def t(self):
        return self._t

    def full(self):
        return self._view(self.shape[0], [slice(None)] * (len(self.shape) - 1))

    def _view(self, npart, idx, p0=0):
        strides = []
        s = 1
        for d in reversed(self.shape[1:]):
            strides.insert(0, s); s *= d
        off = 0; dims = []
        for st_, d, ix in zip(strides, self.shape[1:], idx):
            if isinstance(ix, int):
                off += st_ * ix
            else:
                a, b, _ = ix.indices(d)
                off += st_ * a; dims.append([st_, b - a])
        return bass.AP(self._t, p0 * self.ps + off, [[self.ps, npart]] + dims)

    def __getitem__(self, key):
        if not isinstance(key, tuple):
            key = (key,)
        key = list(key) + [slice(None)] * (len(self.shape) - len(key))
        pk = key[0]
        a, b, _ = pk.indices(self.shape[0]) if isinstance(pk, slice) else (pk, pk + 1, 1)
        return self._view(b - a, key[1:], a)


def ssd_consts():
    r = np.arange(128)[:, None]; c = np.arange(128)[None, :]
    tri = np.stack([(r <= c), (r > c), (r >= c), (r < c)], 1).astype(np.float32)
    return dict(stri=tri)

UPI, LOS, LOI, UPS = 0, 1, 2, 3


class SSDMixin:
    def declare_ssd(self):
        P = self.P
        self.stri = self.const("stri", ssd_consts()['stri'])
        self.ssm_cw = self.inp("ssm_cw", [DEPTH, 128, 8, 3])
        self.ssm_cb = self.inp("ssm_cb", [DEPTH, 128, 8])
        self.ssm_dtb = self.inp("ssm_dtb", [DEPTH, 16])
        self.ssm_alog = self.inp("ssm_alog", [DEPTH, 16])
        self.ssm_dd = self.inp("ssm_dd", [DEPTH, 8])
        self.ssm_nw = self.inp("ssm_nw", [DEPTH, 512])
        self.tri = P.sbuf("tri", [128, 4, 128], F32); P.dma(self.tri[:], self.stri.ap, writes=[self.tri])
        self.onesf = P.sbuf("onesf", [128, 128], F32); P.E('pool', 'memset', self.onesf[:], 1.0, writes=[self.onesf])
        self.Sf0 = P.sbuf("Sf0", [128, 512], F32); self.Sb0 = P.sbuf("Sb0", [128, 512], F32)
        self.one1 = P.sbuf("one1", [128, 1], F32); P.E('pool', 'memset', self.one1[:], 1.0, writes=[self.one1])
        self.epsr = P.sbuf("epsr", [128, 1], F32); P.E('pool', 'memset', self.epsr[:], RMS_EPS, writes=[self.epsr])

    def phase_ssd(self, i, ctxmode, want_y=True):
        P = self.P
        Lx = LC if ctxmode else L
        NC = Lx // 128
        uT = self.ucT if ctxmode else self.uT
        u_sg = self.uc_sg if ctxmode else self.u_sg
        u_dt = self.uc_dt if ctxmode else self.u_dt
        yT = self.ycT if ctxmode else self.yT
        m = P.mark()
        tri = self.tri
        cw = P.sbuf("scw", [128, 8, 3], F32); P.dma(cw[:], self.ssm_cw.ap[i], writes=[cw])
        cb = P.sbuf("scb", [128, 8], F32); P.dma(cb[:], self.ssm_cb.ap[i], writes=[cb])
        x_tm = P.sbuf("x_tm", [128, NC, 512], BF16)
        B_tm = P.sbuf("B_tm", [128, NC, 256], BF16)
        BT = P.sbuf("BT", [128, 2, Lx], BF16)
        CT = P.sbuf("CT", [128, 2, Lx], BF16)
        m1 = P.mark()
        uraw = P.sbuf("suraw", [128, Lx], BF16)
        acc = P.sbuf("sacc", [128, Lx], F32)
        xTc = P.sbuf("sxTc", [128, Lx], BF16)
        tpp = [P.psum("stp%d" % k, [128, 8, 128], BF16) for k in range(2)]
        n = 0
        for ct in range(8):
            P.dma(uraw[:], uT.ap[2048 + ct * 128:2048 + (ct + 1) * 128, :], reads=[(uT, 16 + ct)], writes=[uraw])
            if ct < 4:
                dst, dsl = xTc, xTc[:, :]
            elif ct < 6:
                dst, dsl = BT, BT[:, ct - 4, :]
            else:
                dst, dsl = CT, CT[:, ct - 6, :]
            P.E('act', 'activation', acc[:], uraw[:], AF.Identity, bias=cb[:, ct:ct + 1], scale=cw[:, ct, 1:2], reads=[uraw, cw, cb], writes=[acc])
            P.E('dve', 'scalar_tensor_tensor', acc[:, 1:Lx], uraw[:, 0:Lx - 1], cw[:, ct, 0:1], acc[:, 1:Lx], op0=ALU.mult, op1=ALU.add, reads=[uraw, acc, cw], writes=[acc])
            P.E('dve', 'scalar_tensor_tensor', acc[:, 0:Lx - 1], uraw[:, 1:Lx], cw[:, ct, 2:3], acc[:, 0:Lx - 1], op0=ALU.mult, op1=ALU.add, reads=[uraw, acc, cw], writes=[acc])
            P.E('act', 'activation', dsl, acc[:], AF.Silu, reads=[acc], writes=[(dst, ct)])
            if ct < 6:
                for c0 in range(0, NC, 8):
                    ncc = min(8, NC - c0)
                    ps = tpp[n % 2]; n += 1
                    for j in range(ncc):
                        P.E('pe', 'transpose', ps[:, j, :], dsl[:, (c0 + j) * 128:(c0 + j + 1) * 128], self.ident[:], reads=[(dst, ct), self.ident], writes=[ps])
                    if ct < 4:
                        o_ = pap(x_tm, 0, c0 * 512 + ct * 128, [128, [512, ncc], [1, 128]]); key = (x_tm, (ct, c0))
                    else:
                        o_ = pap(B_tm, 0, c0 * 256 + (ct - 4) * 128, [128, [256, ncc], [1, 128]]); key = (B_tm, (ct, c0))
                    if n % 2:
                        P.E('act', 'copy', o_, ps[:, 0:ncc, :], reads=[ps], writes=[key])
                    else:
                        P.E('dve', 'tensor_copy', o_, ps[:, 0:ncc, :], reads=[ps], writes=[key])
        P.release(m1)
        W8 = NC * 8
        dtr = P.sbuf("dtr", [128, NC, 16], F32)
        P.dma(dtr[:], u_dt.ap.rearrange("(c p) h -> p c h", p=128), reads=[u_dt], writes=[dtr])
        dtb = P.sbuf("dtb", [128, 16], F32); P.dma(dtb[:], self.ssm_dtb.ap[i:i + 1, :].partition_broadcast(128), writes=[dtb])
        alg = P.sbuf("alg", [128, 16], F32); P.dma(alg[:], self.ssm_alog.ap[i:i + 1, :].partition_broadcast(128), writes=[alg])
        dbc = P.sbuf("dbc", [128, 8], F32); P.dma(dbc[:], self.ssm_dd.ap[i:i + 1, :].partition_broadcast(128), writes=[dbc])
        nwb = P.sbuf("nwb", [128, 512], F32); P.dma(nwb[:], self.ssm_nw.ap[i:i + 1, :].partition_broadcast(128), writes=[nwb])
        P.E('act', 'activation', alg[:], alg[:], AF.Exp, reads=[alg], writes=[alg])
        P.E('dve', 'tensor_scalar', alg[:], alg[:], -1.0, None, op0=ALU.mult, reads=[alg], writes=[alg])
        P.E('dve', 'tensor_tensor', dtr[:], dtr[:], dtb[:].unsqueeze(1).to_broadcast([128, NC, 16]), op=ALU.add, reads=[dtr, dtb], writes=[dtr])
        P.E('act', 'activation', dtr[:], dtr[:], AF.Exp, reads=[dtr], writes=[dtr])
        P.E('act', 'activation', dtr[:], dtr[:], AF.Ln, bias=self.one1[:, 0:1], reads=[dtr, self.one1], writes=[dtr])
        dt = [P.sbuf("dt%d" % d, [128, W8], F32) for d in range(2)]
        da = [P.sbuf("da%d" % d, [128, W8], F32) for d in range(2)]
        for d in range(2):
            P.E('dve', 'tensor_copy', dt[d][:].rearrange("p (c h) -> p c h", h=8), dtr[:, :, d * 8:(d + 1) * 8], reads=[dtr], writes=[dt[d]])
            P.E('dve', 'tensor_tensor', da[d][:].rearrange("p (c h) -> p c h", h=8), dtr[:, :, d * 8:(d + 1) * 8],
                alg[:, d * 8:(d + 1) * 8].unsqueeze(1).to_broadcast([128, NC, 8]), op=ALU.mult, reads=[dtr, alg], writes=[da[d]])
        cps = [P.psum("cps%d" % k, [128, 512], F32) for k in range(4)]
        A = [P.sbuf("Acs%d" % d, [128, W8], F32) for d in range(2)]
        din = [P.sbuf("din%d" % d, [128, W8], F32) for d in range(2)]
        dtx = [P.sbuf("dtx%d" % d, [128, W8], F32) for d in range(2)]
        cd = [P.sbuf("cd%d" % d, [128, W8], F32) for d in range(2)]
        for d in range(2):
            pa, pt = cps[d * 2], cps[d * 2 + 1]
            P.E('pe', 'matmul', pa[:, 0:W8], tri[:, UPI if d == 0 else UPS, :], da[d][:], start=True, stop=True, reads=[tri, da[d]], writes=[pa])
            P.E('pe', 'matmul', pt[:, 0:W8], self.onesf[:], da[d][:], start=True, stop=True, reads=[self.onesf, da[d]], writes=[pt])
            P.E('dve', 'tensor_copy', A[d][:], pa[:, 0:W8], reads=[pa], writes=[A[d]])
            P.E('act', 'activation', cd[d][:], pt[:, 0:W8], AF.Exp, reads=[pt], writes=[cd[d]])
            P.E('dve', 'tensor_tensor', dtx[d][:], pt[:, 0:W8], A[d][:], op=ALU.subtract, reads=[pt, A[d]], writes=[dtx[d]])
            if d == 0:
                P.E('act', 'activation', din[d][:], A[d][:], AF.Exp, reads=[A[d]], writes=[din[d]])
                P.E('act', 'activation', dtx[d][:], dtx[d][:], AF.Exp, reads=[dtx[d]], writes=[dtx[d]])
            else:
                P.E('act', 'activation', din[d][:], dtx[d][:], AF.Exp, reads=[dtx[d]], writes=[din[d]])
                P.E('act', 'activation', dtx[d][:], A[d][:], AF.Exp, reads=[A[d], din[d]], writes=[dtx[d]])
            P.E('dve', 'tensor_tensor', dtx[d][:], dtx[d][:], dt[d][:], op=ALU.mult, reads=[dtx[d], dt[d]], writes=[dtx[d]])
        Sf = P.sbuf("Sf", [128, 512], F32); Sb = P.sbuf("Sb", [128, 512], F32)
        if ctxmode:
            P.E('pool', 'memset', Sf[:], 0.0, writes=[Sf]); P.E('pool', 'memset', Sb[:], 0.0, writes=[Sb])
        else:
            P.E('pool', 'tensor_copy', Sf[:], self.Sf0[:], reads=[self.Sf0], writes=[Sf])
            P.E('pool', 'tensor_copy', Sb[:], self.Sb0[:], reads=[self.Sb0], writes=[Sb])
        Sbin = P.sbuf("Sbin", [128, NC, 512], BF16)
        xde = [P.sbuf("xde%d" % k, [128, 512], BF16) for k in range(2)]

        def bc8(t, c):
            return pap(t, 0, c * 8, [128, [1, 8], [0, 64]])

        def v8(t):
            return t[:].rearrange("p (h d) -> p h d", d=64)

        def chunk_state(c, d, S, nn):
            xd_ = xde[nn % 2]
            P.E('dve', 'tensor_tensor', v8(xd_), x_tm[:, c, :].rearrange("p (h d) -> p h d", d=64), bc8(dtx[d], c), op=ALU.mult,
                reads=[x_tm, dtx[d]], writes=[xd_])
            ps = cps[nn % 2]
            for h in range(8):
                g = h // 4
                P.E('pe', 'matmul', ps[:, h * 64:(h + 1) * 64], B_tm[:, c, g * 128:(g + 1) * 128], xd_[:, h * 64:(h + 1) * 64],
                    start=True, stop=True, reads=[B_tm, xd_], writes=[ps])
            P.E('dve', 'tensor_tensor', v8(S), v8(S), bc8(cd[d], c), op=ALU.mult, reads=[S, cd[d]], writes=[S])
            P.E('dve', 'tensor_tensor', S[:], S[:], ps[:], op=ALU.add, reads=[S, ps], writes=[S])

        for nn, c in enumerate(range(NC - 1, -1, -1)):
            P.E('act', 'copy', Sbin[:, c, :], Sb[:], reads=[Sb], writes=[(Sbin, c)])
            chunk_state(c, 1, Sb, nn)
        if ctxmode:
            P.E('pool', 'tensor_copy', self.Sb0[:], Sb[:], reads=[Sb], writes=[self.Sb0])
        if want_y:
            gps = cps[2]; sgp = [cps[3]]
            m3 = P.mark()
            ydp = P.psum("ydp", [128, 512], F32)
            yop = [P.psum("yop%d" % d, [128, 512], F32) for d in range(2)]
            ytp = P.psum("ytp", [128, 4, 128], BF16)
            GTm = P.sbuf("GTm", [128, 2, 2, 128], BF16)
            xd = [P.sbuf("xd%d" % d, [128, 512], BF16) for d in range(2)]
            Sfb = P.sbuf("Sfb", [128, 512], BF16)
            rhsp = [P.sbuf("rhsp%d" % k, [128, 4, 128], F32) for k in range(2)]
            Ee = [P.sbuf("Ee%d" % k, [128, 4, 128], BF16) for k in range(2)]
            Mm = [P.sbuf("Mm%d" % k, [128, 4, 128], BF16) for k in range(2)]
            gt = [P.sbuf("gt%d" % k, [128, 512], BF16) for k in range(2)]
            sg = P.sbuf("ssg", [128, 512], F32)
            y = P.sbuf("sy", [128, 512], F32); t1 = P.sbuf("st1", [128, 512], F32); t2 = P.sbuf("st2", [128, 512], F32)
            ss = P.sbuf("sss", [128, 2], F32)
            yb = P.sbuf("syb", [128, 512], BF16)
            ysT = [P.sbuf("ysT%d" % k, [128, 4, 512], BF16) for k in range(2)]
            nb = 0
        for c in range(NC):
            if want_y:
                cs = slice(c * 128, (c + 1) * 128)
                P.dma(gt[c % 2][:], u_sg.ap[c * 128:(c + 1) * 128, :], reads=[u_sg], writes=[gt[c % 2]])
                for g in range(2):
                    P.E('pe', 'matmul', gps[:, g * 128:(g + 1) * 128], BT[:, g, cs], CT[:, g, cs], start=True, stop=True, reads=[BT, CT], writes=[gps])
                for d in range(2):
                    P.E('dve', 'tensor_tensor', GTm[:, d, :, :], gps[:, 0:256].rearrange("p (g l) -> p g l", g=2),
                        tri[:, UPI if d == 0 else LOI, :].unsqueeze(1).to_broadcast([128, 2, 128]), op=ALU.mult, reads=[gps, tri], writes=[(GTm, d)])
                    P.E('dve', 'tensor_tensor', v8(xd[d]), x_tm[:, c, :].rearrange("p (h d) -> p h d", d=64), bc8(dt[d], c), op=ALU.mult,
                        reads=[x_tm, dt[d]], writes=[xd[d]])
                P.E('act', 'copy', Sfb[:], Sf[:], reads=[Sf], writes=[Sfb])
                for g in range(2):
                    mms = []
                    for d in range(2):
                        rp = rhsp[nb % 2]; ee = Ee[nb % 2]; mm_ = Mm[nb % 2]; sp_ = sgp[0]; nb += 1
                        mms.append(mm_)
                        V = UPI if d == 0 else LOI
                        U = LOS if d == 0 else UPS
                        P.E('dve', 'tensor_tensor', rp[:], tri[:, V, :].unsqueeze(1).to_broadcast([128, 4, 128]),
                            pap(da[d], 0, c * 8 + g * 4, [128, [1, 4], [0, 128]]), op=ALU.mult, reads=[tri, da[d]], writes=[rp])
                        for hh in range(4):
                            P.E('pe', 'matmul', sp_[:, hh * 128:(hh + 1) * 128], tri[:, U, :], rp[:, hh, :], start=True, stop=True, reads=[tri, rp], writes=[sp_])
                        P.E('act', 'activation', ee[:].rearrange("p a b -> p (a b)"), sp_[:], AF.Exp, reads=[sp_], writes=[ee])
                        P.E('pool', 'tensor_tensor', mm_[:], ee[:], GTm[:, d, g, :].unsqueeze(1).to_broadcast([128, 4, 128]), op=ALU.mult,
                            reads=[ee, (GTm, d)], writes=[mm_])
                    for hh in range(4):
                        h = g * 4 + hh
                        for d in range(2):
                            P.E('pe', 'matmul', ydp[:, h * 64:(h + 1) * 64], mms[d][:, hh, :], xd[d][:, h * 64:(h + 1) * 64], start=(d == 0), stop=(d == 1),
                                reads=[mms[d], xd[d]], writes=[ydp])
                for d in range(2):
                    for h in range(8):
                        g = h // 4
                        rhs = Sfb[:, h * 64:(h + 1) * 64] if d == 0 else Sbin[:, c, h * 64:(h + 1) * 64]
                        P.E('pe', 'matmul', yop[d][:, h * 64:(h + 1) * 64], CT[:, g, cs], rhs, start=True, stop=True,
                            reads=[CT, Sfb if d == 0 else (Sbin, c)], writes=[yop[d]])
            chunk_state(c, 0, Sf, c)
            if want_y:
                P.E('dve', 'tensor_tensor', v8(t1), yop[0][:].rearrange("p (h d) -> p h d", d=64), bc8(din[0], c), op=ALU.mult, reads=[yop[0], din[0]], writes=[t1])
                P.E('dve', 'tensor_tensor', v8(t2), yop[1][:].rearrange("p (h d) -> p h d", d=64), bc8(din[1], c), op=ALU.mult, reads=[yop[1], din[1]], writes=[t2])
                P.E('dve', 'tensor_tensor', y[:], ydp[:], t1[:], op=ALU.add, reads=[ydp, t1], writes=[y])
                P.E('pool', 'tensor_tensor', y[:], y[:], t2[:], op=ALU.add, reads=[y, t2], writes=[y])
                P.E('pool', 'tensor_tensor', v8(t1), x_tm[:, c, :].rearrange("p (h d) -> p h d", d=64), pap(dbc, 0, 0, [128, [1, 8], [0, 64]]), op=ALU.mult,
                    reads=[x_tm, dbc, t1], writes=[t1])
                P.E('pool', 'tensor_tensor', y[:], y[:], t1[:], op=ALU.add, reads=[y, t1], writes=[y])
                P.E('act', 'activation', sg[:], gt[c % 2][:], AF.Silu, reads=[gt[c % 2]], writes=[sg])
                P.E('dve', 'tensor_tensor', y[:], y[:], sg[:], op=ALU.mult, reads=[y, sg], writes=[y])
                P.E('pool', 'tensor_tensor', t2[:], y[:], y[:], op=ALU.mult, reads=[y, t2], writes=[t2])
                P.E('dve', 'reduce_sum', ss[:], t2[:].rearrange("p (g f) -> p g f", g=2), axis=AX.X, reads=[t2], writes=[ss])
                P.E('act', 'activation', ss[:], ss[:], AF.Sqrt, bias=self.epsr[:, 0:1], scale=1.0 / 256.0, reads=[ss, self.epsr], writes=[ss])
                P.E('dve', 'reciprocal', ss[:], ss[:], reads=[ss], writes=[ss])
                P.E('dve', 'tensor_tensor', y[:].rearrange("p (g f) -> p g f", g=2), y[:].rearrange("p (g f) -> p g f", g=2),
                    pap(ss, 0, 0, [128, [1, 2], [0, 256]]), op=ALU.mult, reads=[y, ss], writes=[y])
                P.E('pool', 'tensor_tensor', yb[:], y[:], nwb[:], op=ALU.mult, reads=[y, nwb], writes=[yb])
                for ft in range(4):
                    P.E('pe', 'transpose', ytp[:, ft, :], yb[:, ft * 128:(ft + 1) * 128], self.ident[:], reads=[yb, self.ident], writes=[ytp])
                st_ = ysT[(c // 4) % 2]
                P.E('act', 'copy', st_[:, :, (c % 4) * 128:(c % 4 + 1) * 128], ytp[:], reads=[ytp], writes=[(st_, c % 4)])
                if c % 4 == 3 or c == NC - 1:
                    c4 = (c // 4) * 4
                    nt_ = (c - c4 + 1) * 128
                    for ft in range(4):
                        P.dma(yT.ap[512 + ft * 128:512 + (ft + 1) * 128, c4 * 128:c4 * 128 + nt_], st_[:, ft, 0:nt_], reads=[st_],
                              writes=[(yT, 4 + ft)], multi=True)
        if ctxmode:
            P.E('pool', 'tensor_copy', self.Sf0[:], Sf[:], reads=[Sf], writes=[self.Sf0])
        P.release(m)


PERM = [0, 4, 1, 5, 2, 6, 3, 7]


def attn_consts():
    t = np.arange(L)
    row = (t // 64).astype(np.float32); col = (t % 64).astype(np.float32)
    inv = (10000.0 ** (-np.arange(16, dtype=np.float32) / 16)).astype(np.float32)
    ang = np.stack([row[:, None] * inv[None], col[:, None] * inv[None]], 1).astype(np.float32)
    cs = np.stack([np.cos(ang), np.sin(ang)], 1).reshape(L, 2, 32)
    rope = cs.reshape(L // 128, 128, 2, 32).transpose(1, 0, 2, 3)
    r = np.arange(128)[:, None]; c = np.arange(128)[None, :]
    am = np.stack([(r >= c), (r <= c)], 1).astype(np.float32)
    return dict(arope=np.ascontiguousarray(rope).astype(np.float32), amask=bf16_np(am))


class AttnMixin:
    def declare_attn(self):
        c = attn_consts()
        self.arope = self.const("arope", c['arope'])
        self.amask = self.const("amask", c['amask'], BF16)
        self.sinks = self.inp("sinks", [DEPTH, 8])

    def phase_attn(self, i, do_ctx):
        P = self.P
        m = P.mark()
        NT = L // 128
        rope = P.sbuf("rope", [128, NT, 2, 32], F32); P.dma(rope[:], self.arope.ap, writes=[rope])
        msk = P.sbuf("amsk", [128, 2, 128], BF16); P.dma(msk[:], self.amask.ap, writes=[msk])
        esk = P.sbuf("esk", [128, 8], F32); P.dma(esk[:], self.sinks.ap[i:i + 1, :].partition_broadcast(128), writes=[esk])
        P.E('act', 'activation', esk[:], esk[:], AF.Exp, reads=[esk], writes=[esk])
        QT = P.sbuf("QT", [128, 4, L], BF16); KT = P.sbuf("KT", [128, L], BF16)
        Va = P.sbuf("Va", [128, NT, 2, 65], BF16)
        QcT = P.sbuf("QcT", [128, 4, LC], BF16); KcT = P.sbuf("KcT", [128, LC], BF16)
        Vc = P.sbuf("Vc", [128, 2, 2, 65], BF16)
        P.E('pool', 'memset', Va[:], 1.0, writes=[Va]); P.E('pool', 'memset', Vc[:], 1.0, writes=[Vc])
        uin = [P.sbuf("auin%d" % k, [128, 1280], BF16) for k in range(2)]
        qf = P.sbuf("aqf", [128, 640], F32)
        ta = [P.sbuf("ata%d" % k, [128, 320], F32) for k in range(4)]
        qr = P.sbuf("aqr", [128, 640], BF16)
        tp = [P.psum("atp%d" % k, [128, 5, 128], BF16) for k in range(2)]
        for t in range(NT + 2):
            isctx = t >= NT
            tt = t - NT if isctx else t
            src = self.uc_at if isctx else self.u_at
            ui = uin[t % 2]; ps = tp[t % 2]
            P.dma(ui[:], src.ap[tt * 128:(tt + 1) * 128, :], reads=[src], writes=[ui])
            if not isctx:
                P.E('act', 'copy', qf[:], ui[:, 0:640], reads=[ui], writes=[qf])
                xv = lambda off: pap(qf, 0, off, [128, [64, 10], [32, 2], [1, 16]])
                ov = lambda off: pap(qr, 0, off, [128, [64, 10], [32, 2], [1, 16]])
                tv = lambda k: ta[k][:].rearrange("p (h a f) -> p h a f", h=10, a=2)
                cs = lambda k: pap(rope, 0, (t * 2 + k) * 32, [128, [0, 10], [16, 2], [1, 16]])
                P.E('dve', 'tensor_tensor', tv(0), xv(0), cs(0), op=ALU.mult, reads=[qf, rope], writes=[ta[0]])
                P.E('dve', 'tensor_tensor', tv(1), xv(16), cs(1), op=ALU.mult, reads=[qf, rope], writes=[ta[1]])
                P.E('dve', 'tensor_tensor', ov(0), tv(0), tv(1), op=ALU.subtract, reads=[ta[0], ta[1]], writes=[(qr, 0)])
                P.E('pool', 'tensor_tensor', tv(2), xv(0), cs(1), op=ALU.mult, reads=[qf, rope], writes=[ta[2]])
                P.E('pool', 'tensor_tensor', tv(3), xv(16), cs(0), op=ALU.mult, reads=[qf, rope], writes=[ta[3]])
                P.E('pool', 'tensor_tensor', ov(16), tv(2), tv(3), op=ALU.add, reads=[ta[2], ta[3]], writes=[(qr, 1)])
                qsrc = qr
            else:
                qsrc = ui
            for j in range(5):
                P.E('pe', 'transpose', ps[:, j, :], qsrc[:, j * 128:(j + 1) * 128], self.ident[:], reads=[qsrc, self.ident], writes=[ps])
            Qd, Kd, Vd = (QcT, KcT, Vc) if isctx else (QT, KT, Va)
            Lq = LC if isctx else L
            P.E('act', 'copy', pap(Qd, 0, tt * 128, [128, [Lq, 4], [1, 128]]), ps[:, 0:4, :], reads=[ps], writes=[(Qd, tt)])
            P.E('act', 'copy', Kd[:, tt * 128:(tt + 1) * 128], ps[:, 4, :], reads=[ps], writes=[(Kd, tt)])
            P.E('pool', 'tensor_copy', Vd[:, tt, :, 0:64], ui[:, 640:768].rearrange("p (g d) -> p g d", g=2), reads=[ui, Vd], writes=[(Vd, tt)])
        scp = [P.psum("ascp%d" % k, [128, 4, 128], F32) for k in range(2)]
        opp = [P.psum("aopp%d" % k, [128, 4, 65], F32) for k in range(2)]
        ytp = P.psum("aytp", [128, 4, 128], BF16)
        PT = [P.sbuf("aPT%d" % k, [128, 4, 128], BF16) for k in range(10)]
        gt = [P.sbuf("agt%d" % k, [128, 512], BF16) for k in range(2)]
        den = P.sbuf("aden", [128, 8], F32)
        yat = P.sbuf("ayat", [128, 512], F32)
        sg = P.sbuf("asg", [128, 512], F32)
        yb = P.sbuf("ayb", [128, 512], BF16)
        yaT = [P.sbuf("ayaT%d" % k, [128, 4, 512], BF16) for k in range(2)]
        nsc = 0

        def qtile(isctx, i_):
            nonlocal nsc
            Qd = QcT if isctx else QT
            Lq = LC if isctx else L
            src = self.uc_at if isctx else self.u_at
            ydst = self.ycT if isctx else self.yT
            keys = []
            if not isctx:
                if i_ > 0: keys.append((KT, Va, i_ - 1, 0))
                keys.append((KT, Va, i_, None))
                if i_ < NT - 1: keys.append((KT, Va, i_ + 1, 1))
            keys += [(KcT, Vc, 0, None), (KcT, Vc, 1, None)]
            g_ = gt[i_ % 2]
            P.dma(g_[:], src.ap[i_ * 128:(i_ + 1) * 128, 768 + 0:768 + 512] if False else src.ap[i_ * 128:(i_ + 1) * 128, 768:1280], reads=[src], writes=[g_])
            for g in range(2):
                pts = []
                for kidx, (Kd, Vd, kt, mk_) in enumerate(keys):
                    sp_ = scp[nsc % 2]; nsc += 1
                    pt = PT[g * 5 + kidx]
                    pts.append(pt)
                    for j in range(4):
                        P.E('pe', 'matmul', sp_[:, j, :], Kd[g * 64:(g + 1) * 64, kt * 128:(kt + 1) * 128],
                            Qd[g * 64:(g + 1) * 64, j, i_ * 128:(i_ + 1) * 128], start=True, stop=True,
                            reads=[(Kd, kt), (Qd, i_)], writes=[sp_])
                    P.E('act', 'activation', pt[:], sp_[:], AF.Exp, scale=0.125, reads=[sp_], writes=[pt])
                    if mk_ is not None:
                        P.E('pool', 'tensor_tensor', pt[:], pt[:], msk[:, mk_, :].unsqueeze(1).to_broadcast([128, 4, 128]), op=ALU.mult,
                            reads=[pt, msk], writes=[pt])
                op_ = opp[g]
                for j in range(4):
                    for kidx, (Kd, Vd, kt, mk_) in enumerate(keys):
                        P.E('pe', 'matmul', op_[:, j, :], pts[kidx][:, j, :], Vd[:, kt, g, :], start=(kidx == 0), stop=(kidx == len(keys) - 1),
                            reads=[pts[kidx], (Vd, kt)], writes=[op_])
            for g in range(2):
                op_ = opp[g]
                P.E('dve', 'tensor_tensor', pap(den, 0, g, [128, [2, 4]]), pap(op_, 0, 64, [128, [65, 4]]), pap(esk, 0, g, [128, [2, 4]]), op=ALU.add,
                    reads=[op_, esk], writes=[(den, g)])
            P.E('dve', 'reciprocal', den[:], den[:], reads=[den], writes=[den])
            for g in range(2):
                op_ = opp[g]
                P.E('dve', 'tensor_tensor', pap(yat, 0, g * 64, [128, [128, 4], [1, 64]]), pap(op_, 0, 0, [128, [65, 4], [1, 64]]),
                    pap(den, 0, g, [128, [2, 4], [0, 64]]), op=ALU.mult, reads=[op_, den], writes=[(yat, g)])
            P.E('act', 'activation', sg[:], g_[:], AF.Silu, reads=[g_], writes=[sg])
            P.E('pool', 'tensor_tensor', yb[:], yat[:], sg[:], op=ALU.mult, reads=[yat, sg], writes=[yb])
            for ft in range(4):
                P.E('pe', 'transpose', ytp[:, ft, :], yb[:, ft * 128:(ft + 1) * 128], self.ident[:], reads=[yb, self.ident], writes=[ytp])
            st_ = yaT[(i_ // 4) % 2]
            P.E('act', 'copy', st_[:, :, (i_ % 4) * 128:(i_ % 4 + 1) * 128], ytp[:], reads=[ytp], writes=[(st_, i_ % 4)])
            last = (LC // 128 - 1) if isctx else (NT - 1)
            if i_ % 4 == 3 or i_ == last:
                c4 = (i_ // 4) * 4
                nt_ = (i_ - c4 + 1) * 128
                for ft in range(4):
                    P.dma(ydst.ap[1024 + ft * 128:1024 + (ft + 1) * 128, c4 * 128:c4 * 128 + nt_], st_[:, ft, 0:nt_], reads=[st_],
                          writes=[(ydst, 8 + ft)], multi=True)

        if do_ctx:
            for i_ in range(LC // 128):
                qtile(True, i_)
        for i_ in range(NT if ATT_TILES is None else ATT_TILES):
            qtile(False, i_)
        P.release(m)

    def phase_out(self, i, ctxmode):
        P = self.P
        m = P.mark()
        Lx = LC if ctxmode else L
        yT = self.ycT if ctxmode else self.yT
        hsrc = (self.ctx if i == 0 else self.hc1) if ctxmode else (self.x if i == 0 else self.h1)
        hdst = self.hc1 if ctxmode else (self.h1 if i < DEPTH - 1 else self.out)
        gbc = self.gc_bc if ctxmode else self.g_bc[i]
        Wo = P.sbuf("Wo", [128, 12, D], BF16)
        stg = [P.sbuf("wostg%d" % k, [128, D], F32) for k in range(2)]
        for kc in range(12):
            st = stg[kc % 2]
            P.dma(st[:], self.w_out.ap[i, kc * 128:(kc + 1) * 128, :], writes=[st])
            P.E('pool', 'tensor_copy', Wo[:, kc, :], st[:], reads=[st], writes=[(Wo, kc)])
        lng = P.sbuf("lng", [128, D], F32); P.dma(lng[:], self.ln_g.ap[i:i + 1, :].partition_broadcast(128), writes=[lng])
        lnb = P.sbuf("lnb", [128, D], F32); P.dma(lnb[:], self.ln_b.ap[i:i + 1, :].partition_broadcast(128), writes=[lnb])
        yin = [P.sbuf("yin%d" % k, [128, 12, 512], BF16) for k in range(2)]
        hin = [P.sbuf("hin%d" % k, [128, D], F32) for k in range(2)]
        ops = [P.psum("oops%d" % k, [128, 2, 512], F32) for k in range(2)]
        tt = P.sbuf("ott", [128, D], F32); rr = [P.sbuf("orr%d" % k, [128, D], F32) for k in range(2)]
        stt = P.sbuf("ostt", [128, 2, 6], F32); mv = P.sbuf("omv", [128, 2], F32); rstd = P.sbuf("orstd", [128, 1], F32)
        n = 0
        for T in range((Lx + 511) // 512):
            ntok = min(512, Lx - T * 512)
            yi = yin[T % 2]
            for kc in range(12):
                P.dma(yi[:, kc, 0:ntok], yT.ap[kc * 128:(kc + 1) * 128, T * 512:T * 512 + ntok], reads=[(yT, kc)], writes=[(yi, kc)])
            for s in range(ntok // 128):
                r0 = T * 512 + s * 128
                ps = ops[n % 2]; hi = hin[n % 2]; r_ = rr[n % 2]; n += 1
                P.dma(hi[:], hsrc.ap[r0:r0 + 128, :], reads=[hsrc], writes=[hi])
                for nn in range(2):
                    for kc in range(12):
                        P.E('pe', 'matmul', ps[:, nn, :], yi[:, kc, s * 128:(s + 1) * 128], Wo[:, kc, nn * 512:(nn + 1) * 512],
                            start=(kc == 0), stop=(kc == 11), reads=[(yi, kc), (Wo, kc)], writes=[ps])
                P.E('dve', 'tensor_tensor', tt[:], ps[:].rearrange("p a b -> p (a b)"), gbc[:], op=ALU.mult, reads=[ps, gbc], writes=[tt])
                P.E('dve', 'scalar_tensor_tensor', tt[:], hi[:], float(ALPHA), tt[:], op0=ALU.mult, op1=ALU.add, reads=[hi, tt], writes=[tt])
                for hf in range(2):
                    P.E('dve', 'bn_stats', stt[:, hf, :], tt[:, hf * 512:(hf + 1) * 512], reads=[tt], writes=[(stt, hf)])
                P.E('dve', 'bn_aggr', mv[:], stt[:], reads=[stt], writes=[mv])
                P.E('act', 'activation', rstd[:], mv[:, 1:2], AF.Sqrt, bias=self.epsln[:, 0:1], reads=[mv, self.epsln], writes=[rstd])
                P.E('dve', 'reciprocal', rstd[:], rstd[:], reads=[rstd], writes=[rstd])
                P.E('dve', 'tensor_scalar', r_[:], tt[:], mv[:, 0:1], rstd[:, 0:1], op0=ALU.subtract, op1=ALU.mult, reads=[tt, mv, rstd], writes=[r_])
                P.E('pool', 'tensor_tensor', r_[:], r_[:], lng[:], op=ALU.mult, reads=[r_, lng], writes=[r_])
                P.E('pool', 'tensor_tensor', r_[:], r_[:], lnb[:], op=ALU.add, reads=[r_, lnb], writes=[r_])
                P.dma(hdst.ap[r0:r0 + 128, :], r_[:], reads=[r_], writes=[(hdst, 0)], multi=True)
        P.release(m)


ATT_TILES = None


class MK(MKBase, HyenaMixin, SSDMixin, AttnMixin):
    def build(self, skip_ctx_hyena=False):
        P = self.P
        self.declare(); self.declare_hyena(); self.declare_ssd(); self.declare_attn()
        for i in range(DEPTH):
            last = i == DEPTH - 1
            self.phase_mod(i)
            self.phase_in(i)
            self.phase_hyfilt(i)
            self.phase_hyena(i)
            if not last:
                if skip_ctx_hyena:
                    m = P.mark()
                    z = P.sbuf("zfill", [128, LC], BF16)
                    P.E('pool', 'memset', z[:], 0.0, writes=[z])
                    for g in range(4):
                        P.dma(self.ycT.ap[g * 128:(g + 1) * 128, :], z[:], reads=[z], writes=[(self.ycT, g)])
                    P.release(m)
                else:
                    self.phase_hyfilt(i, "c")
                    self.phase_hyena(i, "c")
            self.phase_ssd(i, True, want_y=not last)
            self.phase_ssd(i, False)
            self.phase_attn(i, do_ctx=not last)
            if not last:
                self.phase_out(i, True)
            self.phase_out(i, False)
        P.emit()
        return self


def blockdiag2(w):
    z = np.zeros((128, 128), np.float32); z[:64, :64] = w; z[64:, 64:] = w; return z
def host_inputs(inp, b):
    d = {}
    d['x'] = np.ascontiguousarray(inp['x'][b]); d['ctx'] = np.ascontiguousarray(inp['ctx'][b])
    cv = np.zeros((128, 16), np.float32)
    cv[:, 0:8] = inp['c'][b].reshape(8, 128).T; cv[:, 8:16] = inp['c_ctx'].reshape(8, 128).T
    d['cvec'] = cv
    d['w_mod'] = inp['w_mod']; d['b_mod'] = inp['b_mod']
    d['b_modT'] = np.ascontiguousarray(inp['b_mod'].reshape(DEPTH, 24, 128).transpose(0, 2, 1))
    PERM = [0, 4, 1, 5, 2, 6, 3, 7]
    hp = np.concatenate([np.arange(h * 64, (h + 1) * 64) for h in PERM])
    w_in = inp['w_in'].copy()
    w_in[:, :, 3600:4112] = inp['w_in'][:, :, 3600 + hp]
    w_in[:, :, 4368:4880] = inp['w_in'][:, :, 4368 + hp]
    w_out = inp['w_out'].copy()
    w_out[:, 1024:1536] = inp['w_out'][:, 1024 + hp]
    d['w_in'] = w_in; d['w_out'] = w_out
    d['sinks'] = np.ascontiguousarray(inp['attn_sinks'][:, PERM]); d['ln_g'] = inp['ln_g']; d['ln_b'] = inp['ln_b']
    d['hy_cw'] = np.ascontiguousarray(inp['hy_conv_w'].reshape(DEPTH, 3, 12, 128).transpose(0, 3, 2, 1))
    d['hy_cb'] = np.ascontiguousarray(inp['hy_conv_b'].reshape(DEPTH, 12, 128).transpose(0, 2, 1))
    d['hy_b2'] = np.ascontiguousarray(inp['hy_bias'].reshape(DEPTH, 2, 4, 128).transpose(0, 3, 2, 1))
    d['hy_w1'] = np.ascontiguousarray(np.concatenate([inp['hy_f_w1'], inp['hy_f_w1']], 2))
    d['hy_w23'] = np.stack([np.stack([blockdiag2(inp['hy_f_w2'][i]), blockdiag2(inp['hy_f_w3'][i])]) for i in range(DEPTH)])
    fb = np.stack([inp['hy_f_freq'], inp['hy_f_b1'], inp['hy_f_b2'], inp['hy_f_b3']], -1)
    d['hy_fb'] = np.ascontiguousarray(np.concatenate([fb, fb], 1))
    wo = inp['hy_f_wout'].reshape(DEPTH, 64, 2, 2, 512)
    d['hy_wo'] = np.ascontiguousarray(np.concatenate([wo[:, :, :, 0], wo[:, :, :, 1]], 1))
    d['ssm_cw'] = np.ascontiguousarray(inp['ssm_conv_w'].reshape(DEPTH, 3, 8, 128).transpose(0, 3, 2, 1))
    d['ssm_cb'] = np.ascontiguousarray(inp['ssm_conv_b'].reshape(DEPTH, 8, 128).transpose(0, 2, 1))
    d['ssm_dtb'] = np.ascontiguousarray(inp['ssm_dt_bias'].reshape(DEPTH, 16))
    d['ssm_alog'] = np.ascontiguousarray(inp['ssm_a_log'].reshape(DEPTH, 16))
    d['ssm_dd'] = inp['ssm_d']; d['ssm_nw'] = inp['ssm_norm_w']
    return d


_PROG = {}
SKIP_CTX_HYENA = False


def kernel(**inputs):
    inp = {k: np.asarray(v) for k, v in inputs.items()}
    if 'mk' not in _PROG:
        _PROG['mk'] = MK().build(skip_ctx_hyena=SKIP_CTX_HYENA)
    mk = _PROG['mk']
    nb = inp['x'].shape[0]
    in_maps = []
    for b in range(nb):
        d = host_inputs(inp, b)
        d.update(mk.consts)
        in_maps.append({k: np.ascontiguousarray(v) for k, v in d.items() if k in mk.inputs})
    res = run_bass_kernel_spmd(mk.nc, in_maps, core_ids=list(range(nb)))
    return np.stack([np.asarray(r['out'], dtype=np.float32) for r in res.results], 0)
```

```python
import math


import numpy as np
import concourse.bass as bass
import concourse.mybir as mybir
from concourse.bass_utils import run_bass_kernel_spmd

F32 = mybir.dt.float32
BF16 = mybir.dt.bfloat16
AF = mybir.ActivationFunctionType
ALU = mybir.AluOpType
AX = mybir.AxisListType

COMPUTE = ('pe', 'act', 'dve', 'pool')
SAME_ENGINE_SYNC = True
ALLK = '__all__'


class Buf:
    def __init__(self, t, name):
        self.t = t
        self.name = name
        self.st = {}
        self.is_psum = False

    def __getitem__(self, k):
        return self.t[k]

    def conflicts(self, key):
        if key == ALLK:
            return list(self.st.keys())
        r = []
        if key in self.st:
            r.append(key)
        if ALLK in self.st:
            r.append(ALLK)
        return r


def _norm(lst):
    out = []
    for x in lst:
        if x is None:
            continue
        if isinstance(x, Buf):
            out.append((x, ALLK))
        elif x[0].is_psum:
            out.append((x[0], ALLK))
        else:
            out.append((x[0], x[1]))
    return out


class Prog:
    def __init__(self, nc, n_dma=24):
        self.nc = nc
        self.streams = {e: [] for e in ('pe', 'act', 'dve', 'pool', 'sp')}
        self.know = {e: {} for e in self.streams}
        self.clock_ops = {}
        self.snap = {}
        self.n_dma = n_dma
        self.dma_next = 0
        self.bufs = []
        self._keep = []
        self.fence = None

    def _uniq(self, name):
        self._nid = getattr(self, '_nid', 0) + 1
        return "%s_%d" % (name, self._nid)

    def sbuf(self, name, shape, dtype):
        name = self._uniq(name)
        t = self.nc.alloc_sbuf_tensor(name, list(shape), dtype)
        b = Buf(t, name)
        self.bufs.append(b)
        return b

    def psum(self, name, shape, dtype=F32):
        name = self._uniq(name)
        t = self.nc.alloc_psum_tensor(name, list(shape), dtype)
        b = Buf(t, name)
        b.is_psum = True
        self.bufs.append(b)
        return b

    def dram(self, name, shape, dtype, kind="Internal"):
        t = self.nc.dram_tensor(name, list(shape), dtype, kind=kind)
        b = Buf(t, name)
        b.ap = t.ap()
        self.bufs.append(b)
        return b

    def _collect(self, reads, writes, multi, eng=None):
        deps = set()
        for (b, k) in reads:
            for kk in b.conflicts(k):
                deps.update(b.st[kk][0])
                if b.is_psum:
                    for c, i in b.st[kk][1].items():
                        if c != eng:
                            deps.add((c, i))
        for (b, k) in writes:
            for kk in b.conflicts(k):
                lw, rd = b.st[kk]
                if not multi:
                    deps.update(lw)
                for c, i in rd.items():
                    deps.add((c, i))
        return deps

    def _update(self, reads, writes, ev, multi):
        for (b, k) in reads:
            ent = b.st.setdefault(k, [set(), {}])
            c, i = ev
            if ent[1].get(c, 0) < i:
                ent[1][c] = i
        for (b, k) in writes:
            if multi and k in b.st:
                b.st[k][0].add(ev)
                b.st[k][1] = {}
            elif k == ALLK:
                b.st = {ALLK: [{ev}, {}]}
            else:
                b.st[k] = [{ev}, {}]

    def _issue(self, eng, fn, reads, writes, clock, is_dma, extra_deps=(), multi=False):
        reads = _norm(reads)
        writes = _norm(writes)
        deps = self._collect(reads, writes, multi, eng)
        deps.update(extra_deps)
        if self.fence is not None:
            deps.add(self.fence)
        K = self.know[eng]
        waits = []
        for (c, i) in sorted(deps):
            if (not is_dma) and c == eng and (eng == 'pe' or not SAME_ENGINE_SYNC):
                continue
            if K.get(c, 0) >= i:
                continue
            waits.append((c, i))
            self.clock_ops[c][i - 1]['signal'] = True
            for c2, i2 in self.snap[(c, i)].items():
                if K.get(c2, 0) < i2:
                    K[c2] = i2
        lst = self.clock_ops.setdefault(clock, [])
        idx = len(lst) + 1
        ev = (clock, idx)
        rec = dict(fn=fn, waits=waits, ev=ev, signal=is_dma, is_dma=is_dma)
        lst.append(rec)
        self.streams[eng].append(rec)
        s = dict(K)
        s[clock] = idx
        self.snap[ev] = s
        self._update(reads, writes, ev, multi)
        return ev

    def op(self, eng, fn, reads=(), writes=()):
        return self._issue(eng, fn, reads, writes, eng, False)

    def E(self, eng, meth, *args, reads=(), writes=(), **kw):
        def fn(e, meth=meth, args=args, kw=kw):
            return getattr(e, meth)(*args, **kw)
        return self._issue(eng, fn, reads, writes, eng, False)

    def barrier(self):
        deps = set()
        for c, lst in self.clock_ops.items():
            if lst:
                deps.add((c, len(lst)))
        self.fence = None
        ev = self._issue('sp', lambda e: e.nop(), (), (), 'sp', False, extra_deps=deps)
        self.fence = ev
        return ev

    def mark(self):
        return (self.nc.sbuf_base, self.nc.psum_base)

    def release(self, m):
        self.barrier()
        self.nc.sbuf_base, self.nc.psum_base = m

    def dma(self, out_ap, in_ap, reads=(), writes=(), eng='sp', multi=False, **kw):
        slot = 'd%d' % (self.dma_next % self.n_dma)
        self.dma_next += 1
        prev = len(self.clock_ops.get(slot, []))
        extra = [(slot, prev)] if prev > 0 else []

        def fn(e, out_ap=out_ap, in_ap=in_ap, kw=kw):
            return e.dma_start(out=out_ap, in_=in_ap, **kw)
        return self._issue(eng, fn, reads, writes, slot, True, extra, multi=multi)

    def emit(self):
        nc = self.nc
        deps = set()
        for c, lst in self.clock_ops.items():
            if lst:
                deps.add((c, len(lst)))
        K = self.know['sp']
        fin = []
        for (c, i) in sorted(deps):
            if K.get(c, 0) >= i:
                continue
            fin.append((c, i))
            self.clock_ops[c][i - 1]['signal'] = True
        sems = {}
        for c, lst in self.clock_ops.items():
            sems[c] = nc.alloc_semaphore(name='s_' + c)
            n = 0
            for rec in lst:
                if rec['signal']:
                    n += 16 if rec['is_dma'] else 1
                rec['val'] = n
        self.sems = sems

        def val(c, i):
            return self.clock_ops[c][i - 1]['val']

        def run(eng_obj, stream, tail=None):
            for rec in stream:
                for (c, i) in rec['waits']:
                    eng_obj.wait_ge(sems[c], val(c, i))
                ins = rec['fn'](eng_obj)
                if rec['signal']:
                    ins.then_inc(sems[rec['ev'][0]], 16 if rec['is_dma'] else 1)
            if tail:
                for (c, i) in tail:
                    eng_obj.wait_ge(sems[c], val(c, i))

        with nc.Block() as block:
            @block.tensor
            def _(e):
                run(e, self.streams['pe'])

            @block.scalar
            def _(e):
                run(e, self.streams['act'])

            @block.vector
            def _(e):
                run(e, self.streams['dve'])

            @block.gpsimd
            def _(e):
                run(e, self.streams['pool'])

            @block.sync
            def _(e):
                run(e, self.streams['sp'], fin)

    def stats(self):
        out = {}
        for e, s in self.streams.items():
            out[e] = (len(s), sum(len(r['waits']) for r in s))
        return out


def AP(t, offset, dims):
    if isinstance(t, Buf):
        t = t.t
    return bass.AP(t, offset, [list(d) for d in dims])


D = 1024; L = 4096; LC = 256; NIN = 4880; DEPTH = 2
HYW = 512
LN_EPS = 1e-6; RMS_EPS = 1e-5
ALPHA = (2 * DEPTH) ** 0.25
NFM = 3072
MAGIC = 12582912.0
STAGE = 9
STORE_Q = 'act'
EVAC_DVE_ONLY = False
NT_LAT = L // 512


def bf16_np(a):
    import ml_dtypes
    return np.asarray(a, dtype=np.float32).astype(ml_dtypes.bfloat16)


class MKBase:
    def __init__(self, dbg=(), ext=()):
        self.dbg = set(dbg)
        self.ext = set(ext)
        self.nc = bass.Bass("TRN2", target_bir_lowering=False)
        self.P = Prog(self.nc)
        self.inputs = {}
        self.consts = {}

    def inp(self, name, shape, dtype=F32):
        b = self.P.dram(name, shape, dtype, kind="ExternalInput")
        self.inputs[name] = b
        return b

    def const(self, name, arr, dtype=F32):
        self.consts[name] = arr
        return self.inp(name, arr.shape, dtype)

    def scratch(self, name, shape, dtype):
        kind = "ExternalOutput" if name in self.dbg else ("ExternalInput" if name in self.ext else "Internal")
        return self.P.dram(name, shape, dtype, kind=kind)

    def declare(self):
        P = self.P
        self.x = self.inp("x", [L, D])
        self.ctx = self.inp("ctx", [LC, D])
        self.cvec = self.inp("cvec", [128, 16])
        self.w_mod = self.inp("w_mod", [DEPTH, D, 3 * D])
        self.b_mod = self.inp("b_mod", [DEPTH, 3 * D])
        self.b_modT = self.inp("b_modT", [DEPTH, 128, 24])
        self.w_in = self.inp("w_in", [DEPTH, D, NIN])
        self.w_out = self.inp("w_out", [DEPTH, 1536, D])
        self.ln_g = self.inp("ln_g", [DEPTH, D])
        self.ln_b = self.inp("ln_b", [DEPTH, D])
        self.out = P.dram("out", [L, D], F32, kind="ExternalOutput")
        self.uT = self.scratch("uT", [NFM, L], BF16)
        self.u_sg = self.scratch("u_sg", [L, 512], BF16)
        self.u_dt = self.scratch("u_dt", [L, 16], F32)
        self.u_at = self.scratch("u_at", [L, 1280], BF16)
        self.ucT = self.scratch("ucT", [NFM, LC], BF16)
        self.uc_sg = self.scratch("uc_sg", [LC, 512], BF16)
        self.uc_dt = self.scratch("uc_dt", [LC, 16], F32)
        self.uc_at = self.scratch("uc_at", [LC, 1280], BF16)
        self.yT = self.scratch("yT", [1536, L], BF16)
        self.ycT = self.scratch("ycT", [1536, LC], BF16)
        self.h1 = self.scratch("h1", [L, D], F32)
        self.hc1 = self.scratch("hc1", [LC, D], F32)
        ident = np.eye(128, dtype=np.float32)
        self.identd = self.const("identd", bf16_np(ident), BF16)
        self.ident = P.sbuf("ident", [128, 128], BF16)
        P.dma(self.ident[:], self.identd.ap, writes=[self.ident])
        self.onesb = P.sbuf("onesb", [128, 128], BF16)
        P.E('pool', 'memset', self.onesb[:], 1.0, writes=[self.onesb])
        self.epsln = P.sbuf("epsln", [128, 1], F32)
        P.E('pool', 'memset', self.epsln[:], LN_EPS, writes=[self.epsln])

    def phase_mod(self, i):
        P = self.P
        if not hasattr(self, 'modc'):
            self.modc = [P.sbuf("modc%d" % k, [128, 24, 2], F32) for k in range(DEPTH)]
            self.g_bc = [P.sbuf("g_bc%d" % k, [128, D], F32) for k in range(DEPTH)]
            self.gc_bc = P.sbuf("gc_bc", [128, D], F32)
            self.scc = P.sbuf("scc", [128, 16], F32)
            cv = P.sbuf("cv", [128, 16], F32)
            P.dma(cv[:], self.cvec.ap, writes=[cv])
            P.E('act', 'activation', self.scc[:], cv[:], AF.Silu, reads=[cv], writes=[self.scc])
        m = P.mark()
        wm = P.sbuf("wm", [128, 8, 3 * D], F32)
        for kc in range(8):
            P.dma(wm[:, kc, :], self.w_mod.ap[i, kc * 128:(kc + 1) * 128, :], writes=[(wm, kc)])
        bT = P.sbuf("bT", [128, 24], F32)
        P.dma(bT[:], self.b_modT.ap[i], writes=[bT])
        ps = P.psum("mod_ps", [128, 24, 2], F32)
        modc = self.modc[i]
        for j in range(24):
            for kc in range(8):
                rhs = AP(self.scc, kc, [[16, 128], [8, 2]])
                P.E('pe', 'matmul', ps[:, j, :], wm[:, kc, j * 128:(j + 1) * 128], rhs,
                    start=(kc == 0), stop=(kc == 7), reads=[(wm, kc), self.scc], writes=[(ps, j)])
        P.E('dve', 'tensor_tensor', modc[:], ps[:], bT[:].unsqueeze(2).to_broadcast([128, 24, 2]), op=ALU.add,
            reads=[ps, bT], writes=[modc])
        P.E('dve', 'tensor_scalar_add', modc[:, 8:16, :], modc[:, 8:16, :], 1.0, reads=[modc], writes=[modc])
        brow = P.sbuf("brow", [128, D], F32)
        P.dma(brow[:], self.b_mod.ap[i:i + 1, 2 * D:3 * D].partition_broadcast(128), writes=[brow])
        for which in range(2 if i < DEPTH - 1 else 1):
            dst = self.g_bc[i] if which == 0 else self.gc_bc
            gps = P.psum("g_ps%d" % which, [128, 2, 512], F32)
            for n in range(2):
                for kc in range(8):
                    lhsT = AP(self.scc, which * 8 + kc, [[16, 128], [0, 128]])
                    P.E('pe', 'matmul', gps[:, n, :], lhsT, wm[:, kc, 2 * D + n * 512:2 * D + (n + 1) * 512],
                        start=(kc == 0), stop=(kc == 7), reads=[(wm, kc), self.scc], writes=[(gps, n)])
            P.E('dve', 'tensor_tensor', dst[:], gps[:].rearrange("p a b -> p (a b)"), brow[:], op=ALU.add,
                reads=[gps, brow], writes=[dst])
        P.release(m)

    def phase_in(self, i):
        P = self.P
        m = P.mark()
        modc = self.modc[i]
        Wb = P.sbuf("Wb", [128, 8, NIN], BF16)
        stg = [P.sbuf("wstg%d" % k, [128, NIN // 2], F32) for k in range(2)]
        n = 0
        for kc in range(8):
            for hf in range(2):
                st = stg[n % 2]; n += 1
                c0 = hf * (NIN // 2)
                P.dma(st[:], self.w_in.ap[i, kc * 128:(kc + 1) * 128, c0:c0 + NIN // 2], writes=[st])
                P.E('pool', 'tensor_copy', Wb[:, kc, c0:c0 + NIN // 2], st[:], reads=[st], writes=[(Wb, (kc, hf))])
        wall = [(Wb, (kc, hf)) for kc in range(8) for hf in range(2)]
        xin = [P.sbuf("xin%d" % k, [128, D], F32) for k in range(3)]
        xnb = [P.sbuf("xnb%d" % k, [128, D], BF16) for k in range(2)]
        stt = P.sbuf("stt", [128, 2, 6], F32)
        mv = P.sbuf("mv", [128, 2], F32)
        rstd = P.sbuf("rstd", [128, 1], F32)
        xmT = [P.sbuf("xmT%d" % k, [128, 8, 512], BF16) for k in range(2)]
        tps = [P.psum("tps%d" % k, [128, 8, 128], BF16) for k in range(2)]
        ops = [P.psum("ops%d" % k, [128, 512], F32) for k in range(4)]
        ost = [P.sbuf("ost%d" % k, [128, 512], BF16) for k in range(4)]
        dst_ = [P.sbuf("dst%d" % k, [128, 16], F32) for k in range(2)]
        cnt = dict(x=0, ev=0, o=0, d=0, t=0)

        def tile(src, ntok, tok0, col, uT, u_sg, u_dt, u_at, T):
            xm = xmT[T % 2]
            nsub = ntok // 128
            for s in range(nsub):
                xi = xin[cnt['x'] % 3]; xb = xnb[cnt['x'] % 2]; cnt['x'] += 1
                tp = tps[cnt['t'] % 2]; cnt['t'] += 1
                r0 = tok0 + s * 128
                P.dma(xi[:], src.ap[r0:r0 + 128, :], reads=[src], writes=[xi])
                for hf in range(2):
                    P.E('dve', 'bn_stats', stt[:, hf, :], xi[:, hf * 512:(hf + 1) * 512], reads=[xi], writes=[(stt, hf)])
                P.E('dve', 'bn_aggr', mv[:], stt[:], reads=[stt], writes=[mv])
                P.E('act', 'activation', rstd[:], mv[:, 1:2], AF.Sqrt, bias=self.epsln[:, 0:1], reads=[mv, self.epsln], writes=[rstd])
                P.E('dve', 'reciprocal', rstd[:], rstd[:], reads=[rstd], writes=[rstd])
                P.E('dve', 'tensor_scalar', xb[:], xi[:], mv[:, 0:1], rstd[:, 0:1], op0=ALU.subtract, op1=ALU.mult,
                    reads=[xi, mv, rstd], writes=[xb])
                if STAGE < 2: continue
                for kc in range(8):
                    P.E('pe', 'transpose', tp[:, kc, :], xb[:, kc * 128:(kc + 1) * 128], self.ident[:],
                        reads=[xb, self.ident], writes=[(tp, kc)])
                for kc in range(8):
                    o = xm[:, kc, s * 128:(s + 1) * 128]
                    sc1 = modc[:, 8 + kc, col:col + 1]; sh = modc[:, kc, col:col + 1]
                    if s % 2 == 0 or EVAC_DVE_ONLY:
                        P.E('dve', 'tensor_scalar', o, tp[:, kc, :], sc1, sh, op0=ALU.mult, op1=ALU.add,
                            reads=[(tp, kc), modc], writes=[(xm, (s, kc))])
                    else:
                        P.E('act', 'activation', o, tp[:, kc, :], AF.Identity, bias=sh, scale=sc1,
                            reads=[(tp, kc), modc], writes=[(xm, (s, kc))])
            if STAGE < 3: return
            for j in range(NFM // 128):
                op_ = ops[cnt['o'] % 4]; os_ = ost[cnt['o'] % 4]; cnt['o'] += 1
                for kc in range(8):
                    P.E('pe', 'matmul', op_[:, 0:ntok], Wb[:, kc, j * 128:(j + 1) * 128], xm[:, kc, 0:ntok],
                        start=(kc == 0), stop=(kc == 7), reads=wall + [xm], writes=[op_])
                if cnt['ev'] % 2 == 0:
                    P.E('act', 'copy', os_[:, 0:ntok], op_[:, 0:ntok], reads=[op_], writes=[os_])
                else:
                    P.E('dve', 'tensor_copy', os_[:, 0:ntok], op_[:, 0:ntok], reads=[op_], writes=[os_])
                cnt['ev'] += 1
                P.dma(uT.ap[j * 128:(j + 1) * 128, tok0:tok0 + ntok], os_[:, 0:ntok], reads=[os_],
                      writes=[(uT, j)], multi=True, eng=STORE_Q)
            if STAGE < 4: return
            groups = [(3072, 512, u_sg, 0), (3584, 16, u_dt, 0), (3600, 512, u_at, 0), (4112, 512, u_at, 512), (4624, 256, u_at, 1024)]
            for s in range(nsub):
                r0 = tok0 + s * 128
                for (c0, w, dstb, d0) in groups:
                    op_ = ops[cnt['o'] % 4]; os_ = ost[cnt['o'] % 4]; cnt['o'] += 1
                    for kc in range(8):
                        P.E('pe', 'matmul', op_[:, 0:w], xm[:, kc, s * 128:(s + 1) * 128], Wb[:, kc, c0:c0 + w],
                            start=(kc == 0), stop=(kc == 7), reads=wall + [xm], writes=[op_])
                    if dstb is u_dt:
                        ds_ = dst_[cnt['d'] % 2]; cnt['d'] += 1
                        P.E('dve', 'tensor_copy', ds_[:], op_[:, 0:16], reads=[op_], writes=[ds_])
                        P.dma(dstb.ap[r0:r0 + 128, :], ds_[:], reads=[ds_], writes=[(dstb, 0)], multi=True, eng=STORE_Q)
                    else:
                        if cnt['ev'] % 2 == 0:
                            P.E('act', 'copy', os_[:, 0:w], op_[:, 0:w], reads=[op_], writes=[os_])
                        else:
                            P.E('dve', 'tensor_copy', os_[:, 0:w], op_[:, 0:w], reads=[op_], writes=[os_])
                        cnt['ev'] += 1
                        P.dma(dstb.ap[r0:r0 + 128, d0:d0 + w], os_[:, 0:w], reads=[os_], writes=[(dstb, 0)], multi=True, eng=STORE_Q)

        hsrc = self.x if i == 0 else self.h1
        csrc = self.ctx if i == 0 else self.hc1
        tile(csrc, LC, 0, 1, self.ucT, self.uc_sg, self.uc_dt, self.uc_at, 0)
        for T in range(NT_LAT):
            tile(hsrc, 512, T * 512, 0, self.uT, self.u_sg, self.u_dt, self.u_at, T + 1)
        P.release(m)


NFFT = 8192
HY_BANDS = 16


def pap(buf, p0, off, dims):
    t = buf.t if isinstance(buf, Buf) else buf
    ps = t[:].ap[0][0]
    return bass.AP(t, p0 * ps + off, [[ps, dims[0]]] + [list(d) for d in dims[1:]])


def hyena_consts(Lh, sfx=""):
    N = 2 * Lh
    N1 = N // 64; NK = N1 // 2 + 1; NH = N1 // 2
    n1 = np.arange(N1)[:, None]; k1 = np.arange(NK)[None, :]
    th = 2 * np.pi * n1 * k1 / float(N1)
    F1 = np.concatenate([np.cos(th), -np.sin(th)], 1)
    n2 = np.arange(64)[:, None, None]; k1_ = np.arange(NK)[None, :, None]; k2 = np.arange(64)[None, None, :]
    th = 2 * np.pi * n2 * (k1_ + N1 * k2) / N
    Fr, Fi = np.cos(th), -np.sin(th)
    cb1 = np.concatenate([Fr, -Fi], 0); cb2 = np.concatenate([Fi, Fr], 0)
    S2c = np.concatenate([cb2, cb1, cb2], 2)
    k2 = np.arange(64)[:, None]; n2 = np.arange(64)[None, :]
    ph = 2 * np.pi * k2 * n2 / 64.0
    Gr, Gi = np.cos(ph), np.sin(ph)
    G = np.block([[Gr, Gi], [-Gi, Gr]])
    k1 = np.arange(NK)[:, None, None]; n2 = np.arange(64)[None, :, None]; n1 = np.arange(NH)[None, None, :]
    ps = 2 * np.pi * k1 * (64 * n1 + n2) / N
    w = np.full((NK, 1, 1), 2.0); w[0] = 1.0; w[NK - 1] = 1.0
    T4 = np.stack([w * np.cos(ps) / N, -w * np.sin(ps) / N], 1)
    t = np.linspace(0.0, 1.0, Lh, dtype=np.float32)[:, None]
    wv = (np.float32(2.0 * math.pi) * np.arange(Lh, dtype=np.float32)[:, None] / np.float32(Lh)).astype(np.float32)
    f = np.linspace(1e-4, HY_BANDS - 1, HY_BANDS, dtype=np.float32)[None]
    z = np.concatenate([t, np.cos(f * wv), -np.sin(f * wv)], -1).astype(np.float32)
    pos = np.arange(N); pos = np.where(pos <= Lh, pos, N - pos); pos[Lh] = 0
    zT = np.ascontiguousarray(z[pos].T)
    max_decay = math.log(1e-2) / 0.3; min_decay = math.log(1e-2) / 1.5
    deltas = np.linspace(min_decay, max_decay, HYW, dtype=np.float32)
    window = np.exp(-t * np.abs(deltas)[None, :]).astype(np.float32)
    wf = window[pos]; wf[Lh] = 0.0
    win = wf.reshape(N // 64, 64, HYW)
    d = dict(hF1=bf16_np(F1), hS2=bf16_np(S2c), hG=bf16_np(G), hT4=bf16_np(T4), hzT=zT.astype(np.float32), hwin=bf16_np(win))
    return {k + sfx: v for k, v in d.items()}


def conv_fm(P, u, w3, b, acc, out, Lx, func, cdeps=()):
    P.E('act', 'activation', acc[:, 0:Lx], u[:, 0:Lx], AF.Identity, bias=b, scale=w3[:, 1:2], reads=[u] + list(cdeps), writes=[acc])
    P.E('dve', 'scalar_tensor_tensor', acc[:, 1:Lx], u[:, 0:Lx - 1], w3[:, 0:1], acc[:, 1:Lx], op0=ALU.mult, op1=ALU.add,
        reads=[u, acc] + list(cdeps), writes=[acc])
    P.E('dve', 'scalar_tensor_tensor', acc[:, 0:Lx - 1], u[:, 1:Lx], w3[:, 2:3], acc[:, 0:Lx - 1], op0=ALU.mult, op1=ALU.add,
        reads=[u, acc] + list(cdeps), writes=[acc])
    P.E('act', 'activation', out[:, 0:Lx], acc[:, 0:Lx], func, reads=[acc], writes=[out])


class HyenaMixin:
    def declare_hyena(self):
        P = self.P
        self.hcfg = {}
        for Lh, sfx in ((L, ""), (LC, "c")):
            N = 2 * Lh
            hc = dict(Lh=Lh, N=N, N1=N // 64, NK=N // 128 + 1, NH=N // 128, sfx=sfx)
            for k, v in hyena_consts(Lh, sfx).items():
                hc[k[:len(k) - len(sfx)] if sfx else k] = self.const(k, v, F32 if k.startswith('hzT') else BF16)
            hc['Kf'] = self.scratch("Kf" + sfx, [2, 4, 2, 128, hc['NK'] * 128], BF16)
            self.hcfg[sfx] = hc
        self.hy_cw = self.inp("hy_cw", [DEPTH, 128, 12, 3])
        self.hy_cb = self.inp("hy_cb", [DEPTH, 128, 12])
        self.hy_b2 = self.inp("hy_b2", [DEPTH, 128, 4, 2])
        self.hy_w1 = self.inp("hy_w1", [DEPTH, 33, 128])
        self.hy_w23 = self.inp("hy_w23", [DEPTH, 2, 128, 128])
        self.hy_fb = self.inp("hy_fb", [DEPTH, 128, 4])
        self.hy_wo = self.inp("hy_wo", [DEPTH, 128, 2, 512])

    def hy_load_consts(self, hc):
        P = self.P
        N1, NK, NH = hc['N1'], hc['NK'], hc['NH']
        self.hc = hc
        self.cF1 = P.sbuf("cF1", [N1, 2 * NK], BF16); P.dma(self.cF1[:], hc['hF1'].ap, writes=[self.cF1])
        self.cS2 = P.sbuf("cS2", [128, NK, 192], BF16)
        for q in range(0, NK, 13):
            q1 = min(NK, q + 13)
            P.dma(self.cS2[:, q:q1, :], hc['hS2'].ap[:, q:q1, :], writes=[(self.cS2, q)])
        self.cG = P.sbuf("cG", [128, 128], BF16); P.dma(self.cG[:], hc['hG'].ap, writes=[self.cG])
        self.cT4 = P.sbuf("cT4", [NK, 2, 64, NH], BF16); P.dma(self.cT4[:], hc['hT4'].ap, writes=[self.cT4])

    def fft_fwd(self, lay, K, A, tagbufs, laydep=None):
        P = self.P
        NK = self.hc['NK']
        ps1 = tagbufs['ps1']
        n = 0
        for c0 in range(0, 128, 7):
            nch = min(7, 128 - c0)
            ps = ps1[n % 2]; n += 1
            for j in range(nch):
                lhsT = pap(lay, 0, c0 + j, [K, [128, 64]])
                P.E('pe', 'matmul', ps[0:64, j, :], lhsT, self.cF1[0:K, 0:NK], start=True, stop=True, tile_position=(0, 0),
                    reads=[laydep or lay, self.cF1], writes=[ps])
                P.E('pe', 'matmul', ps[64:128, j, :], lhsT, self.cF1[0:K, NK:2 * NK], start=True, stop=True, tile_position=(0, 64),
                    reads=[laydep or lay, self.cF1], writes=[ps])
            o_ = pap(A, 0, c0, [128, [1, nch], [128, NK]]); i_ = pap(ps, 0, 0, [128, [NK, nch], [1, NK]])
            if n % 2:
                P.E('dve', 'tensor_copy', o_, i_, reads=[ps], writes=[(A, c0)])
            else:
                P.E('act', 'copy', o_, i_, reads=[ps], writes=[(A, c0)])

    def phase_hyfilt(self, i, sfx=""):
        P = self.P
        hc = self.hcfg[sfx]
        Lh, NFFT, N1, NK, NH = hc['Lh'], hc['N'], hc['N1'], hc['NK'], hc['NH']
        m = P.mark()
        self.hy_load_consts(hc)
        Hb = P.sbuf("fHb", [128, NFFT], BF16)
        wo = P.sbuf("fwo", [128, 2, 512], F32); P.dma(wo[:], self.hy_wo.ap[i], writes=[wo])
        wob = P.sbuf("fwob", [128, 2, 512], BF16)
        P.E('pool', 'tensor_copy', wob[:], wo[:], reads=[wo], writes=[wob])
        m2 = P.mark()
        zT = P.sbuf("zT", [33, NFFT], F32)
        ZB = min(2048, NFFT)
        for q in range(NFFT // ZB):
            P.dma(zT[:, q * ZB:(q + 1) * ZB], hc['hzT'].ap[:, q * ZB:(q + 1) * ZB], writes=[(zT, q)])
        w1 = P.sbuf("fw1", [33, 128], F32); P.dma(w1[:], self.hy_w1.ap[i], writes=[w1])
        w23 = P.sbuf("fw23", [128, 2, 128], F32)
        for q in range(2):
            P.dma(w23[:, q, :], self.hy_w23.ap[i, q], writes=[(w23, q)])
        fb = P.sbuf("ffb", [128, 4], F32); P.dma(fb[:], self.hy_fb.ap[i], writes=[fb])
        sb = P.sbuf("fsb", [128, 4], F32)
        P.E('dve', 'tensor_scalar', sb[:, 0:1], fb[:, 0:1], float(1.0 / (2 * np.pi)), None, op0=ALU.mult, reads=[fb], writes=[sb])
        P.E('dve', 'tensor_scalar', sb[:, 1:4], fb[:, 1:4], sb[:, 0:1], None, op0=ALU.mult, reads=[fb, sb], writes=[sb])
        ha = P.sbuf("fha", [128, NFFT], F32)
        hb = P.sbuf("fhb", [128, NFFT], F32)
        t1 = [P.sbuf("ft1_%d" % k, [128, 512], F32) for k in range(2)]
        t2 = [P.sbuf("ft2_%d" % k, [128, 512], F32) for k in range(2)]
        pps = [P.psum("fps%d" % k, [128, 512], F32) for k in range(2)]
        n = 0
        for layer in range(3):
            src = zT if layer == 0 else (ha if layer == 1 else hb)
            dst = ha if layer != 1 else hb
            for blk in range(NFFT // 512):
                ps = pps[n % 2]; a = t1[n % 2]; d = t2[n % 2]; n += 1
                sl = slice(blk * 512, (blk + 1) * 512)
                if layer == 0:
                    P.E('pe', 'matmul', ps[:], w1[:], zT[:, sl], start=True, stop=True, reads=[w1, (zT, (blk * 512) // ZB)], writes=[ps])
                else:
                    P.E('pe', 'matmul', ps[:], w23[:, layer - 1, :], src[:, sl], start=True, stop=True,
                        reads=[w23, (src, blk)], writes=[ps])
                P.E('dve', 'tensor_scalar', a[:], ps[:], sb[:, 0:1], sb[:, 1 + layer:2 + layer], op0=ALU.mult, op1=ALU.add,
                    reads=[ps, sb], writes=[a])
                P.E('dve', 'tensor_scalar_add', d[:], a[:], MAGIC, reads=[a], writes=[d])
                P.E('dve', 'scalar_tensor_tensor', d[:], d[:], MAGIC, a[:], op0=ALU.subtract, op1=ALU.subtract,
                    reads=[a, d], writes=[d])
                P.E('act', 'activation', dst[:, sl], d[:], AF.Sin, scale=-6.28318, reads=[d], writes=[(dst, blk)])
        P.E('pool', 'memset', Hb[:], 0.0, writes=[Hb])
        P.E('act', 'copy', Hb[0:64, 0:Lh], ha[0:64, 0:Lh], reads=[ha, Hb], writes=[(Hb, 0)])
        P.E('dve', 'tensor_copy', Hb[64:128, Lh + 1:NFFT], ha[64:128, Lh + 1:NFFT], reads=[ha, Hb], writes=[(Hb, 1)])
        P.E('dve', 'tensor_copy', Hb[64:128, 0:1], ha[64:128, 0:1], reads=[ha, Hb], writes=[(Hb, 2)])
        P.release(m2)
        klay = P.sbuf("klay", [N1, 64, 128], BF16)
        wing = P.sbuf("wing", [N1, 64, 128], BF16)
        Af = P.sbuf("Af", [128, NK, 128], BF16)
        KA = P.sbuf("KA", [128, NK, 128], BF16)
        KB = P.sbuf("KB", [128, NK, 128], BF16)
        kps = [P.psum("kps%d" % k, [128, 4, 128], F32) for k in range(2)]
        ps1 = [P.psum("fps1_%d" % k, [128, 7, NK], F32) for k in range(2)]
        for g in range(4):
            P.dma(wing[:], hc['hwin'].ap[:, :, g * 128:(g + 1) * 128], reads=[hc['hwin']], writes=[wing])
            for cv in range(2):
                n = 0
                for q in range(16):
                    ps = kps[n % 2]; n += 1
                    for j in range(4):
                        n2 = q * 4 + j
                        P.E('pe', 'matmul', ps[0:N1, j, :], pap(Hb, 0, n2, [128, [64, N1]]), wob[:, cv, g * 128:(g + 1) * 128],
                            start=True, stop=True, reads=[Hb, wob], writes=[ps])
                    P.E('dve', 'tensor_tensor', klay[:, q * 4:q * 4 + 4, :], ps[0:N1, :, :], wing[:, q * 4:q * 4 + 4, :], op=ALU.mult,
                        reads=[ps, wing], writes=[(klay, q)])
                self.fft_fwd(klay, N1, Af, dict(ps1=ps1))
                n = 0
                for q in range(0, NK, 4):
                    nk = min(4, NK - q)
                    psa = kps[n % 2]; n += 1
                    for j in range(nk):
                        k1 = q + j
                        P.E('pe', 'matmul', psa[0:64, j, :], self.cS2[:, k1, 64:128], Af[:, k1, :], tile_position=(0, 0),
                            start=True, stop=True, reads=[self.cS2, Af], writes=[psa])
                        P.E('pe', 'matmul', psa[64:128, j, :], self.cS2[:, k1, 64:128], Af[:, k1, :], tile_position=(0, 64),
                            start=True, stop=True, reads=[self.cS2, Af], writes=[psa])
                    P.E('act', 'copy', KA[:, q:q + nk, :], psa[:, 0:nk, :], reads=[psa], writes=[(KA, q)])
                    psb = kps[n % 2]; n += 1
                    for j in range(nk):
                        k1 = q + j
                        P.E('pe', 'matmul', psb[0:64, j, :], self.cS2[:, k1, 0:64], Af[:, k1, :], tile_position=(0, 0),
                            start=True, stop=True, reads=[self.cS2, Af], writes=[psb])
                        P.E('pe', 'matmul', psb[64:128, j, :], self.cS2[:, k1, 0:64], Af[:, k1, :], tile_position=(0, 64),
                            start=True, stop=True, reads=[self.cS2, Af], writes=[psb])
                    P.E('dve', 'tensor_scalar', KB[0:64, q:q + nk, :], psb[0:64, 0:nk, :], -1.0, None, op0=ALU.mult,
                        reads=[psb], writes=[(KB, q)])
                    P.E('dve', 'tensor_copy', KB[64:128, q:q + nk, :], psb[64:128, 0:nk, :], reads=[psb], writes=[(KB, -q - 1)])
                P.dma(hc['Kf'].ap[cv, g, 0], KA[:].rearrange("p a b -> p (a b)"), reads=[KA], writes=[(hc['Kf'], (cv, g, 0))])
                P.dma(hc['Kf'].ap[cv, g, 1], KB[:].rearrange("p a b -> p (a b)"), reads=[KB], writes=[(hc['Kf'], (cv, g, 1))])
        P.release(m)

    def hy_conv(self, cv, g, xT, zT, bufs):
        P = self.P
        hc = self.hc
        NK, NH = hc['NK'], hc['NH']
        CK = min(13, NK)
        A, Y, Bt, KAc, KBc = bufs['A'], bufs['Y'], bufs['B'], bufs['KA'], bufs['KB']
        tp = bufs['tp']; n = 0
        for q in range(8):
            ps = tp[n % 2]; n += 1
            for j in range(8):
                n2 = q * 8 + j
                P.E('pe', 'transpose', ps[0:NH, j, :], pap(xT, 0, n2, [128, [64, NH]]), self.ident[:], reads=[xT, self.ident], writes=[ps])
            o_ = pap(Bt, 0, q * 1024, [NH, [128, 8], [1, 128]])
            if q % 2 == 0:
                P.E('act', 'copy', o_, ps[0:NH, :, :], reads=[ps], writes=[(Bt, ('lay', q))])
            else:
                P.E('dve', 'tensor_copy', o_, ps[0:NH, :, :], reads=[ps], writes=[(Bt, ('lay', q))])
        self.fft_fwd(Bt, NH, A, bufs, laydep=Bt)
        p2 = bufs['p2']; tm = bufs['tm']; n = 0
        Kf = hc['Kf']
        for k1 in range(NK):
            ch = k1 // CK
            KA = KAc[ch % 2]; KB = KBc[ch % 2]
            if k1 % CK == 0:
                P.dma(KA[:, 0:CK, :].rearrange("p a b -> p (a b)"), Kf.ap[cv, g, 0, :, ch * CK * 128:(ch + 1) * CK * 128], reads=[(Kf, (cv, g, 0))], writes=[KA])
                P.dma(KB[:, 0:CK, :].rearrange("p a b -> p (a b)"), Kf.ap[cv, g, 1, :, ch * CK * 128:(ch + 1) * CK * 128], reads=[(Kf, (cv, g, 1))], writes=[KB])
            kk = k1 % CK
            ps = p2[n % 2]; ta = tm[n % 3]; n += 1
            P.E('pe', 'matmul', ps[:, 0, :], self.cS2[:, k1, 64:192], A[:, k1, :], start=True, stop=True, reads=[self.cS2, A], writes=[ps])
            P.E('pe', 'matmul', ps[:, 1, :], self.cS2[:, k1, 0:128], A[:, k1, :], start=True, stop=True, reads=[self.cS2, A], writes=[ps])
            P.E('dve', 'tensor_tensor', ta[:, 0, :], ps[:, 0, :], KA[:, kk, :], op=ALU.mult, reads=[ps, KA], writes=[(ta, 0)])
            P.E('dve', 'tensor_tensor', ta[:, 1, :], ps[:, 1, :], KB[:, kk, :], op=ALU.mult, reads=[ps, KB], writes=[(ta, 1)])
            P.E('pool', 'tensor_tensor', Y[:, k1, :], ta[:, 0, :], ta[:, 1, :], op=ALU.add, reads=[ta], writes=[(Y, k1)])
        p3 = bufs['p3']; n = 0
        for c0 in range(0, 128, 4):
            ps = p3[n % 2]; n += 1
            for j in range(4):
                P.E('pe', 'matmul', ps[0:NK, j, :], pap(Y, 0, c0 + j, [128, [128, NK]]), self.cG[:], start=True, stop=True,
                    reads=[Y, self.cG], writes=[ps])
            o_ = pap(Bt, 0, c0, [NK, [1, 4], [128, 128]]); i_ = pap(ps, 0, 0, [NK, [128, 4], [1, 128]])
            if n % 2:
                P.E('act', 'copy', o_, i_, reads=[ps], writes=[(Bt, c0)])
            else:
                P.E('dve', 'tensor_copy', o_, i_, reads=[ps], writes=[(Bt, c0)])
        p4 = bufs['p4']; n = 0
        for q in range(8):
            ps = p4[n % 2]; n += 1
            for j in range(8):
                n2 = q * 8 + j
                for ri in range(2):
                    P.E('pe', 'matmul', ps[:, j, 0:NH], Bt[0:NK, ri * 64 + n2, :], self.cT4[0:NK, ri, n2, :], start=(ri == 0), stop=(ri == 1),
                        reads=[Bt, self.cT4], writes=[ps])
            o_ = pap(zT, 0, q * 8, [128, [1, 8], [64, NH]]); i_ = pap(ps, 0, 0, [128, [64, 8], [1, NH]])
            if q % 2:
                P.E('act', 'copy', o_, i_, reads=[ps], writes=[(zT, q)])
            else:
                P.E('dve', 'tensor_copy', o_, i_, reads=[ps], writes=[(zT, q)])

    def phase_hyena(self, i, sfx=""):
        P = self.P
        hc = self.hcfg[sfx]
        L, NK = hc['Lh'], hc['NK']
        uT_ = self.ucT if sfx else self.uT
        yT_ = self.ycT if sfx else self.yT
        m = P.mark()
        self.hy_load_consts(hc)
        cw = P.sbuf("hcw", [128, 12, 3], F32); P.dma(cw[:], self.hy_cw.ap[i], writes=[cw])
        cb = P.sbuf("hcb", [128, 12], F32); P.dma(cb[:], self.hy_cb.ap[i], writes=[cb])
        b2 = P.sbuf("hb2", [128, 4, 2], F32); P.dma(b2[:], self.hy_b2.ap[i], writes=[b2])
        uraw = P.sbuf("huraw", [128, L], BF16)
        acc = P.sbuf("hacc", [128, L], F32)
        vT = P.sbuf("hvT", [128, L], BF16); x1T = P.sbuf("hx1T", [128, L], BF16); x2T = P.sbuf("hx2T", [128, L], BF16)
        bufs = dict(A=P.sbuf("hA", [128, NK, 128], BF16), Y=P.sbuf("hY", [128, NK, 128], BF16),
                    B=P.sbuf("hB", [65, 128, 128], BF16), KA=[P.sbuf("hKA%d" % k, [128, 13, 128], BF16) for k in range(2)],
                    KB=[P.sbuf("hKB%d" % k, [128, 13, 128], BF16) for k in range(2)],
                    tm=[P.sbuf("htm%d" % k, [128, 2, 128], F32) for k in range(3)])
        pA = [P.psum("hpA%d" % k, [128, 512], F32) for k in range(2)]
        pT = [P.psum("hpT%d" % k, [128, 1024], BF16) for k in range(2)]
        bufs['tp'] = [PV(b, [64, 8, 128]) for b in pT]
        bufs['ps1'] = [PV(b, [128, 7, NK]) for b in pA]
        bufs['p2'] = [PV(b, [128, 2, 128]) for b in pA]
        bufs['p3'] = [PV(b, [128, 4, 128]) for b in pA]
        bufs['p4'] = [PV(b, [128, 8, 64]) for b in pA]
        for g in range(4 if HY_GROUPS is None else HY_GROUPS):
            for k, dstT in enumerate((vT, x1T, x2T)):
                ct = k * 4 + g
                P.dma(uraw[:], uT_.ap[ct * 128:(ct + 1) * 128, :], reads=[(uT_, ct)], writes=[uraw])
                conv_fm(P, uraw, cw[:, ct, :], cb[:, ct:ct + 1], acc, dstT, L, AF.Identity, cdeps=[cw, cb])
            self.hy_conv(0, g, vT, acc, bufs)
            P.E('dve', 'scalar_tensor_tensor', acc[:], vT[:], b2[:, g, 0:1], acc[:], op0=ALU.mult, op1=ALU.add, reads=[vT, acc, b2], writes=[acc])
            P.E('dve', 'tensor_tensor', vT[:], acc[:], x1T[:], op=ALU.mult, reads=[acc, x1T], writes=[vT])
            self.hy_conv(1, g, vT, acc, bufs)
            P.dma(uraw[:], uT_.ap[(12 + g) * 128:(13 + g) * 128, :], reads=[(uT_, 12 + g)], writes=[uraw])
            P.E('act', 'activation', x1T[:], uraw[:], AF.Silu, reads=[uraw], writes=[x1T])
            P.E('dve', 'scalar_tensor_tensor', acc[:], vT[:], b2[:, g, 1:2], acc[:], op0=ALU.mult, op1=ALU.add, reads=[vT, acc, b2], writes=[acc])
            P.E('dve', 'tensor_tensor', acc[:], acc[:], x2T[:], op=ALU.mult, reads=[acc, x2T], writes=[acc])
            P.E('dve', 'tensor_tensor', vT[:], acc[:], x1T[:], op=ALU.mult, reads=[acc, x1T], writes=[vT])
            P.dma(yT_.ap[g * 128:(g + 1) * 128, :], vT[:], reads=[vT], writes=[(yT_, g)])
        P.release(m)


HY_GROUPS = None


class PV(Buf):
    def __init__(self, base, shape):
        self.base = base
        self.name = base.name
        self.is_psum = True
        t = base.t
        ps = t[:].ap[0][0]
        self.shape = shape
        self.ps = ps
        self._t = t

    @property
    def st(self):
        return self.base.st

    @st.setter
    def st(self, v):
        self.base.st = v

    @property
    def t(self):
        return self._t

    def full(self):
        return self._view(self.shape[0], [slice(None)] * (len(self.shape) - 1))

    def _view(self, npart, idx, p0=0):
        strides = []
        s = 1
        for d in reversed(self.shape[1:]):
            strides.insert(0, s); s *= d
        off = 0; dims = []
        for st_, d, ix in zip(strides, self.shape[1:], idx):
            if isinstance(ix, int):
                off += st_ * ix
            else:
                a, b, _ = ix.indices(d)
                off += st_ * a; dims.append([st_, b - a])
        return bass.AP(self._t, p0 * self.ps + off, [[self.ps, npart]] + dims)

    def __getitem__(self, key):
        if not isinstance(key, tuple):
            key = (key,)
        key = list(key) + [slice(None)] * (len(self.shape) - len(key))
        pk = key[0]
        a, b, _ = pk.indices(self.shape[0]) if isinstance(pk, slice) else (pk, pk + 1, 1)
        return self._view(b - a, key[1:], a)


def ssd_consts():
    r = np.arange(128)[:, None]; c = np.arange(128)[None, :]
    tri = np.stack([(r <= c), (r > c), (r >= c), (r < c)], 1).astype(np.float32)
    return dict(stri=tri)

UPI, LOS, LOI, UPS = 0, 1, 2, 3


class SSDMixin:
    def declare_ssd(self):
        P = self.P
        self.stri = self.const("stri", ssd_consts()['stri'])
        self.ssm_cw = self.inp("ssm_cw", [DEPTH, 128, 8, 3])
        self.ssm_cb = self.inp("ssm_cb", [DEPTH, 128, 8])
        self.ssm_dtb = self.inp("ssm_dtb", [DEPTH, 16])
        self.ssm_alog = self.inp("ssm_alog", [DEPTH, 16])
        self.ssm_dd = self.inp("ssm_dd", [DEPTH, 8])
        self.ssm_nw = self.inp("ssm_nw", [DEPTH, 512])
        self.tri = P.sbuf("tri", [128, 4, 128], F32); P.dma(self.tri[:], self.stri.ap, writes=[self.tri])
        self.onesf = P.sbuf("onesf", [128, 128], F32); P.E('pool', 'memset', self.onesf[:], 1.0, writes=[self.onesf])
        self.Sf0 = P.sbuf("Sf0", [128, 512], F32); self.Sb0 = P.sbuf("Sb0", [128, 512], F32)
        self.one1 = P.sbuf("one1", [128, 1], F32); P.E('pool', 'memset', self.one1[:], 1.0, writes=[self.one1])
        self.epsr = P.sbuf("epsr", [128, 1], F32); P.E('pool', 'memset', self.epsr[:], RMS_EPS, writes=[self.epsr])

    def phase_ssd(self, i, ctxmode, want_y=True):
        P = self.P
        Lx = LC if ctxmode else L
        NC = Lx // 128
        uT = self.ucT if ctxmode else self.uT
        u_sg = self.uc_sg if ctxmode else self.u_sg
        u_dt = self.uc_dt if ctxmode else self.u_dt
        yT = self.ycT if ctxmode else self.yT
        m = P.mark()
        tri = self.tri
        cw = P.sbuf("scw", [128, 8, 3], F32); P.dma(cw[:], self.ssm_cw.ap[i], writes=[cw])
        cb = P.sbuf("scb", [128, 8], F32); P.dma(cb[:], self.ssm_cb.ap[i], writes=[cb])
        x_tm = P.sbuf("x_tm", [128, NC, 512], BF16)
        B_tm = P.sbuf("B_tm", [128, NC, 256], BF16)
        BT = P.sbuf("BT", [128, 2, Lx], BF16)
        CT = P.sbuf("CT", [128, 2, Lx], BF16)
        m1 = P.mark()
        uraw = P.sbuf("suraw", [128, Lx], BF16)
        acc = P.sbuf("sacc", [128, Lx], F32)
        xTc = P.sbuf("sxTc", [128, Lx], BF16)
        tpp = [P.psum("stp%d" % k, [128, 8, 128], BF16) for k in range(2)]
        n = 0
        for ct in range(8):
            P.dma(uraw[:], uT.ap[2048 + ct * 128:2048 + (ct + 1) * 128, :], reads=[(uT, 16 + ct)], writes=[uraw])
            if ct < 4:
                dst, dsl = xTc, xTc[:, :]
            elif ct < 6:
                dst, dsl = BT, BT[:, ct - 4, :]
            else:
                dst, dsl = CT, CT[:, ct - 6, :]
            P.E('act', 'activation', acc[:], uraw[:], AF.Identity, bias=cb[:, ct:ct + 1], scale=cw[:, ct, 1:2], reads=[uraw, cw, cb], writes=[acc])
            P.E('dve', 'scalar_tensor_tensor', acc[:, 1:Lx], uraw[:, 0:Lx - 1], cw[:, ct, 0:1], acc[:, 1:Lx], op0=ALU.mult, op1=ALU.add, reads=[uraw, acc, cw], writes=[acc])
            P.E('dve', 'scalar_tensor_tensor', acc[:, 0:Lx - 1], uraw[:, 1:Lx], cw[:, ct, 2:3], acc[:, 0:Lx - 1], op0=ALU.mult, op1=ALU.add, reads=[uraw, acc, cw], writes=[acc])
            P.E('act', 'activation', dsl, acc[:], AF.Silu, reads=[acc], writes=[(dst, ct)])
            if ct < 6:
                for c0 in range(0, NC, 8):
                    ncc = min(8, NC - c0)
                    ps = tpp[n % 2]; n += 1
                    for j in range(ncc):
                        P.E('pe', 'transpose', ps[:, j, :], dsl[:, (c0 + j) * 128:(c0 + j + 1) * 128], self.ident[:], reads=[(dst, ct), self.ident], writes=[ps])
                    if ct < 4:
                        o_ = pap(x_tm, 0, c0 * 512 + ct * 128, [128, [512, ncc], [1, 128]]); key = (x_tm, (ct, c0))
                    else:
                        o_ = pap(B_tm, 0, c0 * 256 + (ct - 4) * 128, [128, [256, ncc], [1, 128]]); key = (B_tm, (ct, c0))
                    if n % 2:
                        P.E('act', 'copy', o_, ps[:, 0:ncc, :], reads=[ps], writes=[key])
                    else:
                        P.E('dve', 'tensor_copy', o_, ps[:, 0:ncc, :], reads=[ps], writes=[key])
        P.release(m1)
        W8 = NC * 8
        dtr = P.sbuf("dtr", [128, NC, 16], F32)
        P.dma(dtr[:], u_dt.ap.rearrange("(c p) h -> p c h", p=128), reads=[u_dt], writes=[dtr])
        dtb = P.sbuf("dtb", [128, 16], F32); P.dma(dtb[:], self.ssm_dtb.ap[i:i + 1, :].partition_broadcast(128), writes=[dtb])
        alg = P.sbuf("alg", [128, 16], F32); P.dma(alg[:], self.ssm_alog.ap[i:i + 1, :].partition_broadcast(128), writes=[alg])
        dbc = P.sbuf("dbc", [128, 8], F32); P.dma(dbc[:], self.ssm_dd.ap[i:i + 1, :].partition_broadcast(128), writes=[dbc])
        nwb = P.sbuf("nwb", [128, 512], F32); P.dma(nwb[:], self.ssm_nw.ap[i:i + 1, :].partition_broadcast(128), writes=[nwb])
        P.E('act', 'activation', alg[:], alg[:], AF.Exp, reads=[alg], writes=[alg])
        P.E('dve', 'tensor_scalar', alg[:], alg[:], -1.0, None, op0=ALU.mult, reads=[alg], writes=[alg])
        P.E('dve', 'tensor_tensor', dtr[:], dtr[:], dtb[:].unsqueeze(1).to_broadcast([128, NC, 16]), op=ALU.add, reads=[dtr, dtb], writes=[dtr])
        P.E('act', 'activation', dtr[:], dtr[:], AF.Exp, reads=[dtr], writes=[dtr])
        P.E('act', 'activation', dtr[:], dtr[:], AF.Ln, bias=self.one1[:, 0:1], reads=[dtr, self.one1], writes=[dtr])
        dt = [P.sbuf("dt%d" % d, [128, W8], F32) for d in range(2)]
        da = [P.sbuf("da%d" % d, [128, W8], F32) for d in range(2)]
        for d in range(2):
            P.E('dve', 'tensor_copy', dt[d][:].rearrange("p (c h) -> p c h", h=8), dtr[:, :, d * 8:(d + 1) * 8], reads=[dtr], writes=[dt[d]])
            P.E('dve', 'tensor_tensor', da[d][:].rearrange("p (c h) -> p c h", h=8), dtr[:, :, d * 8:(d + 1) * 8],
                alg[:, d * 8:(d + 1) * 8].unsqueeze(1).to_broadcast([128, NC, 8]), op=ALU.mult, reads=[dtr, alg], writes=[da[d]])
        cps = [P.psum("cps%d" % k, [128, 512], F32) for k in range(4)]
        A = [P.sbuf("Acs%d" % d, [128, W8], F32) for d in range(2)]
        din = [P.sbuf("din%d" % d, [128, W8], F32) for d in range(2)]
        dtx = [P.sbuf("dtx%d" % d, [128, W8], F32) for d in range(2)]
        cd = [P.sbuf("cd%d" % d, [128, W8], F32) for d in range(2)]
        for d in range(2):
            pa, pt = cps[d * 2], cps[d * 2 + 1]
            P.E('pe', 'matmul', pa[:, 0:W8], tri[:, UPI if d == 0 else UPS, :], da[d][:], start=True, stop=True, reads=[tri, da[d]], writes=[pa])
            P.E('pe', 'matmul', pt[:, 0:W8], self.onesf[:], da[d][:], start=True, stop=True, reads=[self.onesf, da[d]], writes=[pt])
            P.E('dve', 'tensor_copy', A[d][:], pa[:, 0:W8], reads=[pa], writes=[A[d]])
            P.E('act', 'activation', cd[d][:], pt[:, 0:W8], AF.Exp, reads=[pt], writes=[cd[d]])
            P.E('dve', 'tensor_tensor', dtx[d][:], pt[:, 0:W8], A[d][:], op=ALU.subtract, reads=[pt, A[d]], writes=[dtx[d]])
            if d == 0:
                P.E('act', 'activation', din[d][:], A[d][:], AF.Exp, reads=[A[d]], writes=[din[d]])
                P.E('act', 'activation', dtx[d][:], dtx[d][:], AF.Exp, reads=[dtx[d]], writes=[dtx[d]])
            else:
                P.E('act', 'activation', din[d][:], dtx[d][:], AF.Exp, reads=[dtx[d]], writes=[din[d]])
                P.E('act', 'activation', dtx[d][:], A[d][:], AF.Exp, reads=[A[d], din[d]], writes=[dtx[d]])
            P.E('dve', 'tensor_tensor', dtx[d][:], dtx[d][:], dt[d][:], op=ALU.mult, reads=[dtx[d], dt[d]], writes=[dtx[d]])
        Sf = P.sbuf("Sf", [128, 512], F32); Sb = P.sbuf("Sb", [128, 512], F32)
        if ctxmode:
            P.E('pool', 'memset', Sf[:], 0.0, writes=[Sf]); P.E('pool', 'memset', Sb[:], 0.0, writes=[Sb])
        else:
            P.E('pool', 'tensor_copy', Sf[:], self.Sf0[:], reads=[self.Sf0], writes=[Sf])
            P.E('pool', 'tensor_copy', Sb[:], self.Sb0[:], reads=[self.Sb0], writes=[Sb])
        Sbin = P.sbuf("Sbin", [128, NC, 512], BF16)
        xde = [P.sbuf("xde%d" % k, [128, 512], BF16) for k in range(2)]

        def bc8(t, c):
            return pap(t, 0, c * 8, [128, [1, 8], [0, 64]])

        def v8(t):
            return t[:].rearrange("p (h d) -> p h d", d=64)

        def chunk_state(c, d, S, nn):
            xd_ = xde[nn % 2]
            P.E('dve', 'tensor_tensor', v8(xd_), x_tm[:, c, :].rearrange("p (h d) -> p h d", d=64), bc8(dtx[d], c), op=ALU.mult,
                reads=[x_tm, dtx[d]], writes=[xd_])
            ps = cps[0]
            for h in range(8):
                g = h // 4
                P.E('pe', 'matmul', ps[:, h * 64:(h + 1) * 64], B_tm[:, c, g * 128:(g + 1) * 128], xd_[:, h * 64:(h + 1) * 64],
                    start=True, stop=True, reads=[B_tm, xd_], writes=[ps])
            P.E('dve', 'tensor_tensor', v8(S), v8(S), bc8(cd[d], c), op=ALU.mult, reads=[S, cd[d]], writes=[S])
            P.E('dve', 'tensor_tensor', S[:], S[:], ps[:], op=ALU.add, reads=[S, ps], writes=[S])

        for nn, c in enumerate(range(NC - 1, -1, -1)):
            P.E('act', 'copy', Sbin[:, c, :], Sb[:], reads=[Sb], writes=[(Sbin, c)])
            chunk_state(c, 1, Sb, nn)
        if ctxmode:
            P.E('pool', 'tensor_copy', self.Sb0[:], Sb[:], reads=[Sb], writes=[self.Sb0])
        if want_y:
            gps = cps[2]; sgp = [cps[3], cps[1]]
            m3 = P.mark()
            ydp = P.psum("ydp", [128, 512], F32)
            yop = [P.psum("yop%d" % d, [128, 512], F32) for d in range(2)]
            ytp = P.psum("ytp", [128, 4, 128], BF16)
            GTm = P.sbuf("GTm", [128, 2, 2, 128], BF16)
            xd = [P.sbuf("xd%d" % d, [128, 512], BF16) for d in range(2)]
            Sfb = P.sbuf("Sfb", [128, 512], BF16)
            rhsp = [P.sbuf("rhsp%d" % k, [128, 4, 128], F32) for k in range(2)]
            Ee = [P.sbuf("Ee%d" % k, [128, 4, 128], BF16) for k in range(2)]
            Mm = [P.sbuf("Mm%d" % k, [128, 4, 128], BF16) for k in range(2)]
            gt = [P.sbuf("gt%d" % k, [128, 512], BF16) for k in range(2)]
            sg = P.sbuf("ssg", [128, 512], F32)
            y = P.sbuf("sy", [128, 512], F32); t1 = P.sbuf("st1", [128, 512], F32); t2 = P.sbuf("st2", [128, 512], F32)
            ss = P.sbuf("sss", [128, 2], F32)
            yb = P.sbuf("syb", [128, 512], BF16)
            ysT = [P.sbuf("ysT%d" % k, [128, 4, 512], BF16) for k in range(2)]
            nb = 0
        for c in range(NC):
            if want_y:
                cs = slice(c * 128, (c + 1) * 128)
                P.dma(gt[c % 2][:], u_sg.ap[c * 128:(c + 1) * 128, :], reads=[u_sg], writes=[gt[c % 2]])
                for g in range(2):
                    P.E('pe', 'matmul', gps[:, g * 128:(g + 1) * 128], BT[:, g, cs], CT[:, g, cs], start=True, stop=True, reads=[BT, CT], writes=[gps])
                for d in range(2):
                    P.E('dve', 'tensor_tensor', GTm[:, d, :, :], gps[:, 0:256].rearrange("p (g l) -> p g l", g=2),
                        tri[:, UPI if d == 0 else LOI, :].unsqueeze(1).to_broadcast([128, 2, 128]), op=ALU.mult, reads=[gps, tri], writes=[(GTm, d)])
                    P.E('dve', 'tensor_tensor', v8(xd[d]), x_tm[:, c, :].rearrange("p (h d) -> p h d", d=64), bc8(dt[d], c), op=ALU.mult,
                        reads=[x_tm, dt[d]], writes=[xd[d]])
                P.E('act', 'copy', Sfb[:], Sf[:], reads=[Sf], writes=[Sfb])
                for g in range(2):
                    mms = []
                    for d in range(2):
                        rp = rhsp[nb % 2]; ee = Ee[nb % 2]; mm_ = Mm[nb % 2]; sp_ = sgp[nb % 2]; nb += 1
                        mms.append(mm_)
                        V = UPI if d == 0 else LOI
                        U = LOS if d == 0 else UPS
                        P.E('dve', 'tensor_tensor', rp[:], tri[:, V, :].unsqueeze(1).to_broadcast([128, 4, 128]),
                            pap(da[d], 0, c * 8 + g * 4, [128, [1, 4], [0, 128]]), op=ALU.mult, reads=[tri, da[d]], writes=[rp])
                        for hh in range(4):
                            P.E('pe', 'matmul', sp_[:, hh * 128:(hh + 1) * 128], tri[:, U, :], rp[:, hh, :], start=True, stop=True, reads=[tri, rp], writes=[sp_])
                        P.E('act', 'activation', ee[:].rearrange("p a b -> p (a b)"), sp_[:], AF.Exp, reads=[sp_], writes=[ee])
                        P.E('pool', 'tensor_tensor', mm_[:], ee[:], GTm[:, d, g, :].unsqueeze(1).to_broadcast([128, 4, 128]), op=ALU.mult,
                            reads=[ee, (GTm, d)], writes=[mm_])
                    for hh in range(4):
                        h = g * 4 + hh
                        for d in range(2):
                            P.E('pe', 'matmul', ydp[:, h * 64:(h + 1) * 64], mms[d][:, hh, :], xd[d][:, h * 64:(h + 1) * 64], start=(d == 0), stop=(d == 1),
                                reads=[mms[d], xd[d]], writes=[ydp])
                for d in range(2):
                    for h in range(8):
                        g = h // 4
                        rhs = Sfb[:, h * 64:(h + 1) * 64] if d == 0 else Sbin[:, c, h * 64:(h + 1) * 64]
                        P.E('pe', 'matmul', yop[d][:, h * 64:(h + 1) * 64], CT[:, g, cs], rhs, start=True, stop=True,
                            reads=[CT, Sfb if d == 0 else (Sbin, c)], writes=[yop[d]])
            chunk_state(c, 0, Sf, c)
            if want_y:
                P.E('dve', 'tensor_tensor', v8(t1), yop[0][:].rearrange("p (h d) -> p h d", d=64), bc8(din[0], c), op=ALU.mult, reads=[yop[0], din[0]], writes=[t1])
                P.E('dve', 'tensor_tensor', v8(t2), yop[1][:].rearrange("p (h d) -> p h d", d=64), bc8(din[1], c), op=ALU.mult, reads=[yop[1], din[1]], writes=[t2])
                P.E('dve', 'tensor_tensor', y[:], ydp[:], t1[:], op=ALU.add, reads=[ydp, t1], writes=[y])
                P.E('pool', 'tensor_tensor', y[:], y[:], t2[:], op=ALU.add, reads=[y, t2], writes=[y])
                P.E('pool', 'tensor_tensor', v8(t1), x_tm[:, c, :].rearrange("p (h d) -> p h d", d=64), pap(dbc, 0, 0, [128, [1, 8], [0, 64]]), op=ALU.mult,
                    reads=[x_tm, dbc, t1], writes=[t1])
                P.E('pool', 'tensor_tensor', y[:], y[:], t1[:], op=ALU.add, reads=[y, t1], writes=[y])
                P.E('act', 'activation', sg[:], gt[c % 2][:], AF.Silu, reads=[gt[c % 2]], writes=[sg])
                P.E('dve', 'tensor_tensor', y[:], y[:], sg[:], op=ALU.mult, reads=[y, sg], writes=[y])
                P.E('pool', 'tensor_tensor', t2[:], y[:], y[:], op=ALU.mult, reads=[y, t2], writes=[t2])
                P.E('dve', 'reduce_sum', ss[:], t2[:].rearrange("p (g f) -> p g f", g=2), axis=AX.X, reads=[t2], writes=[ss])
                P.E('act', 'activation', ss[:], ss[:], AF.Sqrt, bias=self.epsr[:, 0:1], scale=1.0 / 256.0, reads=[ss, self.epsr], writes=[ss])
                P.E('dve', 'reciprocal', ss[:], ss[:], reads=[ss], writes=[ss])
                P.E('dve', 'tensor_tensor', y[:].rearrange("p (g f) -> p g f", g=2), y[:].rearrange("p (g f) -> p g f", g=2),
                    pap(ss, 0, 0, [128, [1, 2], [0, 256]]), op=ALU.mult, reads=[y, ss], writes=[y])
                P.E('pool', 'tensor_tensor', yb[:], y[:], nwb[:], op=ALU.mult, reads=[y, nwb], writes=[yb])
                for ft in range(4):
                    P.E('pe', 'transpose', ytp[:, ft, :], yb[:, ft * 128:(ft + 1) * 128], self.ident[:], reads=[yb, self.ident], writes=[ytp])
                st_ = ysT[(c // 4) % 2]
                P.E('act', 'copy', st_[:, :, (c % 4) * 128:(c % 4 + 1) * 128], ytp[:], reads=[ytp], writes=[(st_, c % 4)])
                if c % 4 == 3 or c == NC - 1:
                    c4 = (c // 4) * 4
                    nt_ = (c - c4 + 1) * 128
                    for ft in range(4):
                        P.dma(yT.ap[512 + ft * 128:512 + (ft + 1) * 128, c4 * 128:c4 * 128 + nt_], st_[:, ft, 0:nt_], reads=[st_],
                              writes=[(yT, 4 + ft)], multi=True)
        if ctxmode:
            P.E('pool', 'tensor_copy', self.Sf0[:], Sf[:], reads=[Sf], writes=[self.Sf0])
        P.release(m)


PERM = [0, 4, 1, 5, 2, 6, 3, 7]


def attn_consts():
    t = np.arange(L)
    row = (t // 64).astype(np.float32); col = (t % 64).astype(np.float32)
    inv = (10000.0 ** (-np.arange(16, dtype=np.float32) / 16)).astype(np.float32)
    ang = np.stack([row[:, None] * inv[None], col[:, None] * inv[None]], 1).astype(np.float32)
    cs = np.stack([np.cos(ang), np.sin(ang)], 1).reshape(L, 2, 32)
    rope = cs.reshape(L // 128, 128, 2, 32).transpose(1, 0, 2, 3)
    r = np.arange(128)[:, None]; c = np.arange(128)[None, :]
    am = np.stack([(r >= c), (r <= c)], 1).astype(np.float32)
    return dict(arope=np.ascontiguousarray(rope).astype(np.float32), amask=bf16_np(am))


class AttnMixin:
    def declare_attn(self):
        c = attn_consts()
        self.arope = self.const("arope", c['arope'])
        self.amask = self.const("amask", c['amask'], BF16)
        self.sinks = self.inp("sinks", [DEPTH, 8])

    def phase_attn(self, i, do_ctx):
        P = self.P
        m = P.mark()
        NT = L // 128
        rope = P.sbuf("rope", [128, NT, 2, 32], F32); P.dma(rope[:], self.arope.ap, writes=[rope])
        msk = P.sbuf("amsk", [128, 2, 128], BF16); P.dma(msk[:], self.amask.ap, writes=[msk])
        esk = P.sbuf("esk", [128, 8], F32); P.dma(esk[:], self.sinks.ap[i:i + 1, :].partition_broadcast(128), writes=[esk])
        P.E('act', 'activation', esk[:], esk[:], AF.Exp, reads=[esk], writes=[esk])
        QT = P.sbuf("QT", [128, 4, L], BF16); KT = P.sbuf("KT", [128, L], BF16)
        Va = P.sbuf("Va", [128, NT, 2, 65], BF16)
        QcT = P.sbuf("QcT", [128, 4, LC], BF16); KcT = P.sbuf("KcT", [128, LC], BF16)
        Vc = P.sbuf("Vc", [128, 2, 2, 65], BF16)
        P.E('pool', 'memset', Va[:], 1.0, writes=[Va]); P.E('pool', 'memset', Vc[:], 1.0, writes=[Vc])
        uin = [P.sbuf("auin%d" % k, [128, 1280], BF16) for k in range(2)]
        qf = P.sbuf("aqf", [128, 640], F32)
        ta = [P.sbuf("ata%d" % k, [128, 320], F32) for k in range(4)]
        qr = P.sbuf("aqr", [128, 640], BF16)
        tp = [P.psum("atp%d" % k, [128, 5, 128], BF16) for k in range(2)]
        for t in range(NT + 2):
            isctx = t >= NT
            tt = t - NT if isctx else t
            src = self.uc_at if isctx else self.u_at
            ui = uin[t % 2]; ps = tp[t % 2]
            P.dma(ui[:], src.ap[tt * 128:(tt + 1) * 128, :], reads=[src], writes=[ui])
            if not isctx:
                P.E('act', 'copy', qf[:], ui[:, 0:640], reads=[ui], writes=[qf])
                xv = lambda off: pap(qf, 0, off, [128, [64, 10], [32, 2], [1, 16]])
                ov = lambda off: pap(qr, 0, off, [128, [64, 10], [32, 2], [1, 16]])
                tv = lambda k: ta[k][:].rearrange("p (h a f) -> p h a f", h=10, a=2)
                cs = lambda k: pap(rope, 0, (t * 2 + k) * 32, [128, [0, 10], [16, 2], [1, 16]])
                P.E('dve', 'tensor_tensor', tv(0), xv(0), cs(0), op=ALU.mult, reads=[qf, rope], writes=[ta[0]])
                P.E('dve', 'tensor_tensor', tv(1), xv(16), cs(1), op=ALU.mult, reads=[qf, rope], writes=[ta[1]])
                P.E('dve', 'tensor_tensor', ov(0), tv(0), tv(1), op=ALU.subtract, reads=[ta[0], ta[1]], writes=[(qr, 0)])
                P.E('pool', 'tensor_tensor', tv(2), xv(0), cs(1), op=ALU.mult, reads=[qf, rope], writes=[ta[2]])
                P.E('pool', 'tensor_tensor', tv(3), xv(16), cs(0), op=ALU.mult, reads=[qf, rope], writes=[ta[3]])
                P.E('pool', 'tensor_tensor', ov(16), tv(2), tv(3), op=ALU.add, reads=[ta[2], ta[3]], writes=[(qr, 1)])
                qsrc = qr
            else:
                qsrc = ui
            for j in range(5):
                P.E('pe', 'transpose', ps[:, j, :], qsrc[:, j * 128:(j + 1) * 128], self.ident[:], reads=[qsrc, self.ident], writes=[ps])
            Qd, Kd, Vd = (QcT, KcT, Vc) if isctx else (QT, KT, Va)
            Lq = LC if isctx else L
            P.E('act', 'copy', pap(Qd, 0, tt * 128, [128, [Lq, 4], [1, 128]]), ps[:, 0:4, :], reads=[ps], writes=[(Qd, tt)])
            P.E('act', 'copy', Kd[:, tt * 128:(tt + 1) * 128], ps[:, 4, :], reads=[ps], writes=[(Kd, tt)])
            P.E('pool', 'tensor_copy', Vd[:, tt, :, 0:64], ui[:, 640:768].rearrange("p (g d) -> p g d", g=2), reads=[ui, Vd], writes=[(Vd, tt)])
        scp = [P.psum("ascp%d" % k, [128, 4, 128], F32) for k in range(2)]
        opp = [P.psum("aopp%d" % k, [128, 4, 65], F32) for k in range(2)]
        ytp = P.psum("aytp", [128, 4, 128], BF16)
        PT = [P.sbuf("aPT%d" % k, [128, 4, 128], BF16) for k in range(10)]
        gt = [P.sbuf("agt%d" % k, [128, 512], BF16) for k in range(2)]
        den = P.sbuf("aden", [128, 8], F32)
        yat = P.sbuf("ayat", [128, 512], F32)
        sg = P.sbuf("asg", [128, 512], F32)
        yb = P.sbuf("ayb", [128, 512], BF16)
        yaT = [P.sbuf("ayaT%d" % k, [128, 4, 512], BF16) for k in range(2)]
        nsc = 0

        def qtile(isctx, i_):
            nonlocal nsc
            Qd = QcT if isctx else QT
            Lq = LC if isctx else L
            src = self.uc_at if isctx else self.u_at
            ydst = self.ycT if isctx else self.yT
            keys = []
            if not isctx:
                if i_ > 0: keys.append((KT, Va, i_ - 1, 0))
                keys.append((KT, Va, i_, None))
                if i_ < NT - 1: keys.append((KT, Va, i_ + 1, 1))
            keys += [(KcT, Vc, 0, None), (KcT, Vc, 1, None)]
            g_ = gt[i_ % 2]
            P.dma(g_[:], src.ap[i_ * 128:(i_ + 1) * 128, 768 + 0:768 + 512] if False else src.ap[i_ * 128:(i_ + 1) * 128, 768:1280], reads=[src], writes=[g_])
            for g in range(2):
                pts = []
                for kidx, (Kd, Vd, kt, mk_) in enumerate(keys):
                    sp_ = scp[nsc % 2]; nsc += 1
                    pt = PT[g * 5 + kidx]
                    pts.append(pt)
                    for j in range(4):
                        P.E('pe', 'matmul', sp_[:, j, :], Kd[g * 64:(g + 1) * 64, kt * 128:(kt + 1) * 128],
                            Qd[g * 64:(g + 1) * 64, j, i_ * 128:(i_ + 1) * 128], start=True, stop=True,
                            reads=[(Kd, kt), (Qd, i_)], writes=[sp_])
                    P.E('act', 'activation', pt[:], sp_[:], AF.Exp, scale=0.125, reads=[sp_], writes=[pt])
                    if mk_ is not None:
                        P.E('pool', 'tensor_tensor', pt[:], pt[:], msk[:, mk_, :].unsqueeze(1).to_broadcast([128, 4, 128]), op=ALU.mult,
                            reads=[pt, msk], writes=[pt])
                op_ = opp[g]
                for j in range(4):
                    for kidx, (Kd, Vd, kt, mk_) in enumerate(keys):
                        P.E('pe', 'matmul', op_[:, j, :], pts[kidx][:, j, :], Vd[:, kt, g, :], start=(kidx == 0), stop=(kidx == len(keys) - 1),
                            reads=[pts[kidx], (Vd, kt)], writes=[op_])
            for g in range(2):
                op_ = opp[g]
                P.E('dve', 'tensor_tensor', pap(den, 0, g, [128, [2, 4]]), pap(op_, 0, 64, [128, [65, 4]]), pap(esk, 0, g, [128, [2, 4]]), op=ALU.add,
                    reads=[op_, esk], writes=[(den, g)])
            P.E('dve', 'reciprocal', den[:], den[:], reads=[den], writes=[den])
            for g in range(2):
                op_ = opp[g]
                P.E('dve', 'tensor_tensor', pap(yat, 0, g * 64, [128, [128, 4], [1, 64]]), pap(op_, 0, 0, [128, [65, 4], [1, 64]]),
                    pap(den, 0, g, [128, [2, 4], [0, 64]]), op=ALU.mult, reads=[op_, den], writes=[(yat, g)])
            P.E('act', 'activation', sg[:], g_[:], AF.Silu, reads=[g_], writes=[sg])
            P.E('pool', 'tensor_tensor', yb[:], yat[:], sg[:], op=ALU.mult, reads=[yat, sg], writes=[yb])
            for ft in range(4):
                P.E('pe', 'transpose', ytp[:, ft, :], yb[:, ft * 128:(ft + 1) * 128], self.ident[:], reads=[yb, self.ident], writes=[ytp])
            st_ = yaT[(i_ // 4) % 2]
            P.E('act', 'copy', st_[:, :, (i_ % 4) * 128:(i_ % 4 + 1) * 128], ytp[:], reads=[ytp], writes=[(st_, i_ % 4)])
            last = (LC // 128 - 1) if isctx else (NT - 1)
            if i_ % 4 == 3 or i_ == last:
                c4 = (i_ // 4) * 4
                nt_ = (i_ - c4 + 1) * 128
                for ft in range(4):
                    P.dma(ydst.ap[1024 + ft * 128:1024 + (ft + 1) * 128, c4 * 128:c4 * 128 + nt_], st_[:, ft, 0:nt_], reads=[st_],
                          writes=[(ydst, 8 + ft)], multi=True)

        if do_ctx:
            for i_ in range(LC // 128):
                qtile(True, i_)
        for i_ in range(NT if ATT_TILES is None else ATT_TILES):
            qtile(False, i_)
        P.release(m)

    def phase_out(self, i, ctxmode):
        P = self.P
        m = P.mark()
        Lx = LC if ctxmode else L
        yT = self.ycT if ctxmode else self.yT
        hsrc = (self.ctx if i == 0 else self.hc1) if ctxmode else (self.x if i == 0 else self.h1)
        hdst = self.hc1 if ctxmode else (self.h1 if i < DEPTH - 1 else self.out)
        gbc = self.gc_bc if ctxmode else self.g_bc[i]
        Wo = P.sbuf("Wo", [128, 12, D], BF16)
        stg = [P.sbuf("wostg%d" % k, [128, D], F32) for k in range(2)]
        for kc in range(12):
            st = stg[kc % 2]
            P.dma(st[:], self.w_out.ap[i, kc * 128:(kc + 1) * 128, :], writes=[st])
            P.E('pool', 'tensor_copy', Wo[:, kc, :], st[:], reads=[st], writes=[(Wo, kc)])
        lng = P.sbuf("lng", [128, D], F32); P.dma(lng[:], self.ln_g.ap[i:i + 1, :].partition_broadcast(128), writes=[lng])
        lnb = P.sbuf("lnb", [128, D], F32); P.dma(lnb[:], self.ln_b.ap[i:i + 1, :].partition_broadcast(128), writes=[lnb])
        yin = [P.sbuf("yin%d" % k, [128, 12, 512], BF16) for k in range(2)]
        hin = [P.sbuf("hin%d" % k, [128, D], F32) for k in range(2)]
        ops = [P.psum("oops%d" % k, [128, 2, 512], F32) for k in range(2)]
        tt = P.sbuf("ott", [128, D], F32); rr = [P.sbuf("orr%d" % k, [128, D], F32) for k in range(2)]
        stt = P.sbuf("ostt", [128, 2, 6], F32); mv = P.sbuf("omv", [128, 2], F32); rstd = P.sbuf("orstd", [128, 1], F32)
        n = 0
        for T in range((Lx + 511) // 512):
            ntok = min(512, Lx - T * 512)
            yi = yin[T % 2]
            for kc in range(12):
                P.dma(yi[:, kc, 0:ntok], yT.ap[kc * 128:(kc + 1) * 128, T * 512:T * 512 + ntok], reads=[(yT, kc)], writes=[(yi, kc)])
            for s in range(ntok // 128):
                r0 = T * 512 + s * 128
                ps = ops[n % 2]; hi = hin[n % 2]; r_ = rr[n % 2]; n += 1
                P.dma(hi[:], hsrc.ap[r0:r0 + 128, :], reads=[hsrc], writes=[hi])
                for nn in range(2):
                    for kc in range(12):
                        P.E('pe', 'matmul', ps[:, nn, :], yi[:, kc, s * 128:(s + 1) * 128], Wo[:, kc, nn * 512:(nn + 1) * 512],
                            start=(kc == 0), stop=(kc == 11), reads=[(yi, kc), (Wo, kc)], writes=[ps])
                P.E('dve', 'tensor_tensor', tt[:], ps[:].rearrange("p a b -> p (a b)"), gbc[:], op=ALU.mult, reads=[ps, gbc], writes=[tt])
                P.E('dve', 'scalar_tensor_tensor', tt[:], hi[:], float(ALPHA), tt[:], op0=ALU.mult, op1=ALU.add, reads=[hi, tt], writes=[tt])
                for hf in range(2):
                    P.E('dve', 'bn_stats', stt[:, hf, :], tt[:, hf * 512:(hf + 1) * 512], reads=[tt], writes=[(stt, hf)])
                P.E('dve', 'bn_aggr', mv[:], stt[:], reads=[stt], writes=[mv])
                P.E('act', 'activation', rstd[:], mv[:, 1:2], AF.Sqrt, bias=self.epsln[:, 0:1], reads=[mv, self.epsln], writes=[rstd])
                P.E('dve', 'reciprocal', rstd[:], rstd[:], reads=[rstd], writes=[rstd])
                P.E('dve', 'tensor_scalar', r_[:], tt[:], mv[:, 0:1], rstd[:, 0:1], op0=ALU.subtract, op1=ALU.mult, reads=[tt, mv, rstd], writes=[r_])
                P.E('pool', 'tensor_tensor', r_[:], r_[:], lng[:], op=ALU.mult, reads=[r_, lng], writes=[r_])
                P.E('pool', 'tensor_tensor', r_[:], r_[:], lnb[:], op=ALU.add, reads=[r_, lnb], writes=[r_])
                P.dma(hdst.ap[r0:r0 + 128, :], r_[:], reads=[r_], writes=[(hdst, 0)], multi=True, eng='act')
        P.release(m)


ATT_TILES = None


class MK(MKBase, HyenaMixin, SSDMixin, AttnMixin):
    def build(self, skip_ctx_hyena=False):
        P = self.P
        self.declare(); self.declare_hyena(); self.declare_ssd(); self.declare_attn()
        for i in range(DEPTH):
            last = i == DEPTH - 1
            self.phase_mod(i)
            self.phase_in(i)
            self.phase_hyfilt(i)
            self.phase_hyena(i)
            if not last:
                if skip_ctx_hyena:
                    m = P.mark()
                    z = P.sbuf("zfill", [128, LC], BF16)
                    P.E('pool', 'memset', z[:], 0.0, writes=[z])
                    for g in range(4):
                        P.dma(self.ycT.ap[g * 128:(g + 1) * 128, :], z[:], reads=[z], writes=[(self.ycT, g)])
                    P.release(m)
                else:
                    self.phase_hyfilt(i, "c")
                    self.phase_hyena(i, "c")
            self.phase_ssd(i, True, want_y=not last)
            self.phase_ssd(i, False)
            self.phase_attn(i, do_ctx=not last)
            if not last:
                self.phase_out(i, True)
            self.phase_out(i, False)
        P.emit()
        return self


def blockdiag2(w):
    z = np.zeros((128, 128), np.float32); z[:64, :64] = w; z[64:, 64:] = w; return z
def host_inputs(inp, b):
    d = {}
    d['x'] = np.ascontiguousarray(inp['x'][b]); d['ctx'] = np.ascontiguousarray(inp['ctx'][b])
    cv = np.zeros((128, 16), np.float32)
    cv[:, 0:8] = inp['c'][b].reshape(8, 128).T; cv[:, 8:16] = inp['c_ctx'].reshape(8, 128).T
    d['cvec'] = cv
    d['w_mod'] = inp['w_mod']; d['b_mod'] = inp['b_mod']
    d['b_modT'] = np.ascontiguousarray(inp['b_mod'].reshape(DEPTH, 24, 128).transpose(0, 2, 1))
    PERM = [0, 4, 1, 5, 2, 6, 3, 7]
    hp = np.concatenate([np.arange(h * 64, (h + 1) * 64) for h in PERM])
    w_in = inp['w_in'].copy()
    w_in[:, :, 3600:4112] = inp['w_in'][:, :, 3600 + hp]
    w_in[:, :, 4368:4880] = inp['w_in'][:, :, 4368 + hp]
    w_out = inp['w_out'].copy()
    w_out[:, 1024:1536] = inp['w_out'][:, 1024 + hp]
    d['w_in'] = w_in; d['w_out'] = w_out
    d['sinks'] = np.ascontiguousarray(inp['attn_sinks'][:, PERM]); d['ln_g'] = inp['ln_g']; d['ln_b'] = inp['ln_b']
    d['hy_cw'] = np.ascontiguousarray(inp['hy_conv_w'].reshape(DEPTH, 3, 12, 128).transpose(0, 3, 2, 1))
    d['hy_cb'] = np.ascontiguousarray(inp['hy_conv_b'].reshape(DEPTH, 12, 128).transpose(0, 2, 1))
    d['hy_b2'] = np.ascontiguousarray(inp['hy_bias'].reshape(DEPTH, 2, 4, 128).transpose(0, 3, 2, 1))
    d['hy_w1'] = np.ascontiguousarray(np.concatenate([inp['hy_f_w1'], inp['hy_f_w1']], 2))
    d['hy_w23'] = np.stack([np.stack([blockdiag2(inp['hy_f_w2'][i]), blockdiag2(inp['hy_f_w3'][i])]) for i in range(DEPTH)])
    fb = np.stack([inp['hy_f_freq'], inp['hy_f_b1'], inp['hy_f_b2'], inp['hy_f_b3']], -1)
    d['hy_fb'] = np.ascontiguousarray(np.concatenate([fb, fb], 1))
    wo = inp['hy_f_wout'].reshape(DEPTH, 64, 2, 2, 512)
    d['hy_wo'] = np.ascontiguousarray(np.concatenate([wo[:, :, :, 0], wo[:, :, :, 1]], 1))
    d['ssm_cw'] = np.ascontiguousarray(inp['ssm_conv_w'].reshape(DEPTH, 3, 8, 128).transpose(0, 3, 2, 1))
    d['ssm_cb'] = np.ascontiguousarray(inp['ssm_conv_b'].reshape(DEPTH, 8, 128).transpose(0, 2, 1))
    d['ssm_dtb'] = np.ascontiguousarray(inp['ssm_dt_bias'].reshape(DEPTH, 16))
    d['ssm_alog'] = np.ascontiguousarray(inp['ssm_a_log'].reshape(DEPTH, 16))
    d['ssm_dd'] = inp['ssm_d']; d['ssm_nw'] = inp['ssm_norm_w']
    return d


_PROG = {}
SKIP_CTX_HYENA = False


def kernel(**inputs):
    inp = {k: np.asarray(v) for k, v in inputs.items()}
    if 'mk' not in _PROG:
        _PROG['mk'] = MK().build(skip_ctx_hyena=SKIP_CTX_HYENA)
    mk = _PROG['mk']
    nb = inp['x'].shape[0]
    in_maps = []
    for b in range(nb):
        d = host_inputs(inp, b)
        d.update(mk.consts)
        in_maps.append({k: np.ascontiguousarray(v) for k, v in d.items() if k in mk.inputs})
    res = run_bass_kernel_spmd(mk.nc, in_maps, core_ids=list(range(nb)))
    return np.stack([np.asarray(r['out'], dtype=np.float32) for r in res.results], 0)
```

```python
import math


import numpy as np
import concourse.bass as bass
import concourse.mybir as mybir
from concourse.bass_utils import run_bass_kernel_spmd

F32 = mybir.dt.float32
BF16 = mybir.dt.bfloat16
AF = mybir.ActivationFunctionType
ALU = mybir.AluOpType
AX = mybir.AxisListType

COMPUTE = ('pe', 'act', 'dve', 'pool')
SAME_ENGINE_SYNC = True
ALLK = '__all__'


class Buf:
    def __init__(self, t, name):
        self.t = t
        self.name = name
        self.st = {}
        self.is_psum = False

    def __getitem__(self, k):
        return self.t[k]

    def conflicts(self, key):
        if key == ALLK:
            return list(self.st.keys())
        r = []
        if key in self.st:
            r.append(key)
        if ALLK in self.st:
            r.append(ALLK)
        return r


def _norm(lst):
    out = []
    for x in lst:
        if x is None:
            continue
        if isinstance(x, Buf):
            out.append((x, ALLK))
        elif x[0].is_psum:
            out.append((x[0], ALLK))
        else:
            out.append((x[0], x[1]))
    return out


class Prog:
    def __init__(self, nc, n_dma=24):
        self.nc = nc
        self.streams = {e: [] for e in ('pe', 'act', 'dve', 'pool', 'sp')}
        self.know = {e: {} for e in self.streams}
        self.clock_ops = {}
        self.snap = {}
        self.n_dma = n_dma
        self.dma_next = 0
        self.bufs = []
        self._keep = []
        self.fence = None

    def _uniq(self, name):
        self._nid = getattr(self, '_nid', 0) + 1
        return "%s_%d" % (name, self._nid)

    def sbuf(self, name, shape, dtype):
        name = self._uniq(name)
        t = self.nc.alloc_sbuf_tensor(name, list(shape), dtype)
        b = Buf(t, name)
        self.bufs.append(b)
        return b

    def psum(self, name, shape, dtype=F32):
        name = self._uniq(name)
        t = self.nc.alloc_psum_tensor(name, list(shape), dtype)
        b = Buf(t, name)
        b.is_psum = True
        self.bufs.append(b)
        return b

    def dram(self, name, shape, dtype, kind="Internal"):
        t = self.nc.dram_tensor(name, list(shape), dtype, kind=kind)
        b = Buf(t, name)
        b.ap = t.ap()
        self.bufs.append(b)
        return b

    def _collect(self, reads, writes, multi, eng=None):
        deps = set()
        for (b, k) in reads:
            for kk in b.conflicts(k):
                deps.update(b.st[kk][0])
                if b.is_psum:
                    for c, i in b.st[kk][1].items():
                        if c != eng:
                            deps.add((c, i))
        for (b, k) in writes:
            for kk in b.conflicts(k):
                lw, rd = b.st[kk]
                if not multi:
                    deps.update(lw)
                for c, i in rd.items():
                    deps.add((c, i))
        return deps

    def _update(self, reads, writes, ev, multi):
        for (b, k) in reads:
            ent = b.st.setdefault(k, [set(), {}])
            c, i = ev
            if ent[1].get(c, 0) < i:
                ent[1][c] = i
        for (b, k) in writes:
            if multi and k in b.st:
                b.st[k][0].add(ev)
                b.st[k][1] = {}
            elif k == ALLK:
                b.st = {ALLK: [{ev}, {}]}
            else:
                b.st[k] = [{ev}, {}]

    def _issue(self, eng, fn, reads, writes, clock, is_dma, extra_deps=(), multi=False):
        reads = _norm(reads)
        writes = _norm(writes)
        deps = self._collect(reads, writes, multi, eng)
        deps.update(extra_deps)
        if self.fence is not None:
            deps.add(self.fence)
        K = self.know[eng]
        waits = []
        for (c, i) in sorted(deps):
            if (not is_dma) and c == eng and (eng == 'pe' or not SAME_ENGINE_SYNC):
                continue
            if K.get(c, 0) >= i:
                continue
            waits.append((c, i))
            self.clock_ops[c][i - 1]['signal'] = True
            for c2, i2 in self.snap[(c, i)].items():
                if K.get(c2, 0) < i2:
                    K[c2] = i2
        lst = self.clock_ops.setdefault(clock, [])
        idx = len(lst) + 1
        ev = (clock, idx)
        rec = dict(fn=fn, waits=waits, ev=ev, signal=is_dma, is_dma=is_dma)
        lst.append(rec)
        self.streams[eng].append(rec)
        s = dict(K)
        s[clock] = idx
        self.snap[ev] = s
        self._update(reads, writes, ev, multi)
        return ev

    def op(self, eng, fn, reads=(), writes=()):
        return self._issue(eng, fn, reads, writes, eng, False)

    def E(self, eng, meth, *args, reads=(), writes=(), **kw):
        def fn(e, meth=meth, args=args, kw=kw):
            return getattr(e, meth)(*args, **kw)
        return self._issue(eng, fn, reads, writes, eng, False)

    def barrier(self):
        deps = set()
        for c, lst in self.clock_ops.items():
            if lst:
                deps.add((c, len(lst)))
        self.fence = None
        ev = self._issue('sp', lambda e: e.nop(), (), (), 'sp', False, extra_deps=deps)
        self.fence = ev
        return ev

    def mark(self):
        return (self.nc.sbuf_base, self.nc.psum_base)

    def release(self, m):
        self.barrier()
        self.nc.sbuf_base, self.nc.psum_base = m

    def dma(self, out_ap, in_ap, reads=(), writes=(), eng='sp', multi=False, **kw):
        slot = 'd%d' % (self.dma_next % self.n_dma)
        self.dma_next += 1
        prev = len(self.clock_ops.get(slot, []))
        extra = [(slot, prev)] if prev > 0 else []

        def fn(e, out_ap=out_ap, in_ap=in_ap, kw=kw):
            return e.dma_start(out=out_ap, in_=in_ap, **kw)
        return self._issue(eng, fn, reads, writes, slot, True, extra, multi=multi)

    def emit(self):
        nc = self.nc
        deps = set()
        for c, lst in self.clock_ops.items():
            if lst:
                deps.add((c, len(lst)))
        K = self.know['sp']
        fin = []
        for (c, i) in sorted(deps):
            if K.get(c, 0) >= i:
                continue
            fin.append((c, i))
            self.clock_ops[c][i - 1]['signal'] = True
        sems = {}
        for c, lst in self.clock_ops.items():
            sems[c] = nc.alloc_semaphore(name='s_' + c)
            n = 0
            for rec in lst:
                if rec['signal']:
                    n += 16 if rec['is_dma'] else 1
                rec['val'] = n
        self.sems = sems

        def val(c, i):
            return self.clock_ops[c][i - 1]['val']

        def run(eng_obj, stream, tail=None):
            for rec in stream:
                for (c, i) in rec['waits']:
                    eng_obj.wait_ge(sems[c], val(c, i))
                ins = rec['fn'](eng_obj)
                if rec['signal']:
                    ins.then_inc(sems[rec['ev'][0]], 16 if rec['is_dma'] else 1)
            if tail:
                for (c, i) in tail:
                    eng_obj.wait_ge(sems[c], val(c, i))

        with nc.Block() as block:
            @block.tensor
            def _(e):
                run(e, self.streams['pe'])

            @block.scalar
            def _(e):
                run(e, self.streams['act'])

            @block.vector
            def _(e):
                run(e, self.streams['dve'])

            @block.gpsimd
            def _(e):
                run(e, self.streams['pool'])

            @block.sync
            def _(e):
                run(e, self.streams['sp'], fin)

    def stats(self):
        out = {}
        for e, s in self.streams.items():
            out[e] = (len(s), sum(len(r['waits']) for r in s))
        return out


def AP(t, offset, dims):
    if isinstance(t, Buf):
        t = t.t
    return bass.AP(t, offset, [list(d) for d in dims])


D = 1024; L = 4096; LC = 256; NIN = 4880; DEPTH = 2
HYW = 512
LN_EPS = 1e-6; RMS_EPS = 1e-5
ALPHA = (2 * DEPTH) ** 0.25
NFM = 3072
MAGIC = 12582912.0
STAGE = 9
STORE_Q = 'act'
EVAC_DVE_ONLY = False
NT_LAT = L // 512


def bf16_np(a):
    import ml_dtypes
    return np.asarray(a, dtype=np.float32).astype(ml_dtypes.bfloat16)


class MKBase:
    def __init__(self, dbg=(), ext=()):
        self.dbg = set(dbg)
        self.ext = set(ext)
        self.nc = bass.Bass("TRN2", target_bir_lowering=False)
        self.P = Prog(self.nc)
        self.inputs = {}
        self.consts = {}

    def inp(self, name, shape, dtype=F32):
        b = self.P.dram(name, shape, dtype, kind="ExternalInput")
        self.inputs[name] = b
        return b

    def const(self, name, arr, dtype=F32):
        self.consts[name] = arr
        return self.inp(name, arr.shape, dtype)

    def scratch(self, name, shape, dtype):
        kind = "ExternalOutput" if name in self.dbg else ("ExternalInput" if name in self.ext else "Internal")
        return self.P.dram(name, shape, dtype, kind=kind)

    def declare(self):
        P = self.P
        self.x = self.inp("x", [L, D])
        self.ctx = self.inp("ctx", [LC, D])
        self.cvec = self.inp("cvec", [128, 16])
        self.w_mod = self.inp("w_mod", [DEPTH, D, 3 * D])
        self.b_mod = self.inp("b_mod", [DEPTH, 3 * D])
        self.b_modT = self.inp("b_modT", [DEPTH, 128, 24])
        self.w_in = self.inp("w_in", [DEPTH, D, NIN])
        self.w_out = self.inp("w_out", [DEPTH, 1536, D])
        self.ln_g = self.inp("ln_g", [DEPTH, D])
        self.ln_b = self.inp("ln_b", [DEPTH, D])
        self.out = P.dram("out", [L, D], F32, kind="ExternalOutput")
        self.uT = self.scratch("uT", [NFM, L], BF16)
        self.u_sg = self.scratch("u_sg", [L, 512], BF16)
        self.u_dt = self.scratch("u_dt", [L, 16], F32)
        self.u_at = self.scratch("u_at", [L, 1280], BF16)
        self.ucT = self.scratch("ucT", [NFM, LC], BF16)
        self.uc_sg = self.scratch("uc_sg", [LC, 512], BF16)
        self.uc_dt = self.scratch("uc_dt", [LC, 16], F32)
        self.uc_at = self.scratch("uc_at", [LC, 1280], BF16)
        self.yT = self.scratch("yT", [1536, L], BF16)
        self.ycT = self.scratch("ycT", [1536, LC], BF16)
        self.h1 = self.scratch("h1", [L, D], F32)
        self.hc1 = self.scratch("hc1", [LC, D], F32)
        ident = np.eye(128, dtype=np.float32)
        self.identd = self.const("identd", bf16_np(ident), BF16)
        self.ident = P.sbuf("ident", [128, 128], BF16)
        P.dma(self.ident[:], self.identd.ap, writes=[self.ident])
        self.onesb = P.sbuf("onesb", [128, 128], BF16)
        P.E('pool', 'memset', self.onesb[:], 1.0, writes=[self.onesb])
        self.epsln = P.sbuf("epsln", [128, 1], F32)
        P.E('pool', 'memset', self.epsln[:], LN_EPS, writes=[self.epsln])

    def phase_mod(self, i):
        P = self.P
        if not hasattr(self, 'modc'):
            self.modc = [P.sbuf("modc%d" % k, [128, 24, 2], F32) for k in range(DEPTH)]
            self.g_bc = [P.sbuf("g_bc%d" % k, [128, D], F32) for k in range(DEPTH)]
            self.gc_bc = P.sbuf("gc_bc", [128, D], F32)
            self.scc = P.sbuf("scc", [128, 16], F32)
            cv = P.sbuf("cv", [128, 16], F32)
            P.dma(cv[:], self.cvec.ap, writes=[cv])
            P.E('act', 'activation', self.scc[:], cv[:], AF.Silu, reads=[cv], writes=[self.scc])
        m = P.mark()
        wm = P.sbuf("wm", [128, 8, 3 * D], F32)
        for kc in range(8):
            P.dma(wm[:, kc, :], self.w_mod.ap[i, kc * 128:(kc + 1) * 128, :], writes=[(wm, kc)])
        bT = P.sbuf("bT", [128, 24], F32)
        P.dma(bT[:], self.b_modT.ap[i], writes=[bT])
        ps = P.psum("mod_ps", [128, 24, 2], F32)
        modc = self.modc[i]
        for j in range(24):
            for kc in range(8):
                rhs = AP(self.scc, kc, [[16, 128], [8, 2]])
                P.E('pe', 'matmul', ps[:, j, :], wm[:, kc, j * 128:(j + 1) * 128], rhs,
                    start=(kc == 0), stop=(kc == 7), reads=[(wm, kc), self.scc], writes=[(ps, j)])
        P.E('dve', 'tensor_tensor', modc[:], ps[:], bT[:].unsqueeze(2).to_broadcast([128, 24, 2]), op=ALU.add,
            reads=[ps, bT], writes=[modc])
        P.E('dve', 'tensor_scalar_add', modc[:, 8:16, :], modc[:, 8:16, :], 1.0, reads=[modc], writes=[modc])
        brow = P.sbuf("brow", [128, D], F32)
        P.dma(brow[:], self.b_mod.ap[i:i + 1, 2 * D:3 * D].partition_broadcast(128), writes=[brow])
        for which in range(2 if i < DEPTH - 1 else 1):
            dst = self.g_bc[i] if which == 0 else self.gc_bc
            gps = P.psum("g_ps%d" % which, [128, 2, 512], F32)
            for n in range(2):
                for kc in range(8):
                    lhsT = AP(self.scc, which * 8 + kc, [[16, 128], [0, 128]])
                    P.E('pe', 'matmul', gps[:, n, :], lhsT, wm[:, kc, 2 * D + n * 512:2 * D + (n + 1) * 512],
                        start=(kc == 0), stop=(kc == 7), reads=[(wm, kc), self.scc], writes=[(gps, n)])
            P.E('dve', 'tensor_tensor', dst[:], gps[:].rearrange("p a b -> p (a b)"), brow[:], op=ALU.add,
                reads=[gps, brow], writes=[dst])
        P.release(m)

    def phase_in(self, i):
        P = self.P
        m = P.mark()
        modc = self.modc[i]
        Wb = P.sbuf("Wb", [128, 8, NIN], BF16)
        stg = [P.sbuf("wstg%d" % k, [128, NIN // 2], F32) for k in range(2)]
        n = 0
        for kc in range(8):
            for hf in range(2):
                st = stg[n % 2]; n += 1
                c0 = hf * (NIN // 2)
                P.dma(st[:], self.w_in.ap[i, kc * 128:(kc + 1) * 128, c0:c0 + NIN // 2], writes=[st])
                P.E('pool', 'tensor_copy', Wb[:, kc, c0:c0 + NIN // 2], st[:], reads=[st], writes=[(Wb, (kc, hf))])
        wall = [(Wb, (kc, hf)) for kc in range(8) for hf in range(2)]
        xin = [P.sbuf("xin%d" % k, [128, D], F32) for k in range(3)]
        xnb = [P.sbuf("xnb%d" % k, [128, D], BF16) for k in range(2)]
        stt = P.sbuf("stt", [128, 2, 6], F32)
        mv = P.sbuf("mv", [128, 2], F32)
        rstd = P.sbuf("rstd", [128, 1], F32)
        xmT = [P.sbuf("xmT%d" % k, [128, 8, 512], BF16) for k in range(2)]
        tps = [P.psum("tps%d" % k, [128, 8, 128], BF16) for k in range(2)]
        ops = [P.psum("ops%d" % k, [128, 512], F32) for k in range(4)]
        ost = [P.sbuf("ost%d" % k, [128, 512], BF16) for k in range(4)]
        dst_ = [P.sbuf("dst%d" % k, [128, 16], F32) for k in range(2)]
        cnt = dict(x=0, ev=0, o=0, d=0, t=0)

        def prep(src, ntok, tok0, col, uT, u_sg, u_dt, u_at, T):
            xm = xmT[T % 2]
            nsub = ntok // 128
            for s in range(nsub):
                xi = xin[cnt['x'] % 3]; xb = xnb[cnt['x'] % 2]; cnt['x'] += 1
                tp = tps[cnt['t'] % 2]; cnt['t'] += 1
                r0 = tok0 + s * 128
                P.dma(xi[:], src.ap[r0:r0 + 128, :], reads=[src], writes=[xi])
                for hf in range(2):
                    P.E('dve', 'bn_stats', stt[:, hf, :], xi[:, hf * 512:(hf + 1) * 512], reads=[xi], writes=[(stt, hf)])
                P.E('dve', 'bn_aggr', mv[:], stt[:], reads=[stt], writes=[mv])
                P.E('act', 'activation', rstd[:], mv[:, 1:2], AF.Sqrt, bias=self.epsln[:, 0:1], reads=[mv, self.epsln], writes=[rstd])
                P.E('dve', 'reciprocal', rstd[:], rstd[:], reads=[rstd], writes=[rstd])
                P.E('dve', 'tensor_scalar', xb[:], xi[:], mv[:, 0:1], rstd[:, 0:1], op0=ALU.subtract, op1=ALU.mult,
                    reads=[xi, mv, rstd], writes=[xb])
                if STAGE < 2: continue
                for kc in range(8):
                    P.E('pe', 'transpose', tp[:, kc, :], xb[:, kc * 128:(kc + 1) * 128], self.ident[:],
                        reads=[xb, self.ident], writes=[(tp, kc)])
                for kc in range(8):
                    o = xm[:, kc, s * 128:(s + 1) * 128]
                    sc1 = modc[:, 8 + kc, col:col + 1]; sh = modc[:, kc, col:col + 1]
                    if s % 2 == 0 or EVAC_DVE_ONLY:
                        P.E('dve', 'tensor_scalar', o, tp[:, kc, :], sc1, sh, op0=ALU.mult, op1=ALU.add,
                            reads=[(tp, kc), modc], writes=[(xm, (s, kc))])
                    else:
                        P.E('act', 'activation', o, tp[:, kc, :], AF.Identity, bias=sh, scale=sc1,
                            reads=[(tp, kc), modc], writes=[(xm, (s, kc))])

        def mm(src, ntok, tok0, col, uT, u_sg, u_dt, u_at, T):
            xm = xmT[T % 2]
            nsub = ntok // 128
            for j in range(NFM // 128):
                op_ = ops[cnt['o'] % 4]; os_ = ost[cnt['o'] % 4]; cnt['o'] += 1
                for kc in range(8):
                    P.E('pe', 'matmul', op_[:, 0:ntok], Wb[:, kc, j * 128:(j + 1) * 128], xm[:, kc, 0:ntok],
                        start=(kc == 0), stop=(kc == 7), reads=wall + [xm], writes=[op_])
                if cnt['ev'] % 2 == 0:
                    P.E('act', 'copy', os_[:, 0:ntok], op_[:, 0:ntok], reads=[op_], writes=[os_])
                else:
                    P.E('dve', 'tensor_copy', os_[:, 0:ntok], op_[:, 0:ntok], reads=[op_], writes=[os_])
                cnt['ev'] += 1
                P.dma(uT.ap[j * 128:(j + 1) * 128, tok0:tok0 + ntok], os_[:, 0:ntok], reads=[os_],
                      writes=[(uT, j)], multi=True, eng=STORE_Q)
            if STAGE < 4: return
            groups = [(3072, 512, u_sg, 0), (3584, 16, u_dt, 0), (3600, 512, u_at, 0), (4112, 512, u_at, 512), (4624, 256, u_at, 1024)]
            for s in range(nsub):
                r0 = tok0 + s * 128
                for (c0, w, dstb, d0) in groups:
                    op_ = ops[cnt['o'] % 4]; os_ = ost[cnt['o'] % 4]; cnt['o'] += 1
                    for kc in range(8):
                        P.E('pe', 'matmul', op_[:, 0:w], xm[:, kc, s * 128:(s + 1) * 128], Wb[:, kc, c0:c0 + w],
                            start=(kc == 0), stop=(kc == 7), reads=wall + [xm], writes=[op_])
                    if dstb is u_dt:
                        ds_ = dst_[cnt['d'] % 2]; cnt['d'] += 1
                        P.E('dve', 'tensor_copy', ds_[:], op_[:, 0:16], reads=[op_], writes=[ds_])
                        P.dma(dstb.ap[r0:r0 + 128, :], ds_[:], reads=[ds_], writes=[(dstb, 0)], multi=True, eng=STORE_Q)
                    else:
                        if cnt['ev'] % 2 == 0:
                            P.E('act', 'copy', os_[:, 0:w], op_[:, 0:w], reads=[op_], writes=[os_])
                        else:
                            P.E('dve', 'tensor_copy', os_[:, 0:w], op_[:, 0:w], reads=[op_], writes=[os_])
                        cnt['ev'] += 1
                        P.dma(dstb.ap[r0:r0 + 128, d0:d0 + w], os_[:, 0:w], reads=[os_], writes=[(dstb, 0)], multi=True, eng=STORE_Q)

        hsrc = self.x if i == 0 else self.h1
        csrc = self.ctx if i == 0 else self.hc1
        jobs = [(csrc, LC, 0, 1, self.ucT, self.uc_sg, self.uc_dt, self.uc_at, 0)]
        for T in range(NT_LAT):
            jobs.append((hsrc, 512, T * 512, 0, self.uT, self.u_sg, self.u_dt, self.u_at, T + 1))
        prep(*jobs[0])
        for k, jb in enumerate(jobs):
            if k + 1 < len(jobs):
                prep(*jobs[k + 1])
            mm(*jb)
        P.release(m)


NFFT = 8192
HY_BANDS = 16


def pap(buf, p0, off, dims):
    t = buf.t if isinstance(buf, Buf) else buf
    ps = t[:].ap[0][0]
    return bass.AP(t, p0 * ps + off, [[ps, dims[0]]] + [list(d) for d in dims[1:]])


def hyena_consts(Lh, sfx=""):
    N = 2 * Lh
    N1 = N // 64; NK = N1 // 2 + 1; NH = N1 // 2
    n1 = np.arange(N1)[:, None]; k1 = np.arange(NK)[None, :]
    th = 2 * np.pi * n1 * k1 / float(N1)
    F1 = np.concatenate([np.cos(th), -np.sin(th)], 1)
    n2 = np.arange(64)[:, None, None]; k1_ = np.arange(NK)[None, :, None]; k2 = np.arange(64)[None, None, :]
    th = 2 * np.pi * n2 * (k1_ + N1 * k2) / N
    Fr, Fi = np.cos(th), -np.sin(th)
    cb1 = np.concatenate([Fr, -Fi], 0); cb2 = np.concatenate([Fi, Fr], 0)
    S2c = np.concatenate([cb2, cb1, cb2], 2)
    k2 = np.arange(64)[:, None]; n2 = np.arange(64)[None, :]
    ph = 2 * np.pi * k2 * n2 / 64.0
    Gr, Gi = np.cos(ph), np.sin(ph)
    G = np.block([[Gr, Gi], [-Gi, Gr]])
    k1 = np.arange(NK)[:, None, None]; n2 = np.arange(64)[None, :, None]; n1 = np.arange(NH)[None, None, :]
    ps = 2 * np.pi * k1 * (64 * n1 + n2) / N
    w = np.full((NK, 1, 1), 2.0); w[0] = 1.0; w[NK - 1] = 1.0
    T4 = np.stack([w * np.cos(ps) / N, -w * np.sin(ps) / N], 1)
    t = np.linspace(0.0, 1.0, Lh, dtype=np.float32)[:, None]
    wv = (np.float32(2.0 * math.pi) * np.arange(Lh, dtype=np.float32)[:, None] / np.float32(Lh)).astype(np.float32)
    f = np.linspace(1e-4, HY_BANDS - 1, HY_BANDS, dtype=np.float32)[None]
    z = np.concatenate([t, np.cos(f * wv), -np.sin(f * wv)], -1).astype(np.float32)
    pos = np.arange(N); pos = np.where(pos <= Lh, pos, N - pos); pos[Lh] = 0
    zT = np.ascontiguousarray(z[pos].T)
    max_decay = math.log(1e-2) / 0.3; min_decay = math.log(1e-2) / 1.5
    deltas = np.linspace(min_decay, max_decay, HYW, dtype=np.float32)
    window = np.exp(-t * np.abs(deltas)[None, :]).astype(np.float32)
    wf = window[pos]; wf[Lh] = 0.0
    win = wf.reshape(N // 64, 64, HYW)
    d = dict(hF1=bf16_np(F1), hS2=bf16_np(S2c), hG=bf16_np(G), hT4=bf16_np(T4), hzT=zT.astype(np.float32), hwin=bf16_np(win))
    return {k + sfx: v for k, v in d.items()}


def conv_fm(P, u, w3, b, acc, out, Lx, func, cdeps=()):
    P.E('act', 'activation', acc[:, 0:Lx], u[:, 0:Lx], AF.Identity, bias=b, scale=w3[:, 1:2], reads=[u] + list(cdeps), writes=[acc])
    P.E('dve', 'scalar_tensor_tensor', acc[:, 1:Lx], u[:, 0:Lx - 1], w3[:, 0:1], acc[:, 1:Lx], op0=ALU.mult, op1=ALU.add,
        reads=[u, acc] + list(cdeps), writes=[acc])
    P.E('dve', 'scalar_tensor_tensor', acc[:, 0:Lx - 1], u[:, 1:Lx], w3[:, 2:3], acc[:, 0:Lx - 1], op0=ALU.mult, op1=ALU.add,
        reads=[u, acc] + list(cdeps), writes=[acc])
    P.E('act', 'activation', out[:, 0:Lx], acc[:, 0:Lx], func, reads=[acc], writes=[out])


class HyenaMixin:
    def declare_hyena(self):
        P = self.P
        self.hcfg = {}
        for Lh, sfx in ((L, ""), (LC, "c")):
            N = 2 * Lh
            hc = dict(Lh=Lh, N=N, N1=N // 64, NK=N // 128 + 1, NH=N // 128, sfx=sfx)
            for k, v in hyena_consts(Lh, sfx).items():
                hc[k[:len(k) - len(sfx)] if sfx else k] = self.const(k, v, F32 if k.startswith('hzT') else BF16)
            hc['Kf'] = self.scratch("Kf" + sfx, [2, 4, 2, 128, hc['NK'] * 128], BF16)
            self.hcfg[sfx] = hc
        self.hy_cw = self.inp("hy_cw", [DEPTH, 128, 12, 3])
        self.hy_cb = self.inp("hy_cb", [DEPTH, 128, 12])
        self.hy_b2 = self.inp("hy_b2", [DEPTH, 128, 4, 2])
        self.hy_w1 = self.inp("hy_w1", [DEPTH, 33, 128])
        self.hy_w23 = self.inp("hy_w23", [DEPTH, 2, 128, 128])
        self.hy_fb = self.inp("hy_fb", [DEPTH, 128, 4])
        self.hy_wo = self.inp("hy_wo", [DEPTH, 128, 2, 512])

    def hy_load_consts(self, hc):
        P = self.P
        N1, NK, NH = hc['N1'], hc['NK'], hc['NH']
        self.hc = hc
        self.cF1 = P.sbuf("cF1", [N1, 2 * NK], BF16); P.dma(self.cF1[:], hc['hF1'].ap, writes=[self.cF1])
        self.cS2 = P.sbuf("cS2", [128, NK, 192], BF16)
        for q in range(0, NK, 13):
            q1 = min(NK, q + 13)
            P.dma(self.cS2[:, q:q1, :], hc['hS2'].ap[:, q:q1, :], writes=[(self.cS2, q)])
        self.cG = P.sbuf("cG", [128, 128], BF16); P.dma(self.cG[:], hc['hG'].ap, writes=[self.cG])
        self.cT4 = P.sbuf("cT4", [NK, 2, 64, NH], BF16); P.dma(self.cT4[:], hc['hT4'].ap, writes=[self.cT4])

    def fft_fwd(self, lay, K, A, tagbufs, laydep=None):
        P = self.P
        NK = self.hc['NK']
        ps1 = tagbufs['ps1']
        n = 0
        for c0 in range(0, 128, 7):
            nch = min(7, 128 - c0)
            ps = ps1[n % 2]; n += 1
            for j in range(nch):
                lhsT = pap(lay, 0, c0 + j, [K, [128, 64]])
                P.E('pe', 'matmul', ps[0:64, j, :], lhsT, self.cF1[0:K, 0:NK], start=True, stop=True, tile_position=(0, 0),
                    reads=[laydep or lay, self.cF1], writes=[ps])
                P.E('pe', 'matmul', ps[64:128, j, :], lhsT, self.cF1[0:K, NK:2 * NK], start=True, stop=True, tile_position=(0, 64),
                    reads=[laydep or lay, self.cF1], writes=[ps])
            o_ = pap(A, 0, c0, [128, [1, nch], [128, NK]]); i_ = pap(ps, 0, 0, [128, [NK, nch], [1, NK]])
            if n % 2:
                P.E('dve', 'tensor_copy', o_, i_, reads=[ps], writes=[(A, c0)])
            else:
                P.E('act', 'copy', o_, i_, reads=[ps], writes=[(A, c0)])

    def phase_hyfilt(self, i, sfx=""):
        P = self.P
        hc = self.hcfg[sfx]
        Lh, NFFT, N1, NK, NH = hc['Lh'], hc['N'], hc['N1'], hc['NK'], hc['NH']
        m = P.mark()
        self.hy_load_consts(hc)
        Hb = P.sbuf("fHb", [128, NFFT], BF16)
        wo = P.sbuf("fwo", [128, 2, 512], F32); P.dma(wo[:], self.hy_wo.ap[i], writes=[wo])
        wob = P.sbuf("fwob", [128, 2, 512], BF16)
        P.E('pool', 'tensor_copy', wob[:], wo[:], reads=[wo], writes=[wob])
        m2 = P.mark()
        zT = P.sbuf("zT", [33, NFFT], F32)
        ZB = min(2048, NFFT)
        for q in range(NFFT // ZB):
            P.dma(zT[:, q * ZB:(q + 1) * ZB], hc['hzT'].ap[:, q * ZB:(q + 1) * ZB], writes=[(zT, q)])
        w1 = P.sbuf("fw1", [33, 128], F32); P.dma(w1[:], self.hy_w1.ap[i], writes=[w1])
        w23 = P.sbuf("fw23", [128, 2, 128], F32)
        for q in range(2):
            P.dma(w23[:, q, :], self.hy_w23.ap[i, q], writes=[(w23, q)])
        fb = P.sbuf("ffb", [128, 4], F32); P.dma(fb[:], self.hy_fb.ap[i], writes=[fb])
        sb = P.sbuf("fsb", [128, 4], F32)
        P.E('dve', 'tensor_scalar', sb[:, 0:1], fb[:, 0:1], float(1.0 / (2 * np.pi)), None, op0=ALU.mult, reads=[fb], writes=[sb])
        P.E('dve', 'tensor_scalar', sb[:, 1:4], fb[:, 1:4], sb[:, 0:1], None, op0=ALU.mult, reads=[fb, sb], writes=[sb])
        ha = P.sbuf("fha", [128, NFFT], F32)
        hb = P.sbuf("fhb", [128, NFFT], F32)
        t1 = [P.sbuf("ft1_%d" % k, [128, 512], F32) for k in range(2)]
        t2 = [P.sbuf("ft2_%d" % k, [128, 512], F32) for k in range(2)]
        pps = [P.psum("fps%d" % k, [128, 512], F32) for k in range(2)]
        n = 0
        for layer in range(3):
            src = zT if layer == 0 else (ha if layer == 1 else hb)
            dst = ha if layer != 1 else hb
            for blk in range(NFFT // 512):
                ps = pps[n % 2]; a = t1[n % 2]; d = t2[n % 2]; n += 1
                sl = slice(blk * 512, (blk + 1) * 512)
                if layer == 0:
                    P.E('pe', 'matmul', ps[:], w1[:], zT[:, sl], start=True, stop=True, reads=[w1, (zT, (blk * 512) // ZB)], writes=[ps])
                else:
                    P.E('pe', 'matmul', ps[:], w23[:, layer - 1, :], src[:, sl], start=True, stop=True,
                        reads=[w23, (src, blk)], writes=[ps])
                P.E('dve', 'tensor_scalar', a[:], ps[:], sb[:, 0:1], sb[:, 1 + layer:2 + layer], op0=ALU.mult, op1=ALU.add,
                    reads=[ps, sb], writes=[a])
                P.E('dve', 'tensor_scalar_add', d[:], a[:], MAGIC, reads=[a], writes=[d])
                P.E('dve', 'scalar_tensor_tensor', d[:], d[:], MAGIC, a[:], op0=ALU.subtract, op1=ALU.subtract,
                    reads=[a, d], writes=[d])
                P.E('act', 'activation', dst[:, sl], d[:], AF.Sin, scale=-6.28318, reads=[d], writes=[(dst, blk)])
        P.E('pool', 'memset', Hb[:], 0.0, writes=[Hb])
        P.E('act', 'copy', Hb[0:64, 0:Lh], ha[0:64, 0:Lh], reads=[ha, Hb], writes=[(Hb, 0)])
        P.E('dve', 'tensor_copy', Hb[64:128, Lh + 1:NFFT], ha[64:128, Lh + 1:NFFT], reads=[ha, Hb], writes=[(Hb, 1)])
        P.E('dve', 'tensor_copy', Hb[64:128, 0:1], ha[64:128, 0:1], reads=[ha, Hb], writes=[(Hb, 2)])
        P.release(m2)
        klay = P.sbuf("klay", [N1, 64, 128], BF16)
        wing = P.sbuf("wing", [N1, 64, 128], BF16)
        Af = P.sbuf("Af", [128, NK, 128], BF16)
        KA = P.sbuf("KA", [128, NK, 128], BF16)
        KB = P.sbuf("KB", [128, NK, 128], BF16)
        kps = [P.psum("kps%d" % k, [128, 4, 128], F32) for k in range(2)]
        ps1 = [P.psum("fps1_%d" % k, [128, 7, NK], F32) for k in range(2)]
        for g in range(4):
            P.dma(wing[:], hc['hwin'].ap[:, :, g * 128:(g + 1) * 128], reads=[hc['hwin']], writes=[wing])
            for cv in range(2):
                n = 0
                for q in range(16):
                    ps = kps[n % 2]; n += 1
                    for j in range(4):
                        n2 = q * 4 + j
                        P.E('pe', 'matmul', ps[0:N1, j, :], pap(Hb, 0, n2, [128, [64, N1]]), wob[:, cv, g * 128:(g + 1) * 128],
                            start=True, stop=True, reads=[Hb, wob], writes=[ps])
                    P.E('dve', 'tensor_tensor', klay[:, q * 4:q * 4 + 4, :], ps[0:N1, :, :], wing[:, q * 4:q * 4 + 4, :], op=ALU.mult,
                        reads=[ps, wing], writes=[(klay, q)])
                self.fft_fwd(klay, N1, Af, dict(ps1=ps1))
                n = 0
                for q in range(0, NK, 4):
                    nk = min(4, NK - q)
                    psa = kps[n % 2]; n += 1
                    for j in range(nk):
                        k1 = q + j
                        P.E('pe', 'matmul', psa[0:64, j, :], self.cS2[:, k1, 64:128], Af[:, k1, :], tile_position=(0, 0),
                            start=True, stop=True, reads=[self.cS2, Af], writes=[psa])
                        P.E('pe', 'matmul', psa[64:128, j, :], self.cS2[:, k1, 64:128], Af[:, k1, :], tile_position=(0, 64),
                            start=True, stop=True, reads=[self.cS2, Af], writes=[psa])
                    P.E('act', 'copy', KA[:, q:q + nk, :], psa[:, 0:nk, :], reads=[psa], writes=[(KA, q)])
                    psb = kps[n % 2]; n += 1
                    for j in range(nk):
                        k1 = q + j
                        P.E('pe', 'matmul', psb[0:64, j, :], self.cS2[:, k1, 0:64], Af[:, k1, :], tile_position=(0, 0),
                            start=True, stop=True, reads=[self.cS2, Af], writes=[psb])
                        P.E('pe', 'matmul', psb[64:128, j, :], self.cS2[:, k1, 0:64], Af[:, k1, :], tile_position=(0, 64),
                            start=True, stop=True, reads=[self.cS2, Af], writes=[psb])
                    P.E('dve', 'tensor_scalar', KB[0:64, q:q + nk, :], psb[0:64, 0:nk, :], -1.0, None, op0=ALU.mult,
                        reads=[psb], writes=[(KB, q)])
                    P.E('dve', 'tensor_copy', KB[64:128, q:q + nk, :], psb[64:128, 0:nk, :], reads=[psb], writes=[(KB, -q - 1)])
                P.dma(hc['Kf'].ap[cv, g, 0], KA[:].rearrange("p a b -> p (a b)"), reads=[KA], writes=[(hc['Kf'], (cv, g, 0))], eng='act')
                P.dma(hc['Kf'].ap[cv, g, 1], KB[:].rearrange("p a b -> p (a b)"), reads=[KB], writes=[(hc['Kf'], (cv, g, 1))], eng='act')
        P.release(m)

    def hy_conv(self, cv, g, xT, zT, bufs):
        P = self.P
        hc = self.hc
        NK, NH = hc['NK'], hc['NH']
        CK = min(13, NK)
        A, Y, Bt, KAc, KBc = bufs['A'], bufs['Y'], bufs['B'], bufs['KA'], bufs['KB']
        tp = bufs['tp']; n = 0
        for q in range(8):
            ps = tp[n % 2]; n += 1
            for j in range(8):
                n2 = q * 8 + j
                P.E('pe', 'transpose', ps[0:NH, j, :], pap(xT, 0, n2, [128, [64, NH]]), self.ident[:], reads=[xT, self.ident], writes=[ps])
            o_ = pap(Bt, 0, q * 1024, [NH, [128, 8], [1, 128]])
            if q % 2 == 0:
                P.E('act', 'copy', o_, ps[0:NH, :, :], reads=[ps], writes=[(Bt, ('lay', q))])
            else:
                P.E('dve', 'tensor_copy', o_, ps[0:NH, :, :], reads=[ps], writes=[(Bt, ('lay', q))])
        self.fft_fwd(Bt, NH, A, bufs, laydep=Bt)
        p2 = bufs['p2']; tm = bufs['tm']; n = 0
        Kf = hc['Kf']
        for k1 in range(NK):
            ch = k1 // CK
            KA = KAc[ch % 2]; KB = KBc[ch % 2]
            if k1 % CK == 0:
                P.dma(KA[:, 0:CK, :].rearrange("p a b -> p (a b)"), Kf.ap[cv, g, 0, :, ch * CK * 128:(ch + 1) * CK * 128], reads=[(Kf, (cv, g, 0))], writes=[KA])
                P.dma(KB[:, 0:CK, :].rearrange("p a b -> p (a b)"), Kf.ap[cv, g, 1, :, ch * CK * 128:(ch + 1) * CK * 128], reads=[(Kf, (cv, g, 1))], writes=[KB])
            kk = k1 % CK
            ps = p2[n % 2]; ta = tm[n % 3]; n += 1
            P.E('pe', 'matmul', ps[:, 0, :], self.cS2[:, k1, 64:192], A[:, k1, :], start=True, stop=True, reads=[self.cS2, A], writes=[ps])
            P.E('pe', 'matmul', ps[:, 1, :], self.cS2[:, k1, 0:128], A[:, k1, :], start=True, stop=True, reads=[self.cS2, A], writes=[ps])
            P.E('dve', 'tensor_tensor', ta[:, 0, :], ps[:, 0, :], KA[:, kk, :], op=ALU.mult, reads=[ps, KA], writes=[(ta, 0)])
            P.E('dve', 'tensor_tensor', ta[:, 1, :], ps[:, 1, :], KB[:, kk, :], op=ALU.mult, reads=[ps, KB], writes=[(ta, 1)])
            P.E('pool', 'tensor_tensor', Y[:, k1, :], ta[:, 0, :], ta[:, 1, :], op=ALU.add, reads=[ta], writes=[(Y, k1)])
        p3 = bufs['p3']; n = 0
        for c0 in range(0, 128, 4):
            ps = p3[n % 2]; n += 1
            for j in range(4):
                P.E('pe', 'matmul', ps[0:NK, j, :], pap(Y, 0, c0 + j, [128, [128, NK]]), self.cG[:], start=True, stop=True,
                    reads=[Y, self.cG], writes=[ps])
            o_ = pap(Bt, 0, c0, [NK, [1, 4], [128, 128]]); i_ = pap(ps, 0, 0, [NK, [128, 4], [1, 128]])
            if n % 2:
                P.E('act', 'copy', o_, i_, reads=[ps], writes=[(Bt, c0)])
            else:
                P.E('dve', 'tensor_copy', o_, i_, reads=[ps], writes=[(Bt, c0)])
        p4 = bufs['p4']; n = 0
        for q in range(8):
            ps = p4[n % 2]; n += 1
            for j in range(8):
                n2 = q * 8 + j
                for ri in range(2):
                    P.E('pe', 'matmul', ps[:, j, 0:NH], Bt[0:NK, ri * 64 + n2, :], self.cT4[0:NK, ri, n2, :], start=(ri == 0), stop=(ri == 1),
                        reads=[Bt, self.cT4], writes=[ps])
            o_ = pap(zT, 0, q * 8, [128, [1, 8], [64, NH]]); i_ = pap(ps, 0, 0, [128, [64, 8], [1, NH]])
            if q % 2:
                P.E('act', 'copy', o_, i_, reads=[ps], writes=[(zT, q)])
            else:
                P.E('dve', 'tensor_copy', o_, i_, reads=[ps], writes=[(zT, q)])

    def phase_hyena(self, i, sfx=""):
        P = self.P
        hc = self.hcfg[sfx]
        L, NK = hc['Lh'], hc['NK']
        uT_ = self.ucT if sfx else self.uT
        yT_ = self.ycT if sfx else self.yT
        m = P.mark()
        self.hy_load_consts(hc)
        cw = P.sbuf("hcw", [128, 12, 3], F32); P.dma(cw[:], self.hy_cw.ap[i], writes=[cw])
        cb = P.sbuf("hcb", [128, 12], F32); P.dma(cb[:], self.hy_cb.ap[i], writes=[cb])
        b2 = P.sbuf("hb2", [128, 4, 2], F32); P.dma(b2[:], self.hy_b2.ap[i], writes=[b2])
        urs = [P.sbuf("huraw%d" % k, [128, L], BF16) for k in range(2)]
        nur = 0
        acc = P.sbuf("hacc", [128, L], F32)
        vT = P.sbuf("hvT", [128, L], BF16); x1T = P.sbuf("hx1T", [128, L], BF16); x2T = P.sbuf("hx2T", [128, L], BF16)
        bufs = dict(A=P.sbuf("hA", [128, NK, 128], BF16), Y=P.sbuf("hY", [128, NK, 128], BF16),
                    B=P.sbuf("hB", [65, 128, 128], BF16), KA=[P.sbuf("hKA%d" % k, [128, 13, 128], BF16) for k in range(2)],
                    KB=[P.sbuf("hKB%d" % k, [128, 13, 128], BF16) for k in range(2)],
                    tm=[P.sbuf("htm%d" % k, [128, 2, 128], F32) for k in range(3)])
        pA = [P.psum("hpA%d" % k, [128, 512], F32) for k in range(2)]
        pT = [P.psum("hpT%d" % k, [128, 1024], BF16) for k in range(2)]
        bufs['tp'] = [PV(b, [64, 8, 128]) for b in pT]
        bufs['ps1'] = [PV(b, [128, 7, NK]) for b in pA]
        bufs['p2'] = [PV(b, [128, 2, 128]) for b in pA]
        bufs['p3'] = [PV(b, [128, 4, 128]) for b in pA]
        bufs['p4'] = [PV(b, [128, 8, 64]) for b in pA]
        for g in range(4 if HY_GROUPS is None else HY_GROUPS):
            for k, dstT in enumerate((vT, x1T, x2T)):
                ct = k * 4 + g
                uraw = urs[nur % 2]; nur += 1
                P.dma(uraw[:], uT_.ap[ct * 128:(ct + 1) * 128, :], reads=[(uT_, ct)], writes=[uraw])
                conv_fm(P, uraw, cw[:, ct, :], cb[:, ct:ct + 1], acc, dstT, L, AF.Identity, cdeps=[cw, cb])
            self.hy_conv(0, g, vT, acc, bufs)
            P.E('dve', 'scalar_tensor_tensor', acc[:], vT[:], b2[:, g, 0:1], acc[:], op0=ALU.mult, op1=ALU.add, reads=[vT, acc, b2], writes=[acc])
            P.E('dve', 'tensor_tensor', vT[:], acc[:], x1T[:], op=ALU.mult, reads=[acc, x1T], writes=[vT])
            self.hy_conv(1, g, vT, acc, bufs)
            uraw = urs[nur % 2]; nur += 1
            P.dma(uraw[:], uT_.ap[(12 + g) * 128:(13 + g) * 128, :], reads=[(uT_, 12 + g)], writes=[uraw])
            P.E('act', 'activation', x1T[:], uraw[:], AF.Silu, reads=[uraw], writes=[x1T])
            P.E('dve', 'scalar_tensor_tensor', acc[:], vT[:], b2[:, g, 1:2], acc[:], op0=ALU.mult, op1=ALU.add, reads=[vT, acc, b2], writes=[acc])
            P.E('dve', 'tensor_tensor', acc[:], acc[:], x2T[:], op=ALU.mult, reads=[acc, x2T], writes=[acc])
            P.E('dve', 'tensor_tensor', vT[:], acc[:], x1T[:], op=ALU.mult, reads=[acc, x1T], writes=[vT])
            P.dma(yT_.ap[g * 128:(g + 1) * 128, :], vT[:], reads=[vT], writes=[(yT_, g)], eng='act')
        P.release(m)


HY_GROUPS = None


class PV(Buf):
    def __init__(self, base, shape):
        self.base = base
        self.name = base.name
        self.is_psum = True
        t = base.t
        ps = t[:].ap[0][0]
        self.shape = shape
        self.ps = ps
        self._t = t

    @property
    def st(self):
        return self.base.st

    @st.setter
    def st(self, v):
        self.base.st = v

    @property
    def t(self):
        return self._t

    def full(self):
        return self._view(self.shape[0], [slice(None)] * (len(self.shape) - 1))

    def _view(self, npart, idx, p0=0):
        strides = []
        s = 1
        for d in reversed(self.shape[1:]):
            strides.insert(0, s); s *= d
        off = 0; dims = []
        for st_, d, ix in zip(strides, self.shape[1:], idx):
            if isinstance(ix, int):
                off += st_ * ix
            else:
                a, b, _ = ix.indices(d)
                off += st_ * a; dims.append([st_, b - a])
        return bass.AP(self._t, p0 * self.ps + off, [[self.ps, npart]] + dims)

    def __getitem__(self, key):
        if not isinstance(key, tuple):
            key = (key,)
        key = list(key) + [slice(None)] * (len(self.shape) - len(key))
        pk = key[0]
        a, b, _ = pk.indices(self.shape[0]) if isinstance(pk, slice) else (pk, pk + 1, 1)
        return self._view(b - a, key[1:], a)


def ssd_consts():
    r = np.arange(128)[:, None]; c = np.arange(128)[None, :]
    tri = np.stack([(r <= c), (r > c), (r >= c), (r < c)], 1).astype(np.float32)
    return dict(stri=tri)

UPI, LOS, LOI, UPS = 0, 1, 2, 3


class SSDMixin:
    def declare_ssd(self):
        P = self.P
        self.stri = self.const("stri", ssd_consts()['stri'])
        self.ssm_cw = self.inp("ssm_cw", [DEPTH, 128, 8, 3])
        self.ssm_cb = self.inp("ssm_cb", [DEPTH, 128, 8])
        self.ssm_dtb = self.inp("ssm_dtb", [DEPTH, 16])
        self.ssm_alog = self.inp("ssm_alog", [DEPTH, 16])
        self.ssm_dd = self.inp("ssm_dd", [DEPTH, 8])
        self.ssm_nw = self.inp("ssm_nw", [DEPTH, 512])
        self.tri = P.sbuf("tri", [128, 4, 128], F32); P.dma(self.tri[:], self.stri.ap, writes=[self.tri])
        self.onesf = P.sbuf("onesf", [128, 128], F32); P.E('pool', 'memset', self.onesf[:], 1.0, writes=[self.onesf])
        self.Sf0 = P.sbuf("Sf0", [128, 512], F32); self.Sb0 = P.sbuf("Sb0", [128, 512], F32)
        self.one1 = P.sbuf("one1", [128, 1], F32); P.E('pool', 'memset', self.one1[:], 1.0, writes=[self.one1])
        self.epsr = P.sbuf("epsr", [128, 1], F32); P.E('pool', 'memset', self.epsr[:], RMS_EPS, writes=[self.epsr])

    def phase_ssd(self, i, ctxmode, want_y=True):
        P = self.P
        Lx = LC if ctxmode else L
        NC = Lx // 128
        uT = self.ucT if ctxmode else self.uT
        u_sg = self.uc_sg if ctxmode else self.u_sg
        u_dt = self.uc_dt if ctxmode else self.u_dt
        yT = self.ycT if ctxmode else self.yT
        m = P.mark()
        tri = self.tri
        cw = P.sbuf("scw", [128, 8, 3], F32); P.dma(cw[:], self.ssm_cw.ap[i], writes=[cw])
        cb = P.sbuf("scb", [128, 8], F32); P.dma(cb[:], self.ssm_cb.ap[i], writes=[cb])
        x_tm = P.sbuf("x_tm", [128, NC, 512], BF16)
        B_tm = P.sbuf("B_tm", [128, NC, 256], BF16)
        BT = P.sbuf("BT", [128, 2, Lx], BF16)
        CT = P.sbuf("CT", [128, 2, Lx], BF16)
        m1 = P.mark()
        surs = [P.sbuf("suraw%d" % k, [128, Lx], BF16) for k in range(2)]
        acc = P.sbuf("sacc", [128, Lx], F32)
        xTc = P.sbuf("sxTc", [128, Lx], BF16)
        tpp = [P.psum("stp%d" % k, [128, 8, 128], BF16) for k in range(2)]
        n = 0
        for ct in range(8):
            uraw = surs[ct % 2]
            P.dma(uraw[:], uT.ap[2048 + ct * 128:2048 + (ct + 1) * 128, :], reads=[(uT, 16 + ct)], writes=[uraw])
            if ct < 4:
                dst, dsl = xTc, xTc[:, :]
            elif ct < 6:
                dst, dsl = BT, BT[:, ct - 4, :]
            else:
                dst, dsl = CT, CT[:, ct - 6, :]
            P.E('act', 'activation', acc[:], uraw[:], AF.Identity, bias=cb[:, ct:ct + 1], scale=cw[:, ct, 1:2], reads=[uraw, cw, cb], writes=[acc])
            P.E('dve', 'scalar_tensor_tensor', acc[:, 1:Lx], uraw[:, 0:Lx - 1], cw[:, ct, 0:1], acc[:, 1:Lx], op0=ALU.mult, op1=ALU.add, reads=[uraw, acc, cw], writes=[acc])
            P.E('dve', 'scalar_tensor_tensor', acc[:, 0:Lx - 1], uraw[:, 1:Lx], cw[:, ct, 2:3], acc[:, 0:Lx - 1], op0=ALU.mult, op1=ALU.add, reads=[uraw, acc, cw], writes=[acc])
            P.E('act', 'activation', dsl, acc[:], AF.Silu, reads=[acc], writes=[(dst, ct)])
            if ct < 6:
                for c0 in range(0, NC, 8):
                    ncc = min(8, NC - c0)
                    ps = tpp[n % 2]; n += 1
                    for j in range(ncc):
                        P.E('pe', 'transpose', ps[:, j, :], dsl[:, (c0 + j) * 128:(c0 + j + 1) * 128], self.ident[:], reads=[(dst, ct), self.ident], writes=[ps])
                    if ct < 4:
                        o_ = pap(x_tm, 0, c0 * 512 + ct * 128, [128, [512, ncc], [1, 128]]); key = (x_tm, (ct, c0))
                    else:
                        o_ = pap(B_tm, 0, c0 * 256 + (ct - 4) * 128, [128, [256, ncc], [1, 128]]); key = (B_tm, (ct, c0))
                    if n % 2:
                        P.E('act', 'copy', o_, ps[:, 0:ncc, :], reads=[ps], writes=[key])
                    else:
                        P.E('dve', 'tensor_copy', o_, ps[:, 0:ncc, :], reads=[ps], writes=[key])
        P.release(m1)
        W8 = NC * 8
        dtr = P.sbuf("dtr", [128, NC, 16], F32)
        P.dma(dtr[:], u_dt.ap.rearrange("(c p) h -> p c h", p=128), reads=[u_dt], writes=[dtr])
        dtb = P.sbuf("dtb", [128, 16], F32); P.dma(dtb[:], self.ssm_dtb.ap[i:i + 1, :].partition_broadcast(128), writes=[dtb])
        alg = P.sbuf("alg", [128, 16], F32); P.dma(alg[:], self.ssm_alog.ap[i:i + 1, :].partition_broadcast(128), writes=[alg])
        dbc = P.sbuf("dbc", [128, 8], F32); P.dma(dbc[:], self.ssm_dd.ap[i:i + 1, :].partition_broadcast(128), writes=[dbc])
        nwb = P.sbuf("nwb", [128, 512], F32); P.dma(nwb[:], self.ssm_nw.ap[i:i + 1, :].partition_broadcast(128), writes=[nwb])
        P.E('act', 'activation', alg[:], alg[:], AF.Exp, reads=[alg], writes=[alg])
        P.E('dve', 'tensor_scalar', alg[:], alg[:], -1.0, None, op0=ALU.mult, reads=[alg], writes=[alg])
        P.E('dve', 'tensor_tensor', dtr[:], dtr[:], dtb[:].unsqueeze(1).to_broadcast([128, NC, 16]), op=ALU.add, reads=[dtr, dtb], writes=[dtr])
        P.E('act', 'activation', dtr[:], dtr[:], AF.Exp, reads=[dtr], writes=[dtr])
        P.E('act', 'activation', dtr[:], dtr[:], AF.Ln, bias=self.one1[:, 0:1], reads=[dtr, self.one1], writes=[dtr])
        dt = [P.sbuf("dt%d" % d, [128, W8], F32) for d in range(2)]
        da = [P.sbuf("da%d" % d, [128, W8], F32) for d in range(2)]
        for d in range(2):
            P.E('dve', 'tensor_copy', dt[d][:].rearrange("p (c h) -> p c h", h=8), dtr[:, :, d * 8:(d + 1) * 8], reads=[dtr], writes=[dt[d]])
            P.E('dve', 'tensor_tensor', da[d][:].rearrange("p (c h) -> p c h", h=8), dtr[:, :, d * 8:(d + 1) * 8],
                alg[:, d * 8:(d + 1) * 8].unsqueeze(1).to_broadcast([128, NC, 8]), op=ALU.mult, reads=[dtr, alg], writes=[da[d]])
        cps = [P.psum("cps%d" % k, [128, 512], F32) for k in range(4)]
        A = [P.sbuf("Acs%d" % d, [128, W8], F32) for d in range(2)]
        din = [P.sbuf("din%d" % d, [128, W8], F32) for d in range(2)]
        dtx = [P.sbuf("dtx%d" % d, [128, W8], F32) for d in range(2)]
        cd = [P.sbuf("cd%d" % d, [128, W8], F32) for d in range(2)]
        for d in range(2):
            pa, pt = cps[d * 2], cps[d * 2 + 1]
            P.E('pe', 'matmul', pa[:, 0:W8], tri[:, UPI if d == 0 else UPS, :], da[d][:], start=True, stop=True, reads=[tri, da[d]], writes=[pa])
            P.E('pe', 'matmul', pt[:, 0:W8], self.onesf[:], da[d][:], start=True, stop=True, reads=[self.onesf, da[d]], writes=[pt])
            P.E('dve', 'tensor_copy', A[d][:], pa[:, 0:W8], reads=[pa], writes=[A[d]])
            P.E('act', 'activation', cd[d][:], pt[:, 0:W8], AF.Exp, reads=[pt], writes=[cd[d]])
            P.E('dve', 'tensor_tensor', dtx[d][:], pt[:, 0:W8], A[d][:], op=ALU.subtract, reads=[pt, A[d]], writes=[dtx[d]])
            if d == 0:
                P.E('act', 'activation', din[d][:], A[d][:], AF.Exp, reads=[A[d]], writes=[din[d]])
                P.E('act', 'activation', dtx[d][:], dtx[d][:], AF.Exp, reads=[dtx[d]], writes=[dtx[d]])
            else:
                P.E('act', 'activation', din[d][:], dtx[d][:], AF.Exp, reads=[dtx[d]], writes=[din[d]])
                P.E('act', 'activation', dtx[d][:], A[d][:], AF.Exp, reads=[A[d], din[d]], writes=[dtx[d]])
            P.E('dve', 'tensor_tensor', dtx[d][:], dtx[d][:], dt[d][:], op=ALU.mult, reads=[dtx[d], dt[d]], writes=[dtx[d]])
        Sf = P.sbuf("Sf", [128, 512], F32); Sb = P.sbuf("Sb", [128, 512], F32)
        if ctxmode:
            P.E('pool', 'memset', Sf[:], 0.0, writes=[Sf]); P.E('pool', 'memset', Sb[:], 0.0, writes=[Sb])
        else:
            P.E('pool', 'tensor_copy', Sf[:], self.Sf0[:], reads=[self.Sf0], writes=[Sf])
            P.E('pool', 'tensor_copy', Sb[:], self.Sb0[:], reads=[self.Sb0], writes=[Sb])
        Sbin = P.sbuf("Sbin", [128, NC, 512], BF16)
        xde = [P.sbuf("xde%d" % k, [128, 512], BF16) for k in range(2)]

        def bc8(t, c):
            return pap(t, 0, c * 8, [128, [1, 8], [0, 64]])

        def v8(t):
            return t[:].rearrange("p (h d) -> p h d", d=64)

        def chunk_state(c, d, S, nn):
            xd_ = xde[nn % 2]
            P.E('dve', 'tensor_tensor', v8(xd_), x_tm[:, c, :].rearrange("p (h d) -> p h d", d=64), bc8(dtx[d], c), op=ALU.mult,
                reads=[x_tm, dtx[d]], writes=[xd_])
            ps = cps[0]
            for h in range(8):
                g = h // 4
                P.E('pe', 'matmul', ps[:, h * 64:(h + 1) * 64], B_tm[:, c, g * 128:(g + 1) * 128], xd_[:, h * 64:(h + 1) * 64],
                    start=True, stop=True, reads=[B_tm, xd_], writes=[ps])
            P.E('dve', 'tensor_tensor', v8(S), v8(S), bc8(cd[d], c), op=ALU.mult, reads=[S, cd[d]], writes=[S])
            P.E('dve', 'tensor_tensor', S[:], S[:], ps[:], op=ALU.add, reads=[S, ps], writes=[S])

        for nn, c in enumerate(range(NC - 1, -1, -1)):
            P.E('act', 'copy', Sbin[:, c, :], Sb[:], reads=[Sb], writes=[(Sbin, c)])
            chunk_state(c, 1, Sb, nn)
        if ctxmode:
            P.E('pool', 'tensor_copy', self.Sb0[:], Sb[:], reads=[Sb], writes=[self.Sb0])
        if want_y:
            gps = cps[2]; sgp = [cps[3], cps[1]]
            m3 = P.mark()
            ydp = P.psum("ydp", [128, 512], F32)
            yop = [P.psum("yop%d" % d, [128, 512], F32) for d in range(2)]
            ytp = P.psum("ytp", [128, 4, 128], BF16)
            GTm = P.sbuf("GTm", [128, 2, 2, 128], BF16)
            xd = [P.sbuf("xd%d" % d, [128, 512], BF16) for d in range(2)]
            Sfb = P.sbuf("Sfb", [128, 512], BF16)
            rhsp = [P.sbuf("rhsp%d" % k, [128, 4, 128], F32) for k in range(2)]
            Ee = [P.sbuf("Ee%d" % k, [128, 4, 128], BF16) for k in range(2)]
            Mm = [P.sbuf("Mm%d" % k, [128, 4, 128], BF16) for k in range(2)]
            gt = [P.sbuf("gt%d" % k, [128, 512], BF16) for k in range(2)]
            sg = P.sbuf("ssg", [128, 512], F32)
            y = P.sbuf("sy", [128, 512], F32); t1 = P.sbuf("st1", [128, 512], F32); t2 = P.sbuf("st2", [128, 512], F32)
            ss = P.sbuf("sss", [128, 2], F32)
            yb = P.sbuf("syb", [128, 512], BF16)
            ysT = [P.sbuf("ysT%d" % k, [128, 4, 512], BF16) for k in range(2)]
            nb = 0
        for c in range(NC):
            if want_y:
                cs = slice(c * 128, (c + 1) * 128)
                P.dma(gt[c % 2][:], u_sg.ap[c * 128:(c + 1) * 128, :], reads=[u_sg], writes=[gt[c % 2]])
                for g in range(2):
                    P.E('pe', 'matmul', gps[:, g * 128:(g + 1) * 128], BT[:, g, cs], CT[:, g, cs], start=True, stop=True, reads=[BT, CT], writes=[gps])
                for d in range(2):
                    P.E('dve', 'tensor_tensor', GTm[:, d, :, :], gps[:, 0:256].rearrange("p (g l) -> p g l", g=2),
                        tri[:, UPI if d == 0 else LOI, :].unsqueeze(1).to_broadcast([128, 2, 128]), op=ALU.mult, reads=[gps, tri], writes=[(GTm, d)])
                    P.E('dve', 'tensor_tensor', v8(xd[d]), x_tm[:, c, :].rearrange("p (h d) -> p h d", d=64), bc8(dt[d], c), op=ALU.mult,
                        reads=[x_tm, dt[d]], writes=[xd[d]])
                P.E('act', 'copy', Sfb[:], Sf[:], reads=[Sf], writes=[Sfb])
                for g in range(2):
                    mms = []
                    for d in range(2):
                        rp = rhsp[nb % 2]; ee = Ee[nb % 2]; mm_ = Mm[nb % 2]; sp_ = sgp[nb % 2]; nb += 1
                        mms.append(mm_)
                        V = UPI if d == 0 else LOI
                        U = LOS if d == 0 else UPS
                        P.E('dve', 'tensor_tensor', rp[:], tri[:, V, :].unsqueeze(1).to_broadcast([128, 4, 128]),
                            pap(da[d], 0, c * 8 + g * 4, [128, [1, 4], [0, 128]]), op=ALU.mult, reads=[tri, da[d]], writes=[rp])
                        for hh in range(4):
                            P.E('pe', 'matmul', sp_[:, hh * 128:(hh + 1) * 128], tri[:, U, :], rp[:, hh, :], start=True, stop=True, reads=[tri, rp], writes=[sp_])
                        P.E('act', 'activation', ee[:].rearrange("p a b -> p (a b)"), sp_[:], AF.Exp, reads=[sp_], writes=[ee])
                        P.E('pool', 'tensor_tensor', mm_[:], ee[:], GTm[:, d, g, :].unsqueeze(1).to_broadcast([128, 4, 128]), op=ALU.mult,
                            reads=[ee, (GTm, d)], writes=[mm_])
                    for hh in range(4):
                        h = g * 4 + hh
                        for d in range(2):
                            P.E('pe', 'matmul', ydp[:, h * 64:(h + 1) * 64], mms[d][:, hh, :], xd[d][:, h * 64:(h + 1) * 64], start=(d == 0), stop=(d == 1),
                                reads=[mms[d], xd[d]], writes=[ydp])
                for d in range(2):
                    for h in range(8):
                        g = h // 4
                        rhs = Sfb[:, h * 64:(h + 1) * 64] if d == 0 else Sbin[:, c, h * 64:(h + 1) * 64]
                        P.E('pe', 'matmul', yop[d][:, h * 64:(h + 1) * 64], CT[:, g, cs], rhs, start=True, stop=True,
                            reads=[CT, Sfb if d == 0 else (Sbin, c)], writes=[yop[d]])
            chunk_state(c, 0, Sf, c)
            if want_y:
                P.E('dve', 'tensor_tensor', v8(t1), yop[0][:].rearrange("p (h d) -> p h d", d=64), bc8(din[0], c), op=ALU.mult, reads=[yop[0], din[0]], writes=[t1])
                P.E('dve', 'tensor_tensor', v8(t2), yop[1][:].rearrange("p (h d) -> p h d", d=64), bc8(din[1], c), op=ALU.mult, reads=[yop[1], din[1]], writes=[t2])
                P.E('dve', 'tensor_tensor', y[:], ydp[:], t1[:], op=ALU.add, reads=[ydp, t1], writes=[y])
                P.E('pool', 'tensor_tensor', y[:], y[:], t2[:], op=ALU.add, reads=[y, t2], writes=[y])
                P.E('pool', 'tensor_tensor', v8(t1), x_tm[:, c, :].rearrange("p (h d) -> p h d", d=64), pap(dbc, 0, 0, [128, [1, 8], [0, 64]]), op=ALU.mult,
                    reads=[x_tm, dbc, t1], writes=[t1])
                P.E('pool', 'tensor_tensor', y[:], y[:], t1[:], op=ALU.add, reads=[y, t1], writes=[y])
                P.E('act', 'activation', sg[:], gt[c % 2][:], AF.Silu, reads=[gt[c % 2]], writes=[sg])
                P.E('dve', 'tensor_tensor', y[:], y[:], sg[:], op=ALU.mult, reads=[y, sg], writes=[y])
                P.E('pool', 'tensor_tensor', t2[:], y[:], y[:], op=ALU.mult, reads=[y, t2], writes=[t2])
                P.E('dve', 'reduce_sum', ss[:], t2[:].rearrange("p (g f) -> p g f", g=2), axis=AX.X, reads=[t2], writes=[ss])
                P.E('act', 'activation', ss[:], ss[:], AF.Sqrt, bias=self.epsr[:, 0:1], scale=1.0 / 256.0, reads=[ss, self.epsr], writes=[ss])
                P.E('dve', 'reciprocal', ss[:], ss[:], reads=[ss], writes=[ss])
                P.E('dve', 'tensor_tensor', y[:].rearrange("p (g f) -> p g f", g=2), y[:].rearrange("p (g f) -> p g f", g=2),
                    pap(ss, 0, 0, [128, [1, 2], [0, 256]]), op=ALU.mult, reads=[y, ss], writes=[y])
                P.E('pool', 'tensor_tensor', yb[:], y[:], nwb[:], op=ALU.mult, reads=[y, nwb], writes=[yb])
                for ft in range(4):
                    P.E('pe', 'transpose', ytp[:, ft, :], yb[:, ft * 128:(ft + 1) * 128], self.ident[:], reads=[yb, self.ident], writes=[ytp])
                st_ = ysT[(c // 4) % 2]
                P.E('act', 'copy', st_[:, :, (c % 4) * 128:(c % 4 + 1) * 128], ytp[:], reads=[ytp], writes=[(st_, c % 4)])
                if c % 4 == 3 or c == NC - 1:
                    c4 = (c // 4) * 4
                    nt_ = (c - c4 + 1) * 128
                    for ft in range(4):
                        P.dma(yT.ap[512 + ft * 128:512 + (ft + 1) * 128, c4 * 128:c4 * 128 + nt_], st_[:, ft, 0:nt_], reads=[st_],
                              writes=[(yT, 4 + ft)], multi=True, eng='act')
        if ctxmode:
            P.E('pool', 'tensor_copy', self.Sf0[:], Sf[:], reads=[Sf], writes=[self.Sf0])
        P.release(m)


PERM = [0, 4, 1, 5, 2, 6, 3, 7]


def attn_consts():
    t = np.arange(L)
    row = (t // 64).astype(np.float32); col = (t % 64).astype(np.float32)
    inv = (10000.0 ** (-np.arange(16, dtype=np.float32) / 16)).astype(np.float32)
    ang = np.stack([row[:, None] * inv[None], col[:, None] * inv[None]], 1).astype(np.float32)
    cs = np.stack([np.cos(ang), np.sin(ang)], 1).reshape(L, 2, 32)
    rope = cs.reshape(L // 128, 128, 2, 32).transpose(1, 0, 2, 3)
    r = np.arange(128)[:, None]; c = np.arange(128)[None, :]
    am = np.stack([(r >= c), (r <= c)], 1).astype(np.float32)
    return dict(arope=np.ascontiguousarray(rope).astype(np.float32), amask=bf16_np(am))


class AttnMixin:
    def declare_attn(self):
        c = attn_consts()
        self.arope = self.const("arope", c['arope'])
        self.amask = self.const("amask", c['amask'], BF16)
        self.sinks = self.inp("sinks", [DEPTH, 8])

    def phase_attn(self, i, do_ctx):
        P = self.P
        m = P.mark()
        NT = L // 128
        rope = P.sbuf("rope", [128, NT, 2, 32], F32); P.dma(rope[:], self.arope.ap, writes=[rope])
        msk = P.sbuf("amsk", [128, 2, 128], BF16); P.dma(msk[:], self.amask.ap, writes=[msk])
        esk = P.sbuf("esk", [128, 8], F32); P.dma(esk[:], self.sinks.ap[i:i + 1, :].partition_broadcast(128), writes=[esk])
        P.E('act', 'activation', esk[:], esk[:], AF.Exp, reads=[esk], writes=[esk])
        QT = P.sbuf("QT", [128, 4, L], BF16); KT = P.sbuf("KT", [128, L], BF16)
        Va = P.sbuf("Va", [128, NT, 2, 65], BF16)
        QcT = P.sbuf("QcT", [128, 4, LC], BF16); KcT = P.sbuf("KcT", [128, LC], BF16)
        Vc = P.sbuf("Vc", [128, 2, 2, 65], BF16)
        P.E('pool', 'memset', Va[:], 1.0, writes=[Va]); P.E('pool', 'memset', Vc[:], 1.0, writes=[Vc])
        uin = [P.sbuf("auin%d" % k, [128, 1280], BF16) for k in range(2)]
        qf = P.sbuf("aqf", [128, 640], F32)
        ta = [P.sbuf("ata%d" % k, [128, 320], F32) for k in range(4)]
        qr = P.sbuf("aqr", [128, 640], BF16)
        tp = [P.psum("atp%d" % k, [128, 5, 128], BF16) for k in range(2)]
        for t in range(NT + 2):
            isctx = t >= NT
            tt = t - NT if isctx else t
            src = self.uc_at if isctx else self.u_at
            ui = uin[t % 2]; ps = tp[t % 2]
            P.dma(ui[:], src.ap[tt * 128:(tt + 1) * 128, :], reads=[src], writes=[ui])
            if not isctx:
                P.E('act', 'copy', qf[:], ui[:, 0:640], reads=[ui], writes=[qf])
                xv = lambda off: pap(qf, 0, off, [128, [64, 10], [32, 2], [1, 16]])
                ov = lambda off: pap(qr, 0, off, [128, [64, 10], [32, 2], [1, 16]])
                tv = lambda k: ta[k][:].rearrange("p (h a f) -> p h a f", h=10, a=2)
                cs = lambda k: pap(rope, 0, (t * 2 + k) * 32, [128, [0, 10], [16, 2], [1, 16]])
                P.E('dve', 'tensor_tensor', tv(0), xv(0), cs(0), op=ALU.mult, reads=[qf, rope], writes=[ta[0]])
                P.E('dve', 'tensor_tensor', tv(1), xv(16), cs(1), op=ALU.mult, reads=[qf, rope], writes=[ta[1]])
                P.E('dve', 'tensor_tensor', ov(0), tv(0), tv(1), op=ALU.subtract, reads=[ta[0], ta[1]], writes=[(qr, 0)])
                P.E('pool', 'tensor_tensor', tv(2), xv(0), cs(1), op=ALU.mult, reads=[qf, rope], writes=[ta[2]])
                P.E('pool', 'tensor_tensor', tv(3), xv(16), cs(0), op=ALU.mult, reads=[qf, rope], writes=[ta[3]])
                P.E('pool', 'tensor_tensor', ov(16), tv(2), tv(3), op=ALU.add, reads=[ta[2], ta[3]], writes=[(qr, 1)])
                qsrc = qr
            else:
                qsrc = ui
            for j in range(5):
                P.E('pe', 'transpose', ps[:, j, :], qsrc[:, j * 128:(j + 1) * 128], self.ident[:], reads=[qsrc, self.ident], writes=[ps])
            Qd, Kd, Vd = (QcT, KcT, Vc) if isctx else (QT, KT, Va)
            Lq = LC if isctx else L
            P.E('act', 'copy', pap(Qd, 0, tt * 128, [128, [Lq, 4], [1, 128]]), ps[:, 0:4, :], reads=[ps], writes=[(Qd, tt)])
            P.E('act', 'copy', Kd[:, tt * 128:(tt + 1) * 128], ps[:, 4, :], reads=[ps], writes=[(Kd, tt)])
            P.E('pool', 'tensor_copy', Vd[:, tt, :, 0:64], ui[:, 640:768].rearrange("p (g d) -> p g d", g=2), reads=[ui, Vd], writes=[(Vd, tt)])
        scp = [P.psum("ascp%d" % k, [128, 4, 128], F32) for k in range(2)]
        opp = [P.psum("aopp%d" % k, [128, 4, 65], F32) for k in range(2)]
        ytp = P.psum("aytp", [128, 4, 128], BF16)
        PT = [P.sbuf("aPT%d" % k, [128, 4, 128], BF16) for k in range(10)]
        gt = [P.sbuf("agt%d" % k, [128, 512], BF16) for k in range(2)]
        den = P.sbuf("aden", [128, 8], F32)
        yat = P.sbuf("ayat", [128, 512], F32)
        sg = P.sbuf("asg", [128, 512], F32)
        yb = P.sbuf("ayb", [128, 512], BF16)
        yaT = [P.sbuf("ayaT%d" % k, [128, 4, 512], BF16) for k in range(2)]
        nsc = 0

        def qtile(isctx, i_):
            nonlocal nsc
            Qd = QcT if isctx else QT
            Lq = LC if isctx else L
            src = self.uc_at if isctx else self.u_at
            ydst = self.ycT if isctx else self.yT
            keys = []
            if not isctx:
                if i_ > 0: keys.append((KT, Va, i_ - 1, 0))
                keys.append((KT, Va, i_, None))
                if i_ < NT - 1: keys.append((KT, Va, i_ + 1, 1))
            keys += [(KcT, Vc, 0, None), (KcT, Vc, 1, None)]
            g_ = gt[i_ % 2]
            P.dma(g_[:], src.ap[i_ * 128:(i_ + 1) * 128, 768 + 0:768 + 512] if False else src.ap[i_ * 128:(i_ + 1) * 128, 768:1280], reads=[src], writes=[g_])
            for g in range(2):
                pts = []
                for kidx, (Kd, Vd, kt, mk_) in enumerate(keys):
                    sp_ = scp[nsc % 2]; nsc += 1
                    pt = PT[g * 5 + kidx]
                    pts.append(pt)
                    for j in range(4):
                        P.E('pe', 'matmul', sp_[:, j, :], Kd[g * 64:(g + 1) * 64, kt * 128:(kt + 1) * 128],
                            Qd[g * 64:(g + 1) * 64, j, i_ * 128:(i_ + 1) * 128], start=True, stop=True,
                            reads=[(Kd, kt), (Qd, i_)], writes=[sp_])
                    P.E('act', 'activation', pt[:], sp_[:], AF.Exp, scale=0.125, reads=[sp_], writes=[pt])
                    if mk_ is not None:
                        P.E('pool', 'tensor_tensor', pt[:], pt[:], msk[:, mk_, :].unsqueeze(1).to_broadcast([128, 4, 128]), op=ALU.mult,
                            reads=[pt, msk], writes=[pt])
                op_ = opp[g]
                for j in range(4):
                    for kidx, (Kd, Vd, kt, mk_) in enumerate(keys):
                        P.E('pe', 'matmul', op_[:, j, :], pts[kidx][:, j, :], Vd[:, kt, g, :], start=(kidx == 0), stop=(kidx == len(keys) - 1),
                            reads=[pts[kidx], (Vd, kt)], writes=[op_])
            for g in range(2):
                op_ = opp[g]
                P.E('dve', 'tensor_tensor', pap(den, 0, g, [128, [2, 4]]), pap(op_, 0, 64, [128, [65, 4]]), pap(esk, 0, g, [128, [2, 4]]), op=ALU.add,
                    reads=[op_, esk], writes=[(den, g)])
            P.E('dve', 'reciprocal', den[:], den[:], reads=[den], writes=[den])
            for g in range(2):
                op_ = opp[g]
                P.E('dve', 'tensor_tensor', pap(yat, 0, g * 64, [128, [128, 4], [1, 64]]), pap(op_, 0, 0, [128, [65, 4], [1, 64]]),
                    pap(den, 0, g, [128, [2, 4], [0, 64]]), op=ALU.mult, reads=[op_, den], writes=[(yat, g)])
            P.E('act', 'activation', sg[:], g_[:], AF.Silu, reads=[g_], writes=[sg])
            P.E('pool', 'tensor_tensor', yb[:], yat[:], sg[:], op=ALU.mult, reads=[yat, sg], writes=[yb])
            for ft in range(4):
                P.E('pe', 'transpose', ytp[:, ft, :], yb[:, ft * 128:(ft + 1) * 128], self.ident[:], reads=[yb, self.ident], writes=[ytp])
            st_ = yaT[(i_ // 4) % 2]
            P.E('act', 'copy', st_[:, :, (i_ % 4) * 128:(i_ % 4 + 1) * 128], ytp[:], reads=[ytp], writes=[(st_, i_ % 4)])
            last = (LC // 128 - 1) if isctx else (NT - 1)
            if i_ % 4 == 3 or i_ == last:
                c4 = (i_ // 4) * 4
                nt_ = (i_ - c4 + 1) * 128
                for ft in range(4):
                    P.dma(ydst.ap[1024 + ft * 128:1024 + (ft + 1) * 128, c4 * 128:c4 * 128 + nt_], st_[:, ft, 0:nt_], reads=[st_],
                          writes=[(ydst, 8 + ft)], multi=True, eng='act')

        if do_ctx:
            for i_ in range(LC // 128):
                qtile(True, i_)
        for i_ in range(NT if ATT_TILES is None else ATT_TILES):
            qtile(False, i_)
        P.release(m)

    def phase_out(self, i, ctxmode):
        P = self.P
        m = P.mark()
        Lx = LC if ctxmode else L
        yT = self.ycT if ctxmode else self.yT
        hsrc = (self.ctx if i == 0 else self.hc1) if ctxmode else (self.x if i == 0 else self.h1)
        hdst = self.hc1 if ctxmode else (self.h1 if i < DEPTH - 1 else self.out)
        gbc = self.gc_bc if ctxmode else self.g_bc[i]
        Wo = P.sbuf("Wo", [128, 12, D], BF16)
        stg = [P.sbuf("wostg%d" % k, [128, D], F32) for k in range(2)]
        for kc in range(12):
            st = stg[kc % 2]
            P.dma(st[:], self.w_out.ap[i, kc * 128:(kc + 1) * 128, :], writes=[st])
            P.E('pool', 'tensor_copy', Wo[:, kc, :], st[:], reads=[st], writes=[(Wo, kc)])
        lng = P.sbuf("lng", [128, D], F32); P.dma(lng[:], self.ln_g.ap[i:i + 1, :].partition_broadcast(128), writes=[lng])
        lnb = P.sbuf("lnb", [128, D], F32); P.dma(lnb[:], self.ln_b.ap[i:i + 1, :].partition_broadcast(128), writes=[lnb])
        yin = [P.sbuf("yin%d" % k, [128, 12, 512], BF16) for k in range(2)]
        hin = [P.sbuf("hin%d" % k, [128, D], F32) for k in range(2)]
        ops = [P.psum("oops%d" % k, [128, 2, 512], F32) for k in range(2)]
        tt = P.sbuf("ott", [128, D], F32); rr = [P.sbuf("orr%d" % k, [128, D], F32) for k in range(2)]
        stt = P.sbuf("ostt", [128, 2, 6], F32); mv = P.sbuf("omv", [128, 2], F32); rstd = P.sbuf("orstd", [128, 1], F32)
        n = 0
        for T in range((Lx + 511) // 512):
            ntok = min(512, Lx - T * 512)
            yi = yin[T % 2]
            for kc in range(12):
                P.dma(yi[:, kc, 0:ntok], yT.ap[kc * 128:(kc + 1) * 128, T * 512:T * 512 + ntok], reads=[(yT, kc)], writes=[(yi, kc)])
            for s in range(ntok // 128):
                r0 = T * 512 + s * 128
                ps = ops[n % 2]; hi = hin[n % 2]; r_ = rr[n % 2]; n += 1
                P.dma(hi[:], hsrc.ap[r0:r0 + 128, :], reads=[hsrc], writes=[hi])
                for nn in range(2):
                    for kc in range(12):
                        P.E('pe', 'matmul', ps[:, nn, :], yi[:, kc, s * 128:(s + 1) * 128], Wo[:, kc, nn * 512:(nn + 1) * 512],
                            start=(kc == 0), stop=(kc == 11), reads=[(yi, kc), (Wo, kc)], writes=[ps])
                P.E('dve', 'tensor_tensor', tt[:], ps[:].rearrange("p a b -> p (a b)"), gbc[:], op=ALU.mult, reads=[ps, gbc], writes=[tt])
                P.E('dve', 'scalar_tensor_tensor', tt[:], hi[:], float(ALPHA), tt[:], op0=ALU.mult, op1=ALU.add, reads=[hi, tt], writes=[tt])
                for hf in range(2):
                    P.E('dve', 'bn_stats', stt[:, hf, :], tt[:, hf * 512:(hf + 1) * 512], reads=[tt], writes=[(stt, hf)])
                P.E('dve', 'bn_aggr', mv[:], stt[:], reads=[stt], writes=[mv])
                P.E('act', 'activation', rstd[:], mv[:, 1:2], AF.Sqrt, bias=self.epsln[:, 0:1], reads=[mv, self.epsln], writes=[rstd])
                P.E('dve', 'reciprocal', rstd[:], rstd[:], reads=[rstd], writes=[rstd])
                P.E('dve', 'tensor_scalar', r_[:], tt[:], mv[:, 0:1], rstd[:, 0:1], op0=ALU.subtract, op1=ALU.mult, reads=[tt, mv, rstd], writes=[r_])
                P.E('pool', 'tensor_tensor', r_[:], r_[:], lng[:], op=ALU.mult, reads=[r_, lng], writes=[r_])
                P.E('pool', 'tensor_tensor', r_[:], r_[:], lnb[:], op=ALU.add, reads=[r_, lnb], writes=[r_])
                P.dma(hdst.ap[r0:r0 + 128, :], r_[:], reads=[r_], writes=[(hdst, 0)], multi=True, eng='act')
        P.release(m)


ATT_TILES = None


class MK(MKBase, HyenaMixin, SSDMixin, AttnMixin):
    def build(self, skip_ctx_hyena=False):
        P = self.P
        self.declare(); self.declare_hyena(); self.declare_ssd(); self.declare_attn()
        for i in range(DEPTH):
            last = i == DEPTH - 1
            self.phase_mod(i)
            self.phase_in(i)
            self.phase_hyfilt(i)
            self.phase_hyena(i)
            if not last:
                if skip_ctx_hyena:
                    m = P.mark()
                    z = P.sbuf("zfill", [128, LC], BF16)
                    P.E('pool', 'memset', z[:], 0.0, writes=[z])
                    for g in range(4):
                        P.dma(self.ycT.ap[g * 128:(g + 1) * 128, :], z[:], reads=[z], writes=[(self.ycT, g)])
                    P.release(m)
                else:
                    self.phase_hyfilt(i, "c")
                    self.phase_hyena(i, "c")
            self.phase_ssd(i, True, want_y=not last)
            self.phase_ssd(i, False)
            self.phase_attn(i, do_ctx=not last)
            if not last:
                self.phase_out(i, True)
            self.phase_out(i, False)
        P.emit()
        return self


def blockdiag2(w):
    z = np.zeros((128, 128), np.float32); z[:64, :64] = w; z[64:, 64:] = w; return z
def host_inputs(inp, b):
    d = {}
    d['x'] = np.ascontiguousarray(inp['x'][b]); d['ctx'] = np.ascontiguousarray(inp['ctx'][b])
    cv = np.zeros((128, 16), np.float32)
    cv[:, 0:8] = inp['c'][b].reshape(8, 128).T; cv[:, 8:16] = inp['c_ctx'].reshape(8, 128).T
    d['cvec'] = cv
    d['w_mod'] = inp['w_mod']; d['b_mod'] = inp['b_mod']
    d['b_modT'] = np.ascontiguousarray(inp['b_mod'].reshape(DEPTH, 24, 128).transpose(0, 2, 1))
    PERM = [0, 4, 1, 5, 2, 6, 3, 7]
    hp = np.concatenate([np.arange(h * 64, (h + 1) * 64) for h in PERM])
    w_in = inp['w_in'].copy()
    w_in[:, :, 3600:4112] = inp['w_in'][:, :, 3600 + hp]
    w_in[:, :, 4368:4880] = inp['w_in'][:, :, 4368 + hp]
    w_out = inp['w_out'].copy()
    w_out[:, 1024:1536] = inp['w_out'][:, 1024 + hp]
    d['w_in'] = w_in; d['w_out'] = w_out
    d['sinks'] = np.ascontiguousarray(inp['attn_sinks'][:, PERM]); d['ln_g'] = inp['ln_g']; d['ln_b'] = inp['ln_b']
    d['hy_cw'] = np.ascontiguousarray(inp['hy_conv_w'].reshape(DEPTH, 3, 12, 128).transpose(0, 3, 2, 1))
    d['hy_cb'] = np.ascontiguousarray(inp['hy_conv_b'].reshape(DEPTH, 12, 128).transpose(0, 2, 1))
    d['hy_b2'] = np.ascontiguousarray(inp['hy_bias'].reshape(DEPTH, 2, 4, 128).transpose(0, 3, 2, 1))
    d['hy_w1'] = np.ascontiguousarray(np.concatenate([inp['hy_f_w1'], inp['hy_f_w1']], 2))
    d['hy_w23'] = np.stack([np.stack([blockdiag2(inp['hy_f_w2'][i]), blockdiag2(inp['hy_f_w3'][i])]) for i in range(DEPTH)])
    fb = np.stack([inp['hy_f_freq'], inp['hy_f_b1'], inp['hy_f_b2'], inp['hy_f_b3']], -1)
    d['hy_fb'] = np.ascontiguousarray(np.concatenate([fb, fb], 1))
    wo = inp['hy_f_wout'].reshape(DEPTH, 64, 2, 2, 512)
    d['hy_wo'] = np.ascontiguousarray(np.concatenate([wo[:, :, :, 0], wo[:, :, :, 1]], 1))
    d['ssm_cw'] = np.ascontiguousarray(inp['ssm_conv_w'].reshape(DEPTH, 3, 8, 128).transpose(0, 3, 2, 1))
    d['ssm_cb'] = np.ascontiguousarray(inp['ssm_conv_b'].reshape(DEPTH, 8, 128).transpose(0, 2, 1))
    d['ssm_dtb'] = np.ascontiguousarray(inp['ssm_dt_bias'].reshape(DEPTH, 16))
    d['ssm_alog'] = np.ascontiguousarray(inp['ssm_a_log'].reshape(DEPTH, 16))
    d['ssm_dd'] = inp['ssm_d']; d['ssm_nw'] = inp['ssm_norm_w']
    return d


_PROG = {}
SKIP_CTX_HYENA = False


def kernel(**inputs):
    inp = {k: np.asarray(v) for k, v in inputs.items()}
    if 'mk' not in _PROG:
        _PROG['mk'] = MK().build(skip_ctx_hyena=SKIP_CTX_HYENA)
    mk = _PROG['mk']
    nb = inp['x'].shape[0]
    in_maps = []
    for b in range(nb):
        d = host_inputs(inp, b)
        d.update(mk.consts)
        in_maps.append({k: np.ascontiguousarray(v) for k, v in d.items() if k in mk.inputs})
    res = run_bass_kernel_spmd(mk.nc, in_maps, core_ids=list(range(nb)))
    return np.stack([np.asarray(r['out'], dtype=np.float32) for r in res.results], 0)
```
